# Optimizing a Trainium2 kernel written in Bass

```python
import jax, jax.numpy as jnp
from jax import lax
import numpy as np


D_MODEL = 1024
BATCH = 4
SEQ = 8192
DEPTH = 1

CHUNK = 64
Q_BLOCK = 128
SB_HEADS = 8
SB_HEAD_DIM = 64
SB_WIDTH = SB_HEADS * SB_HEAD_DIM
CONV_WIDTH = D_MODEL // 2
CONV_K = 3
N_GROUPS = 4
EXPERTS_PER_GROUP = 8
N_EXPERTS = N_GROUPS * EXPERTS_PER_GROUP
TOP_K = 2
D_EXPERT = D_MODEL // 2
EXPERT_BLOCK = 128
PROJ_WIDTH = 3 * SB_WIDTH + 3 * CONV_WIDTH + 2 * D_MODEL
DEEPNORM_ALPHA = (2.0 * DEPTH) ** 0.25
DEEPNORM_BETA = (8.0 * DEPTH) ** -0.25
LN_EPS = 1e-5

kernel_name = 'hybrid_stickbreak_shortconv_hmoe'


def layer_norm(x, g, b):
    xf = x.astype(jnp.float32)
    mu = jnp.mean(xf, axis=-1, keepdims=True)
    var = jnp.mean(jnp.square(xf - mu), axis=-1, keepdims=True)
    return ((xf - mu) * lax.rsqrt(var + LN_EPS) * g + b).astype(x.dtype)


def stick_breaking_attention(q, k, v):
    bsz, h, s, dh = q.shape
    nq = s // Q_BLOCK
    scale = dh ** -0.5
    q_blocks = q.reshape(bsz, h, nq, Q_BLOCK, dh).transpose(2, 0, 1, 3, 4)
    kf = k.astype(jnp.float32)
    vf = v.astype(jnp.float32)
    k_pos = jnp.arange(s)

    def one_block(args):
        qb, blk = args
        z = jnp.einsum('bhqd,bhkd->bhqk', qb.astype(jnp.float32), kf) * scale
        q_pos = blk * Q_BLOCK + jnp.arange(Q_BLOCK)
        mask = k_pos[None, :] < q_pos[:, None]
        log_stay = jnp.where(mask, jax.nn.log_sigmoid(-z), 0.0)
        tail = lax.cumsum(log_stay, axis=3, reverse=True)
        tail = jnp.concatenate([tail[..., 1:], jnp.zeros_like(tail[..., :1])], axis=-1)
        weights = jnp.where(mask, jnp.exp(jax.nn.log_sigmoid(z) + tail), 0.0)
        return jnp.einsum('bhqk,bhkd->bhqd', weights, vf)

    out = lax.map(one_block, (q_blocks, jnp.arange(nq)))
    return out.transpose(1, 2, 0, 3, 4).reshape(bsz, h, s, dh).astype(q.dtype)


def short_gated_conv(cb, cc, ch, conv_w):
    u = cc * ch
    s = u.shape[1]
    u_pad = jnp.pad(u, ((0, 0), (CONV_K - 1, 0), (0, 0)))
    y = sum(conv_w[i] * u_pad[:, i:i + s] for i in range(CONV_K))
    return cb * y


def token_mixer(x, w_in, gate_bias, conv_w, w_branch_a, w_branch_b, w_out):
    bsz, s, _ = x.shape
    proj = x @ w_in
    splits = [SB_WIDTH, 2 * SB_WIDTH, 3 * SB_WIDTH,
              3 * SB_WIDTH + CONV_WIDTH, 3 * SB_WIDTH + 2 * CONV_WIDTH,
              3 * SB_WIDTH + 3 * CONV_WIDTH]
    q, k, v, cb, cc, ch, gate_logits = jnp.split(proj, splits, axis=-1)

    def heads(t):
        return t.reshape(bsz, s, SB_HEADS, SB_HEAD_DIM).transpose(0, 2, 1, 3)

    attn = stick_breaking_attention(heads(q), heads(k), heads(v))
    attn = attn.transpose(0, 2, 1, 3).reshape(bsz, s, SB_WIDTH)
    branch_a = attn @ w_branch_a
    branch_b = short_gated_conv(cb, cc, ch, conv_w) @ w_branch_b
    gates = jax.nn.sigmoid((gate_logits + gate_bias).astype(jnp.float32)).astype(x.dtype)
    gate_a, gate_b = jnp.split(gates, 2, axis=-1)
    return (gate_a * branch_a + gate_b * branch_b) @ w_out


def hierarchical_moe(x, w_router_g, b_router_g, w_router_e, b_router_e, w_gate, w_up, w_down):
    bsz, s, d = x.shape
    t = bsz * s
    xt = x.reshape(t, d)
    group_logits = (xt @ w_router_g).astype(jnp.float32) + b_router_g
    group_prob, group_idx = lax.top_k(jax.nn.softmax(group_logits, axis=-1), 1)
    expert_logits = (xt @ w_router_e).astype(jnp.float32).reshape(t, N_GROUPS, EXPERTS_PER_GROUP) + b_router_e
    in_group = jnp.take_along_axis(expert_logits, group_idx[:, :, None], axis=1)[:, 0]
    exp_prob, exp_idx = lax.top_k(jax.nn.softmax(in_group, axis=-1), TOP_K)
    exp_prob = exp_prob / jnp.sum(exp_prob, axis=-1, keepdims=True)
    route_w = (group_prob * exp_prob).reshape(-1)
    route_e = (group_idx * EXPERTS_PER_GROUP + exp_idx).reshape(-1)
    route_tok = jnp.repeat(jnp.arange(t, dtype=jnp.int32), TOP_K)
    n = t * TOP_K
    order = jnp.argsort(route_e)
    e_s = route_e[order]
    tok_s = route_tok[order]
    w_s = route_w[order]
    counts = jnp.bincount(route_e, length=N_EXPERTS)
    start = jnp.cumsum(counts) - counts
    padded = (counts + EXPERT_BLOCK - 1) // EXPERT_BLOCK * EXPERT_BLOCK
    pad_end = jnp.cumsum(padded)
    dest = pad_end[e_s] - padded[e_s] + jnp.arange(n, dtype=jnp.int32) - start[e_s]
    n_blocks = -(-n // EXPERT_BLOCK) + N_EXPERTS
    slot_tok = jnp.full((n_blocks * EXPERT_BLOCK,), t, jnp.int32).at[dest].set(tok_s)
    block_expert = jnp.minimum(
        jnp.searchsorted(pad_end, jnp.arange(n_blocks) * EXPERT_BLOCK, side='right'), N_EXPERTS - 1)
    x_pad = jnp.concatenate([xt, jnp.zeros((1, d), xt.dtype)], axis=0)
    x_blocks = x_pad[slot_tok].reshape(n_blocks, EXPERT_BLOCK, d)

    def expert_block(args):
        xb, e = args
        hidden = jax.nn.silu(xb @ w_gate[e]) * (xb @ w_up[e])
        return hidden @ w_down[e]

    y_slots = lax.map(expert_block, (x_blocks, block_expert)).reshape(-1, d)
    y_assign = y_slots[dest] * w_s[:, None].astype(x.dtype)
    y = jax.ops.segment_sum(y_assign, tok_s, num_segments=t)
    return y.reshape(bsz, s, d)


def setup_inputs(seed: int = 0) -> dict:
    key = jax.random.key(seed)
    ks = jax.random.split(key, 18)
    L, D = DEPTH, D_MODEL
    beta = DEEPNORM_BETA

    def nrm(k, shape, scale):
        return jax.random.normal(k, shape, jnp.float32) * scale

    col_scale = jnp.concatenate([
        jnp.ones((2 * SB_WIDTH,), jnp.float32), jnp.full((SB_WIDTH,), beta, jnp.float32),
        jnp.ones((2 * CONV_WIDTH,), jnp.float32), jnp.full((CONV_WIDTH,), beta, jnp.float32),
        jnp.ones((2 * D,), jnp.float32)])
    return {
        'x': nrm(ks[0], (BATCH, SEQ, D), 1.0),
        'w_in': nrm(ks[1], (L, D, PROJ_WIDTH), D ** -0.5) * col_scale,
        'gate_bias': nrm(ks[2], (L, 2 * D), 0.02),
        'conv_w': nrm(ks[3], (L, CONV_K, CONV_WIDTH), CONV_K ** -0.5),
        'w_branch_a': nrm(ks[4], (L, SB_WIDTH, D), SB_WIDTH ** -0.5 * beta),
        'w_branch_b': nrm(ks[5], (L, CONV_WIDTH, D), CONV_WIDTH ** -0.5 * beta),
        'w_out': nrm(ks[6], (L, D, D), D ** -0.5 * beta),
        'ln1_g': 1.0 + nrm(ks[7], (L, D), 0.02),
        'ln1_b': nrm(ks[8], (L, D), 0.02),
        'w_router_g': nrm(ks[9], (L, D, N_GROUPS), D ** -0.5),
        'b_router_g': nrm(ks[10], (L, N_GROUPS), 0.01),
        'w_router_e': nrm(ks[11], (L, D, N_EXPERTS), D ** -0.5),
        'b_router_e': nrm(ks[12], (L, N_GROUPS, EXPERTS_PER_GROUP), 0.01),
        'w_gate': nrm(ks[13], (L, N_EXPERTS, D, D_EXPERT), D ** -0.5 * beta),
        'w_up': nrm(ks[14], (L, N_EXPERTS, D, D_EXPERT), D ** -0.5 * beta),
        'w_down': nrm(ks[15], (L, N_EXPERTS, D_EXPERT, D), D_EXPERT ** -0.5 * beta),
        'ln2_g': 1.0 + nrm(ks[16], (L, D), 0.02),
        'ln2_b': nrm(ks[17], (L, D), 0.02),
    }


def reference(x, w_in, gate_bias, conv_w, w_branch_a, w_branch_b, w_out, ln1_g, ln1_b,
              w_router_g, b_router_g, w_router_e, b_router_e, w_gate, w_up, w_down,
              ln2_g, ln2_b):
    h = x
    for l in range(DEPTH):
        mixed = token_mixer(h, w_in[l], gate_bias[l], conv_w[l], w_branch_a[l], w_branch_b[l], w_out[l])
        h = layer_norm(DEEPNORM_ALPHA * h + mixed, ln1_g[l], ln1_b[l])
        ffn = hierarchical_moe(h, w_router_g[l], b_router_g[l], w_router_e[l], b_router_e[l],
                               w_gate[l], w_up[l], w_down[l])
        h = layer_norm(DEEPNORM_ALPHA * h + ffn, ln2_g[l], ln2_b[l])
    return h
```

```python
import os
import numpy as np
from contextlib import ExitStack
import concourse.bass as bass
import concourse.mybir as mybir
from concourse.bass_utils import run_bass_kernel_spmd

F32 = mybir.dt.float32
BF16 = mybir.dt.bfloat16
I32 = mybir.dt.int32
AF = mybir.ActivationFunctionType
ALU = mybir.AluOpType
AX = mybir.AxisListType

S = 8192
D = 1024
NOWN = 4096
NBLK = 32
CAP = 384
NSLOT = 32 * CAP
ALPHA = 2.0 ** 0.25
EPS = 1e-5
MASKV = -30000.0
NDMA = 12


class Buf:
    __slots__ = ("lw", "rd")

    def __init__(self):
        self.lw = {}
        self.rd = {}


class Ctx:
    def __init__(self, nc, es):
        self.nc = nc
        self.eng = {"pe": nc.tensor, "act": nc.scalar, "dve": nc.vector, "pool": nc.gpsimd, "sp": nc.sync}
        self.semobj = {}
        self.cnt = {}
        self.waited = {e: {} for e in self.eng}
        for e in self.eng:
            self.semobj[e] = es.enter_context(nc.semaphore("s_" + e))
            self.cnt[e] = 0
        self.rr = {"sp": 0, "pool": 0}
        for q in ("sp", "pool"):
            for i in range(NDMA):
                k = (q, i)
                self.semobj[k] = es.enter_context(nc.semaphore("d_%s%d" % (q, i)))
                self.cnt[k] = 0

    def _deps(self, reads, writes):
        need = {}
        for b in reads:
            for k, v in b.lw.items():
                if need.get(k, 0) < v:
                    need[k] = v
        for b in writes:
            for k, v in b.lw.items():
                if need.get(k, 0) < v:
                    need[k] = v
            for k, v in b.rd.items():
                if need.get(k, 0) < v:
                    need[k] = v
        return need

    def _wait(self, e, need):
        eng = self.eng[e]
        w = self.waited[e]
        for k, v in need.items():
            if k == e and e == "pe":
                continue
            if w.get(k, 0) >= v:
                continue
            eng.wait_ge(self.semobj[k], v)
            w[k] = v

    def _mark(self, ev, reads, writes):
        for b in writes:
            b.lw[ev[0]] = ev[1]
            b.rd = {}
        for b in reads:
            if b.rd.get(ev[0], 0) < ev[1]:
                b.rd[ev[0]] = ev[1]

    def op(self, e, fn, reads=(), writes=()):
        self._wait(e, self._deps(reads, writes))
        ins = fn()
        self.cnt[e] += 1
        ins.then_inc(self.semobj[e], 1)
        self._mark((e, self.cnt[e]), reads, writes)

    def dma(self, q, fn, reads=(), writes=()):
        need = self._deps(reads, writes)
        i = self.rr[q]
        self.rr[q] = (i + 1) % NDMA
        k = (q, i)
        if self.cnt[k] > 0:
            need[k] = max(need.get(k, 0), self.cnt[k])
        self._wait(q, need)
        ins = fn()
        self.cnt[k] += 16
        ins.then_inc(self.semobj[k], 16)
        self._mark((k, self.cnt[k]), reads, writes)

    def barrier(self):
        allv = {k: v for k, v in self.cnt.items() if v > 0}
        for e in self.eng:
            need = {k: v for k, v in allv.items() if k != e}
            self._wait(e, need)


def build(stage=3):
    nc = bass.Bass("TRN2", target_bir_lowering=False)

    def din(name, shape, dt=F32):
        return nc.dram_tensor(name, list(shape), dt, kind="ExternalInput").ap()

    xT = din("xT", [D, S])
    xTsh = din("xTsh", [D, S])
    xTsho = din("xTsho", [D, NOWN])
    xTs = din("xTs", [D, 32])
    xTo = din("xTo", [D, NOWN])
    xTh = din("xTh", [D, 64])
    xo = din("xo", [NOWN, D])
    w_in = din("w_in", [D, 5120])
    gbias = din("gbias", [128, 16])
    cw = din("cw", [128, 12])
    w_ba = din("w_ba", [512, D])
    w_bb = din("w_bb", [512, D])
    w_out = din("w_out", [D, D])
    lnp = din("lnp", [4, D])
    w_r = din("w_r", [D, 36])
    b_r = din("b_r", [36])
    w_gate = din("w_gate", [32, D, 512])
    w_up = din("w_up", [32, D, 512])
    w_down = din("w_down", [32, 512, D])
    consts = din("consts", [128, 128 * 3 + 256 + 32 + 256])
    out = nc.dram_tensor("out", [NOWN, D], F32, kind="ExternalOutput").ap()
    if stage == 1:
        attn_scr = nc.dram_tensor("attn_scr", [512, NOWN], BF16, kind="ExternalOutput").ap()
    else:
        attn_scr = nc.dram_tensor("attn_scr", [512, NOWN], BF16).ap()
    if stage == 2:
        h1_scr = nc.dram_tensor("h1_scr", [NOWN, D], F32, kind="ExternalOutput").ap()
        dbg_r = nc.dram_tensor("dbg_r", [128, 32 * 4], F32, kind="ExternalOutput").ap()
    else:
        h1_scr = nc.dram_tensor("h1_scr", [NOWN, D], F32).ap()
    xg_scr = nc.dram_tensor("xg_scr", [NSLOT, D], BF16).ap()
    vsh_scr = nc.dram_tensor("vsh_scr", [512, NOWN], BF16).ap()
    ys_scr = nc.dram_tensor("ys_scr", [NSLOT, D], F32).ap()

    w_in_v = w_in.rearrange("(k p) n -> p k n", p=128)
    xT_v = xT.rearrange("(k p) t -> p k t", p=128)
    xTo_v = xTo.rearrange("(k p) t -> p k t", p=128)
    xTh_v = xTh.rearrange("(k p) t -> p k t", p=128)
    xTs_v = xTs.rearrange("(k p) t -> p k t", p=128)
    xTsh_v = xTsh.rearrange("(k p) t -> p k t", p=128)
    xTsho_v = xTsho.rearrange("(k p) t -> p k t", p=128)

    with ExitStack() as es:
        E = es.enter_context
        cx = Ctx(nc, es)
        op, dma = cx.op, cx.dma

        def sb(st, name, shape, dt):
            return st.enter_context(nc.sbuf_tensor(name, list(shape), dt))

        ident_bf = sb(es, "ident_bf", [128, 128], BF16)
        ustr_bf = sb(es, "ustr_bf", [128, 128], BF16)
        ones_bf = sb(es, "ones_bf", [128, 128], BF16)
        mask_bf = sb(es, "mask_bf", [128, 256], BF16)
        ident_f = sb(es, "ident_f", [128, 128], F32)
        ebase = sb(es, "ebase", [128, 32], F32)
        D1 = sb(es, "D1", [128, 32], I32)
        D2 = sb(es, "D2", [128, 32], I32)
        RW1 = sb(es, "RW1", [128, 32], F32)
        RW2 = sb(es, "RW2", [128, 32], F32)
        zeros = sb(es, "zeros", [128, 512], F32)
        zeros_bf = sb(es, "zeros_bf", [128, 512], BF16)
        b_const = Buf()
        bc_reg = nc.gpsimd.alloc_register("bc_reg")
        nc.gpsimd.reg_mov(bc_reg, NSLOT - 1)
        dma("pool", lambda: nc.gpsimd.dma_start(out=ident_bf[:], in_=consts[:, 0:128]), writes=[b_const])
        dma("pool", lambda: nc.gpsimd.dma_start(out=ustr_bf[:], in_=consts[:, 128:256]), writes=[b_const])
        dma("pool", lambda: nc.gpsimd.dma_start(out=ones_bf[:], in_=consts[:, 256:384]), writes=[b_const])
        dma("pool", lambda: nc.gpsimd.dma_start(out=mask_bf[:], in_=consts[:, 384:640]), writes=[b_const])
        dma("sp", lambda: nc.sync.dma_start(out=ident_f[:], in_=consts[:, 0:128]), writes=[b_const])
        dma("sp", lambda: nc.sync.dma_start(out=ebase[:], in_=consts[:, 640:672]), writes=[b_const])
        mask01 = sb(es, "mask01", [128, 256], F32)
        dma("sp", lambda: nc.sync.dma_start(out=mask01[:], in_=consts[:, 672:928]), writes=[b_const])
        op("pool", lambda: nc.gpsimd.memset(zeros[:], 0.0), writes=[b_const])
        op("pool", lambda: nc.gpsimd.memset(zeros_bf[:], 0.0), writes=[b_const])
        ones_f = sb(es, "ones_f", [128, 1], F32)
        op("pool", lambda: nc.gpsimd.memset(ones_f[:], 1.0), writes=[b_const])
        epsT = sb(es, "epsT", [128, 1], F32)
        op("pool", lambda: nc.gpsimd.memset(epsT[:], EPS), writes=[b_const])
        cx.barrier()

        ps_stack = [None]

        def alloc_psum(tag, nf, nb):
            if ps_stack[0] is not None:
                ps_stack[0].close()
            st_ = ExitStack()
            es.callback(st_.close)
            ps_stack[0] = st_
            f_ = st_.enter_context(nc.psum_tensor("psf" + tag, [128, nf, 512], F32))
            b_ = st_.enter_context(nc.psum_tensor("psb" + tag, [128, nb, 1024], BF16))
            return f_, b_

        psf, psb = alloc_psum("A", 6, 2)
        bankbuf = [Buf() for _ in range(6)]
        pbbuf = [Buf(), Buf(), Buf()]
        bank_rr = [0]

        def next_bank(n=6):
            i = bank_rr[0] % n
            bank_rr[0] += 1
            return i

        evac_rr = [0]

        def evac(out_ap, in_ap, reads, writes, scale=None):
            evac_rr[0] += 1
            if evac_rr[0] % 2 == 0:
                if scale is None:
                    op("act", lambda: nc.scalar.copy(out=out_ap, in_=in_ap), reads=reads, writes=writes)
                else:
                    op("act", lambda: nc.scalar.mul(out=out_ap, in_=in_ap, mul=scale), reads=reads, writes=writes)
            else:
                if scale is None:
                    op("dve", lambda: nc.vector.tensor_copy(out=out_ap, in_=in_ap), reads=reads, writes=writes)
                else:
                    op("dve", lambda: nc.vector.tensor_scalar(out=out_ap, in0=in_ap, scalar1=scale, scalar2=None,
                                                              op0=ALU.mult), reads=reads, writes=writes)

        def mm_group(bank_ap, bankb, pairs, reads):
            def fn():
                n = len(pairs)
                ins = None
                for i, (l, r) in enumerate(pairs):
                    ins = nc.tensor.matmul(bank_ap, l, r, start=(i == 0), stop=(i == n - 1))
                return ins
            op("pe", fn, reads=reads, writes=[bankb])

        with ExitStack() as pa:
            KT = sb(pa, "KT", [128, 4, S], BF16)
            V = sb(pa, "dV", [128, 64, 512], BF16)
            VsT = sb(pa, "VsT", [128, 4, 32], F32)
            VsTb = Buf()
            KTb = [[Buf() for _ in range(4)] for _ in range(16)]
            Vb = [Buf() for _ in range(64)]
            QTb = [[Buf() for _ in range(4)] for _ in range(8)]
            with ExitStack() as pa1:
                wqkv = sb(pa1, "wqkv", [128, 8, 1024], BF16)
                wvn = sb(pa1, "wvn", [128, 8, 512], BF16)
                xs = sb(pa1, "xs", [128, 8, 32], BF16)
                xt = [sb(pa1, "xt%d" % i, [128, 8, 512], BF16) for i in range(2)]
                xtsh = [sb(pa1, "xtsh%d" % i, [128, 8, 512], BF16) for i in range(2)]
                xtb = [Buf(), Buf()]
                xtshb = [Buf(), Buf()]
                wb = Buf()
                dma("pool", lambda: nc.gpsimd.dma_start(out=wqkv[:, :, :], in_=w_in_v[:, :, 512:1536]), writes=[wb])
                dma("pool", lambda: nc.gpsimd.dma_start(out=xs[:], in_=xTs_v), writes=[wb])
                op("dve", lambda: nc.vector.tensor_scalar(out=wvn[:, :, :], in0=wqkv[:, :, 512:1024], scalar1=-1.0, scalar2=None,
                                                          op0=ALU.mult), reads=[wb], writes=[wb])
                for j in range(4):
                    bk = next_bank()
                    mm_group(psf[:, bk, 0:32], bankbuf[bk],
                             [(wqkv[:, k, 512 + j * 128:512 + (j + 1) * 128], xs[:, k, :]) for k in range(8)], reads=[wb])
                    op("dve", lambda: nc.vector.tensor_copy(out=VsT[:, j, :], in_=psf[:, bk, 0:32]), reads=[bankbuf[bk]], writes=[VsTb])
                for T in range(16):
                    sl = T % 2
                    dma("pool", lambda: nc.gpsimd.dma_start(out=xt[sl][:], in_=xT_v[:, :, T * 512:(T + 1) * 512]),
                        writes=[xtb[sl]])
                    dma("pool", lambda: nc.gpsimd.dma_start(out=xtsh[sl][:], in_=xTsh_v[:, :, T * 512:(T + 1) * 512]),
                        writes=[xtshb[sl]])
                    for j in range(4):
                        bk = next_bank()
                        mm_group(psf[:, bk, :], bankbuf[bk],
                                 [(wqkv[:, k, j * 128:(j + 1) * 128], xt[sl][:, k, :]) for k in range(8)],
                                 reads=[wb, xtb[sl]])
                        evac(KT[:, j, T * 512:(T + 1) * 512], psf[:, bk, :], [bankbuf[bk]], [KTb[T][j]])
                    for s in range(4):
                        bk = next_bank()
                        mm_group(psf[:, bk, :], bankbuf[bk],
                                 [(xtsh[sl][:, k, s * 128:(s + 1) * 128], wqkv[:, k, 512:1024]) for k in range(8)] +
                                 [(xt[sl][:, k, s * 128:(s + 1) * 128], wvn[:, k, :]) for k in range(8)],
                                 reads=[wb, xtb[sl], xtshb[sl]])
                        evac(V[:, 4 * T + s, :], psf[:, bk, :], [bankbuf[bk]], [Vb[4 * T + s]])
                cx.barrier()
            QT = sb(pa, "QT", [128, 4, NOWN], BF16)
            with ExitStack() as paq:
                wq = sb(paq, "wq", [128, 8, 512], BF16)
                wvq = sb(paq, "wvq", [128, 8, 512], BF16)
                xt = [sb(paq, "xtq%d" % i, [128, 8, 512], BF16) for i in range(1)] * 2
                xtso = [sb(paq, "xtso%d" % i, [128, 8, 512], BF16) for i in range(1)] * 2
                vstg = [sb(paq, "vstg%d" % i, [128, 512], BF16) for i in range(2)]
                xtb = [Buf()] * 2
                xtsob = [Buf()] * 2
                vstgb = [Buf(), Buf()]
                wb = Buf()
                vsh_w = vsh_scr.rearrange("(j p) t -> p j t", p=128)
                dma("pool", lambda: nc.gpsimd.dma_start(out=wq[:, :, :], in_=w_in_v[:, :, 0:512]), writes=[wb])
                dma("pool", lambda: nc.gpsimd.dma_start(out=wvq[:, :, :], in_=w_in_v[:, :, 1024:1536]), writes=[wb])
                for T in range(8):
                    sl = T % 2
                    dma("pool", lambda: nc.gpsimd.dma_start(out=xt[sl][:], in_=xTo_v[:, :, T * 512:(T + 1) * 512]),
                        writes=[xtb[sl]])
                    for j in range(4):
                        bk = next_bank()
                        mm_group(psf[:, bk, :], bankbuf[bk],
                                 [(wq[:, k, j * 128:(j + 1) * 128], xt[sl][:, k, :]) for k in range(8)],
                                 reads=[wb, xtb[sl]])
                        evac(QT[:, j, T * 512:(T + 1) * 512], psf[:, bk, :], [bankbuf[bk]], [QTb[T][j]], scale=0.125)
                    dma("pool", lambda: nc.gpsimd.dma_start(out=xtso[sl][:], in_=xTsho_v[:, :, T * 512:(T + 1) * 512]),
                        writes=[xtsob[sl]])
                    for j in range(4):
                        bk = next_bank()
                        vs_ = (T * 4 + j) % 2
                        mm_group(psf[:, bk, :], bankbuf[bk],
                                 [(wvq[:, k, j * 128:(j + 1) * 128], xtso[sl][:, k, :]) for k in range(8)],
                                 reads=[wb, xtsob[sl]])
                        evac(vstg[vs_][:, :], psf[:, bk, :], [bankbuf[bk]], [vstgb[vs_]])
                        dma("sp", lambda: nc.sync.dma_start(out=vsh_w[:, j, T * 512:(T + 1) * 512], in_=vstg[vs_][:, :]),
                            reads=[vstgb[vs_]])
                cx.barrier()

            if stage == 0:
                return nc
            psf, psb = alloc_psum("T", 5, 3)
            with ExitStack() as pa2:
                NS = 4
                Sb = [sb(pa2, "Sb%d" % i, [128, 512], F32) for i in range(NS)]
                Wb = [sb(pa2, "Wb%d" % i, [128, 512], BF16) for i in range(NS)]
                WTs = [sb(pa2, "WTs%d" % i, [128, 512], BF16) for i in range(NS)]
                carry = sb(pa2, "carry", [128, 8], F32)
                Obf = [sb(pa2, "Obf%d" % i, [128, 512], BF16) for i in range(2)]
                Sbb = [Buf() for _ in range(NS)]
                Wbb = [Buf() for _ in range(NS)]
                WTsb = [Buf() for _ in range(NS)]
                carryb = [Buf() for _ in range(8)]
                Obfb = [Buf(), Buf()]
                NZ = 3
                Ob = [Buf(), Buf()]
                attn_v = attn_scr.rearrange("(j p) t -> p j t", p=128)

                items = []
                for i in range(NBLK):
                    chunks = []
                    s0 = 2 * i
                    while s0 < 64:
                        e0 = min(64, (s0 // 4 + 1) * 4)
                        chunks.append((s0, e0))
                        s0 = e0
                    for c, (s0, e0) in enumerate(chunks):
                        for h in range(8):
                            items.append((i, c, len(chunks), s0, e0, h))
                NIT = len(items)

                QTz = [sb(pa2, "QTz%d" % i_, [128, 8, 128], BF16) for i_ in range(2)]
                QTzb = [Buf(), Buf()]
                Osb = [sb(pa2, "Osb%d" % i_, [128, 512], BF16) for i_ in range(2)]
                Osbb = [Buf(), Buf()]
                for i_ in range(2):
                    op("dve", lambda: nc.vector.memset(QTz[i_][:], 0.0), writes=[QTzb[i_]])

                def st_qk(t):
                    i, c, nch, s0, e0, h = items[t]
                    if c == 0 and h == 0:
                        dma("sp", lambda: nc.sync.dma_start(out=vsh[i % 2][:], in_=vsh_r[:, :, i * 128:(i + 1) * 128]),
                            writes=[vshb[i % 2]])
                        qz = QTz[i % 2]
                        for jj in range(4):
                            op("act", lambda: nc.scalar.copy(out=qz[0:64, 2 * jj, :], in_=QT[0:64, jj, i * 128:(i + 1) * 128]),
                               reads=[QTb[i // 4][jj]], writes=[QTzb[i % 2]])
                            op("act", lambda: nc.scalar.copy(out=qz[64:128, 2 * jj + 1, :], in_=QT[64:128, jj, i * 128:(i + 1) * 128]),
                               reads=[QTb[i // 4][jj]], writes=[QTzb[i % 2]])
                    j, hb = h // 2, (h % 2) * 64
                    n = (e0 - s0) * 128
                    zb = t % NZ
                    rd = [QTzb[i % 2]] + [KTb[ss // 4][j] for ss in range(s0, e0, 4)] + [b_const]

                    def fn():
                        ins = nc.tensor.matmul(psf[:, zb, 0:n], QTz[i % 2][:, h, :],
                                               KT[:, j, s0 * 128:e0 * 128], start=True, stop=(c != 0))
                        if c == 0:
                            ins = nc.tensor.matmul(psf[:, zb, 0:256], ident_bf[:, :], mask_bf[:, :],
                                                   start=False, stop=True)
                        return ins
                    op("pe", fn, reads=rd, writes=[bankbuf[zb]])

                def st_sig(t):
                    i, c, nch, s0, e0, h = items[t]
                    n = (e0 - s0) * 128
                    zb = t % NZ
                    sl = t % NS
                    op("act", lambda: nc.scalar.activation(out=Sb[sl][:, 0:n], in_=psf[:, zb, 0:n], func=AF.Sigmoid,
                                                           scale=-1.0),
                       reads=[bankbuf[zb]], writes=[Sbb[sl]])

                if os.environ.get('ATT_C'):
                    Cb = [sb(pa2, "Cb%d" % i_, [128, 8], F32) for i_ in range(NS)]
                    Cbb = [Buf() for _ in range(NS)]

                if os.environ.get('ATT_E'):
                    Cf = [sb(pa2, "Cf%d" % i_, [128, 512], F32) for i_ in range(NS)]
                    Cfb = [Buf() for _ in range(NS)]

                def st_scan_old(t):
                    i, c, nch, s0, e0, h = items[t]
                    n = (e0 - s0) * 128
                    sl = t % NS
                    if c == 0:
                        op("pool", lambda: nc.gpsimd.memset(Cb[sl][:, 0:1], 1.0), writes=[Cbb[sl]])
                    else:
                        op("pool", lambda: nc.gpsimd.tensor_copy(out=Cb[sl][:, 0:1], in_=carry[:, h:h + 1]),
                           reads=[carryb[h]], writes=[Cbb[sl]])
                    op("dve", lambda: nc.vector.tensor_tensor_scan(out=Cb[sl][:, 1:n + 1], data0=Sb[sl][:, 0:n],
                                                                   data1=zeros[:, 0:n], initial=Cb[sl][:, 0:1],
                                                                   op0=ALU.mult, op1=ALU.add),
                       reads=[Sbb[sl], Cbb[sl]], writes=[Cbb[sl]])
                    if c != nch - 1:
                        op("pool", lambda: nc.gpsimd.tensor_copy(out=carry[:, h:h + 1], in_=Cb[sl][:, n:n + 1]),
                           reads=[Cbb[sl]], writes=[carryb[h]])
                    op("dve", lambda: nc.vector.tensor_tensor(out=Wb[sl][:, 0:n], in0=Cb[sl][:, 0:n],
                                                              in1=Cb[sl][:, 1:n + 1], op=ALU.subtract),
                       reads=[Cbb[sl]], writes=[Wbb[sl]])

                Cb = [sb(pa2, "Cb%d" % i_, [128, 8], F32) for i_ in range(NS)]
                Cbb = [Buf() for _ in range(NS)]
                vsh = [sb(pa2, "vsh%d" % i_, [128, 4, 128], BF16) for i_ in range(2)]
                vshb = [Buf(), Buf()]
                vsh_r = vsh_scr.rearrange("(j p) t -> p j t", p=128)

                def st_pre(t):
                    i, c, nch, s0, e0, h = items[t]
                    sl = t % NS
                    if c == 0:
                        op("pool", lambda: nc.gpsimd.memset(Cb[sl][:, 0:1], 1.0), writes=[Cbb[sl]])
                    else:
                        op("pool", lambda: nc.gpsimd.tensor_copy(out=Cb[sl][:, 0:1], in_=carry[:, h:h + 1]),
                           reads=[carryb[h]], writes=[Cbb[sl]])

                def st_scan_f(t):
                    i, c, nch, s0, e0, h = items[t]
                    n = (e0 - s0) * 128
                    sl = t % NS
                    op("dve", lambda: nc.vector.tensor_tensor_scan(out=Wb[sl][:, 0:n], data0=Sb[sl][:, 0:n],
                                                                   data1=zeros[:, 0:n], initial=Cb[sl][:, 0:1],
                                                                   op0=ALU.mult, op1=ALU.add),
                       reads=[Sbb[sl], Cbb[sl]], writes=[Wbb[sl]])
                    if c != nch - 1:
                        op("pool", lambda: nc.gpsimd.tensor_copy(out=carry[:, h:h + 1], in_=Wb[sl][:, n - 1:n]),
                           reads=[Wbb[sl]], writes=[carryb[h]])
                    if c == 0:
                        op("pool", lambda: nc.gpsimd.tensor_tensor(out=Wb[sl][:, 0:256], in0=Wb[sl][:, 0:256],
                                                                   in1=mask01[:, :], op=ALU.mult),
                           reads=[Wbb[sl], b_const], writes=[Wbb[sl]])

                def st_scan(t):
                    if True:
                        return st_scan_f(t)
                    if os.environ.get('ATT_C'):
                        return st_scan_old(t)
                    i, c, nch, s0, e0, h = items[t]
                    n = (e0 - s0) * 128
                    sl = t % NS
                    init = ones_f[:, 0:1] if c == 0 else carry[:, h:h + 1]
                    rd = [Sbb[sl]] + ([] if c == 0 else [carryb[h]])
                    if os.environ.get('ATT_E'):
                        op("dve", lambda: nc.vector.tensor_tensor_scan(out=Cf[sl][:, 0:n], data0=Sb[sl][:, 0:n],
                                                                       data1=zeros[:, 0:n], initial=init,
                                                                       op0=ALU.mult, op1=ALU.add),
                           reads=rd, writes=[Cfb[sl]])
                        op("act", lambda: nc.scalar.copy(out=Wb[sl][:, 0:n], in_=Cf[sl][:, 0:n]), reads=[Cfb[sl]], writes=[Wbb[sl]])
                    else:
                      op("dve", lambda: nc.vector.tensor_tensor_scan(out=Wb[sl][:, 0:n], data0=Sb[sl][:, 0:n],
                                                                   data1=zeros[:, 0:n], initial=init,
                                                                   op0=ALU.mult, op1=ALU.add),
                       reads=rd, writes=[Wbb[sl]])
                    if c != nch - 1:
                        ce = "dve" if os.environ.get('ATT_D3') else "pool"
                        cpe = nc.vector if os.environ.get('ATT_D3') else nc.gpsimd
                        op(ce, lambda: cpe.tensor_copy(out=carry[:, h:h + 1], in_=Wb[sl][:, n - 1:n]),
                           reads=[Wbb[sl]], writes=[carryb[h]])

                def st_tr(t):
                    i, c, nch, s0, e0, h = items[t]
                    nsb = e0 - s0
                    n = nsb * 128
                    sl = t % NS
                    hf = t % 2

                    def fn():
                        ins = None
                        for q in range(nsb):
                            ins = nc.tensor.transpose(psb[:, hf, q * 128:(q + 1) * 128],
                                                      Wb[sl][:, q * 128:(q + 1) * 128], ident_bf[:, :])
                        return ins
                    op("pe", fn, reads=[Wbb[sl], b_const], writes=[pbbuf[hf]])
                    if False:
                        op("dve", lambda: nc.vector.tensor_copy(out=WTs[sl][:, 0:n], in_=psb[:, hf, 0:n]),
                           reads=[pbbuf[hf]], writes=[WTsb[sl]])
                    else:
                        op("act", lambda: nc.scalar.copy(out=WTs[sl][:, 0:n], in_=psb[:, hf, 0:n]),
                           reads=[pbbuf[hf]], writes=[WTsb[sl]])

                def st_av(t):
                    i, c, nch, s0, e0, h = items[t]
                    nsb = e0 - s0
                    sl = t % NS
                    ob = i % 2

                    def fn():
                        ins = None
                        if c == 0 and h == 0:
                            nc.tensor.matmul(psf[:, 3 + ob, :], zeros_bf[:, 0:128], zeros_bf[:, :], start=True, stop=False,
                                             skip_group_check=True)
                        for q in range(nsb):
                            ins = nc.tensor.matmul(psf[:, 3 + ob, h * 64:(h + 1) * 64],
                                                   WTs[sl][:, q * 128:(q + 1) * 128],
                                                   V[:, s0 + q, h * 64:(h + 1) * 64],
                                                   start=False, stop=(c == nch - 1 and h == 7 and q == nsb - 1),
                                                   skip_group_check=True)
                        return ins
                    op("pe", fn, reads=[WTsb[sl]] + [Vb[ss] for ss in range(s0, e0)], writes=[Ob[ob]])
                    if c == nch - 1 and h == 7:
                        op("act", lambda: nc.scalar.copy(out=Osb[ob][:, :], in_=psf[:, 3 + ob, :]), reads=[Ob[ob]], writes=[Osbb[ob]])

                        def fnt():
                            ins = None
                            for jj in range(4):
                                ins = nc.tensor.transpose(psb[:, 2, jj * 128:(jj + 1) * 128], Osb[ob][:, jj * 128:(jj + 1) * 128],
                                                          ident_bf[:, :])
                            return ins
                        op("pe", fnt, reads=[Osbb[ob], b_const], writes=[pbbuf[2]])
                        op("dve", lambda: nc.vector.tensor_tensor(
                            out=Obf[ob][:, :].rearrange("p (j t) -> p j t", j=4),
                            in0=psb[:, 2, 0:512].rearrange("p (j t) -> p j t", j=4),
                            in1=vsh[ob][:, :, :], op=ALU.add),
                           reads=[pbbuf[2], vshb[ob]], writes=[Obfb[ob]])
                        dma("sp", lambda: nc.sync.dma_start(out=attn_v[:, :, i * 128:(i + 1) * 128],
                                                            in_=Obf[ob][:, :].rearrange("p (j t) -> p j t", j=4)),
                            reads=[Obfb[ob]])

                L1, L2, L3 = 1, 3, 4
                for t in range(NIT + L3 + 1):
                    if t < NIT:
                        st_qk(t)
                        st_sig(t)
                        st_pre(t)
                    if 0 <= t - L1 < NIT:
                        st_scan(t - L1)
                    if 0 <= t - L2 < NIT:
                        st_tr(t - L2)
                    if 0 <= t - L3 < NIT:
                        st_av(t - L3)
                cx.barrier()
        if stage == 1:
            cx.barrier()
            return nc
        psf, psb = alloc_psum("B", 6, 2)

        with ExitStack() as pb:
            wc = sb(pb, "wc", [128, 8, 1536], BF16)
            wg = sb(pb, "wg", [128, 8, 2048], BF16)
            wba = sb(pb, "wba", [128, 4, D], BF16)
            wbb = sb(pb, "wbb", [128, 4, D], BF16)
            wo = sb(pb, "wo", [128, 8, D], BF16)
            wr = sb(pb, "wr", [128, 8, 36], F32)
            gb_sb = sb(pb, "gb_sb", [128, 16], F32)
            cw_sb = sb(pb, "cw_sb", [128, 12], F32)
            g1 = sb(pb, "g1", [128, D], F32)
            b1 = sb(pb, "b1", [128, D], F32)
            br = sb(pb, "br", [128, 36], F32)
            base = sb(pb, "base", [128, 32], F32)
            wB = Buf()
            for c0 in range(0, 1536, 512):
                dma("pool", lambda: nc.gpsimd.dma_start(out=wc[:, :, c0:c0 + 512], in_=w_in_v[:, :, 1536 + c0:1536 + c0 + 512]), writes=[wB])
            for c0 in range(0, 2048, 512):
                dma("pool", lambda: nc.gpsimd.dma_start(out=wg[:, :, c0:c0 + 512], in_=w_in_v[:, :, 3072 + c0:3072 + c0 + 512]), writes=[wB])
            dma("pool", lambda: nc.gpsimd.dma_start(out=wba[:], in_=w_ba.rearrange("(k p) n -> p k n", p=128)), writes=[wB])
            dma("pool", lambda: nc.gpsimd.dma_start(out=wbb[:], in_=w_bb.rearrange("(k p) n -> p k n", p=128)), writes=[wB])
            dma("pool", lambda: nc.gpsimd.dma_start(out=wo[:], in_=w_out.rearrange("(k p) n -> p k n", p=128)), writes=[wB])
            dma("sp", lambda: nc.sync.dma_start(out=wr[:], in_=w_r.rearrange("(k p) n -> p k n", p=128)), writes=[wB])
            dma("sp", lambda: nc.sync.dma_start(out=gb_sb[:], in_=gbias), writes=[wB])
            dma("sp", lambda: nc.sync.dma_start(out=cw_sb[:], in_=cw), writes=[wB])
            dma("sp", lambda: nc.sync.dma_start(out=g1[:], in_=lnp[0].partition_broadcast(128)), writes=[wB])
            dma("sp", lambda: nc.sync.dma_start(out=b1[:], in_=lnp[1].partition_broadcast(128)), writes=[wB])
            dma("sp", lambda: nc.sync.dma_start(out=br[:], in_=b_r.partition_broadcast(128)), writes=[wB])
            op("pool", lambda: nc.gpsimd.memset(base[:], 0.0), writes=[wB])
            cx.barrier()

            xt = [sb(pb, "xtB%d" % i, [128, 8, 512], BF16) for i in range(2)]
            xh = [sb(pb, "xh%d" % i, [128, 8, 8], BF16) for i in range(2)]
            at = [sb(pb, "at%d" % i, [128, 4, 512], BF16) for i in range(2)]
            xtok = [sb(pb, "xtok%d" % i, [128, 4, D], F32) for i in range(1)]
            inb = [Buf(), Buf()]
            xtokb = Buf()
            dummy = sb(pb, "dummyB", [128, 1], F32)
            ccs = sb(pb, "ccs", [128, 512], F32)
            cchs = sb(pb, "cchs", [128, 8], F32)
            U = sb(pb, "U", [128, 4, 130], F32)
            Y = sb(pb, "Y", [128, 4, 128], F32)
            Bin = sb(pb, "Bin", [128, 4, 512], BF16)
            sa = [sb(pb, "sa%d" % i, [128, 512], F32) for i in range(1)] * 2
            sbg = [sb(pb, "sbg%d" % i, [128, 512], F32) for i in range(1)] * 2
            t1 = [sb(pb, "t1%d" % i, [128, 512], F32) for i in range(1)] * 2
            t2 = [sb(pb, "t2%d" % i, [128, 512], F32) for i in range(1)] * 2
            GT = sb(pb, "GT", [128, 8, 512], BF16)
            hbuf = [sb(pb, "hbuf%d" % i, [128, D], F32) for i in range(4)]
            h1b = [sb(pb, "h1b%d" % i, [128, D], BF16) for i in range(4)]
            h1T = [sb(pb, "h1T%d" % i, [128, 8, 128], F32) for i in range(2)]
            stats = [sb(pb, "stB%d" % i, [128, 2, 6], F32) for i in range(4)]
            mv = [sb(pb, "mvB%d" % i, [128, 2], F32) for i in range(4)]
            rstd = [sb(pb, "rstdB%d" % i, [128, 1], F32) for i in range(4)]
            Lg = sb(pb, "Lg", [128, 4, 36], F32)
            ccsb, Ub, Yb, Binb, GTb = Buf(), Buf(), Buf(), Buf(), Buf()
            sab = [Buf()] * 2
            t1b = [Buf()] * 2
            hb_ = [Buf() for _ in range(4)]
            h1bb = [Buf() for _ in range(4)]
            h1Tb, stb, Lgb = [Buf(), Buf()], [Buf() for _ in range(4)], Buf()
            R = {}
            for nm, shp in (("gmax", [128, 4]), ("ohg", [128, 4, 4]), ("eg", [128, 4, 4]), ("sumg", [128, 4]),
                            ("gp", [128, 4]), ("prod", [128, 4, 4, 8]), ("sel", [128, 4, 8]), ("m1", [128, 4]),
                            ("oh1", [128, 4, 8]), ("sel2", [128, 4, 8]), ("m2", [128, 4]), ("oh2", [128, 4, 8]),
                            ("dm", [128, 4]), ("w1", [128, 4]), ("w2", [128, 4]), ("ind8", [128, 4, 8]),
                            ("OH1", [128, 4, 32]), ("OH2", [128, 4, 32]), ("Rk", [128, 4, 32]),
                            ("ov", [128, 4, 32]), ("d1f", [128, 4]), ("d2f", [128, 4])):
                R[nm] = sb(pb, "r_" + nm, shp, F32)
            Ind = sb(pb, "Ind", [128, 4, 32], BF16)
            rb = Buf()
            xT_B = xTo_v
            attn_v = attn_scr.rearrange("(j p) t -> p j t", p=128)
            xo_v = xo.rearrange("(s p) d -> p s d", p=128)
            h1_v = h1_scr.rearrange("(s p) d -> p s d", p=128)

            def load_tile(T_):
                sl_ = T_ % 2
                dma("pool", lambda: nc.gpsimd.dma_start(out=xt[sl_][:], in_=xT_B[:, :, T_ * 512:(T_ + 1) * 512]), writes=[inb[sl_]])
                dma("pool", lambda: nc.gpsimd.dma_start(out=xh[sl_][:], in_=xTh_v[:, :, T_ * 8:(T_ + 1) * 8]), writes=[inb[sl_]])
                dma("sp", lambda: nc.sync.dma_start(out=at[sl_][:], in_=attn_v[:, :, T_ * 512:(T_ + 1) * 512]), writes=[inb[sl_]])

            pending_route = []

            def flush_route():
                while pending_route:
                    Tr = pending_route.pop(0)
                    routing(nc, op, dma, R, Ind, Lg, Lgb, rb, ustr_bf, ones_bf, b_const, psf, bankbuf, next_bank,
                            base, ebase, D1, D2, RW1, RW2, Tr, wB)
                    for s_ in range(4):
                        gs_ = 4 * Tr + s_
                        for Dk in (D1, D2):
                            dma("pool", lambda: nc.gpsimd.indirect_dma_start(
                                out=xg_scr, out_offset=bass.IndirectOffsetOnAxis(ap=Dk[:, gs_:gs_ + 1], axis=0),
                                in_=h1b[s_][:, :], in_offset=None, bounds_check=bc_reg, oob_is_err=False),
                                reads=[h1bb[s_], rb])

            load_tile(0)
            dma("sp", lambda: nc.sync.dma_start(out=xtok[0][:], in_=xo_v[:, 0:4, :]), writes=[xtokb])
            for T in range(8):
                sl = T % 2
                if T + 1 < 8:
                    load_tile(T + 1)
                for m in range(4):
                    bcc, bch, bcb, bh = next_bank(), next_bank(), next_bank(), next_bank()
                    mm_group(psf[:, bcc, :], bankbuf[bcc],
                             [(wc[:, k, 512 + m * 128:512 + (m + 1) * 128], xt[sl][:, k, :]) for k in range(8)], [wB, inb[sl]])
                    mm_group(psf[:, bch, :], bankbuf[bch],
                             [(wc[:, k, 1024 + m * 128:1024 + (m + 1) * 128], xt[sl][:, k, :]) for k in range(8)], [wB, inb[sl]])
                    mm_group(psf[:, bcb, :], bankbuf[bcb],
                             [(wc[:, k, m * 128:(m + 1) * 128], xt[sl][:, k, :]) for k in range(8)], [wB, inb[sl]])

                    def fnh():
                        ins = None
                        for k in range(8):
                            ins = nc.tensor.matmul(psf[:, bh, 0:8], wc[:, k, 512 + m * 128:512 + (m + 1) * 128],
                                                   xh[sl][:, k, :], start=(k == 0), stop=(k == 7))
                        for k in range(8):
                            ins = nc.tensor.matmul(psf[:, bh, 8:16], wc[:, k, 1024 + m * 128:1024 + (m + 1) * 128],
                                                   xh[sl][:, k, :], start=(k == 0), stop=(k == 7))
                        return ins
                    op("pe", fnh, reads=[wB, inb[sl]], writes=[bankbuf[bh]])
                    op("act", lambda: nc.scalar.copy(out=ccs[:, :], in_=psf[:, bcc, :]), reads=[bankbuf[bcc]], writes=[ccsb])
                    op("act", lambda: nc.scalar.copy(out=cchs[:, :], in_=psf[:, bh, 0:8]), reads=[bankbuf[bh]], writes=[ccsb])
                    op("dve", lambda: nc.vector.tensor_tensor(out=U[:, :, 0:128],
                                                              in0=ccs[:, :].rearrange("p (b t) -> p b t", b=4),
                                                              in1=psf[:, bch, :].rearrange("p (b t) -> p b t", b=4),
                                                              op=ALU.mult), reads=[ccsb, bankbuf[bch]], writes=[Ub])
                    op("dve", lambda: nc.vector.tensor_tensor(out=U[:, :, 128:130],
                                                              in0=cchs[:, :].rearrange("p (b t) -> p b t", b=4),
                                                              in1=psf[:, bh, 8:16].rearrange("p (b t) -> p b t", b=4),
                                                              op=ALU.mult), reads=[ccsb, bankbuf[bh]], writes=[Ub])
                    op("dve", lambda: nc.vector.tensor_scalar(out=Y[:, :, :], in0=U[:, :, 0:128],
                                                              scalar1=cw_sb[:, m * 3 + 2:m * 3 + 3], scalar2=None,
                                                              op0=ALU.mult), reads=[Ub, wB], writes=[Yb])
                    op("dve", lambda: nc.vector.scalar_tensor_tensor(out=Y[:, :, :], in0=U[:, :, 1:129],
                                                                     scalar=cw_sb[:, m * 3 + 1:m * 3 + 2], in1=Y[:, :, :],
                                                                     op0=ALU.mult, op1=ALU.add), reads=[Ub, Yb], writes=[Yb])
                    op("dve", lambda: nc.vector.scalar_tensor_tensor(out=Y[:, :, :], in0=U[:, :, 2:130],
                                                                     scalar=cw_sb[:, m * 3:m * 3 + 1], in1=Y[:, :, :],
                                                                     op0=ALU.mult, op1=ALU.add), reads=[Ub, Yb], writes=[Yb])
                    op("dve", lambda: nc.vector.tensor_tensor(out=Bin[:, m, :], in0=Y[:, :, :].rearrange("p b t -> p (b t)"),
                                                              in1=psf[:, bcb, :], op=ALU.mult),
                       reads=[Yb, bankbuf[bcb]], writes=[Binb])
                flush_route()
                for m in range(8):
                    s2 = m % 2
                    bA, bB, bga, bgb = next_bank(), next_bank(), next_bank(), next_bank()
                    mm_group(psf[:, bA, :], bankbuf[bA],
                             [(wba[:, k, m * 128:(m + 1) * 128], at[sl][:, k, :]) for k in range(4)], [wB, inb[sl]])
                    mm_group(psf[:, bB, :], bankbuf[bB],
                             [(wbb[:, k, m * 128:(m + 1) * 128], Bin[:, k, :]) for k in range(4)], [wB, Binb])
                    mm_group(psf[:, bga, :], bankbuf[bga],
                             [(wg[:, k, m * 128:(m + 1) * 128], xt[sl][:, k, :]) for k in range(8)], [wB, inb[sl]])
                    mm_group(psf[:, bgb, :], bankbuf[bgb],
                             [(wg[:, k, 1024 + m * 128:1024 + (m + 1) * 128], xt[sl][:, k, :]) for k in range(8)], [wB, inb[sl]])
                    op("act", lambda: nc.scalar.activation(out=sa[s2][:, :], in_=psf[:, bga, :], func=AF.Sigmoid,
                                                           bias=gb_sb[:, m:m + 1]), reads=[bankbuf[bga], wB], writes=[sab[s2]])
                    op("act", lambda: nc.scalar.activation(out=sbg[s2][:, :], in_=psf[:, bgb, :], func=AF.Sigmoid,
                                                           bias=gb_sb[:, 8 + m:9 + m]), reads=[bankbuf[bgb], wB], writes=[sab[s2]])
                    op("dve", lambda: nc.vector.tensor_tensor(out=t1[s2][:, :], in0=sa[s2][:, :], in1=psf[:, bA, :], op=ALU.mult),
                       reads=[sab[s2], bankbuf[bA]], writes=[t1b[s2]])
                    op("dve", lambda: nc.vector.tensor_tensor(out=t2[s2][:, :], in0=sbg[s2][:, :], in1=psf[:, bB, :], op=ALU.mult),
                       reads=[sab[s2], bankbuf[bB]], writes=[t1b[s2]])
                    op("pool", lambda: nc.gpsimd.tensor_tensor(out=GT[:, m, :], in0=t1[s2][:, :], in1=t2[s2][:, :], op=ALU.add),
                       reads=[t1b[s2]], writes=[GTb])
                for s in range(4):
                    s2 = s
                    bk0, bk1 = next_bank(), next_bank()
                    for half, bk in ((0, bk0), (1, bk1)):
                        mm_group(psf[:, bk, :], bankbuf[bk],
                                 [(GT[:, k, s * 128:(s + 1) * 128], wo[:, k, half * 512:(half + 1) * 512]) for k in range(8)],
                                 [wB, GTb])
                        op("dve", lambda: nc.vector.scalar_tensor_tensor(out=hbuf[s2][:, half * 512:(half + 1) * 512],
                                                                         in0=xtok[0][:, s, half * 512:(half + 1) * 512],
                                                                         scalar=ALPHA, in1=psf[:, bk, :],
                                                                         op0=ALU.mult, op1=ALU.add),
                           reads=[xtokb, bankbuf[bk]], writes=[hb_[s2]])
                if T + 1 < 8:
                    dma("sp", lambda: nc.sync.dma_start(out=xtok[0][:], in_=xo_v[:, 4 * (T + 1):4 * (T + 1) + 4, :]), writes=[xtokb])
                for s in range(4):
                    s2 = s
                    gs = 4 * T + s
                    layer_norm(nc, op, hbuf[s2], hb_[s2], stats[s], mv[s], rstd[s], stb[s], g1, b1, wB, epsT, gmul_on_dve=True)
                    dma("sp", lambda: nc.sync.dma_start(out=h1_v[:, gs, :], in_=hbuf[s2][:, :]), reads=[hb_[s2]])
                    op("act", lambda: nc.scalar.copy(out=h1b[s][:, :], in_=hbuf[s2][:, :]), reads=[hb_[s2]], writes=[h1bb[s]])
                for s in range(4):
                    s2 = s
                    hT_ = h1T[s % 2]
                    hTb_ = h1Tb[s % 2]
                    tb0, tb1 = next_bank(), next_bank()

                    def fntr():
                        ins = None
                        for k in range(8):
                            tb = tb0 if k < 4 else tb1
                            ins = nc.tensor.transpose(psf[:, tb, (k % 4) * 128:(k % 4 + 1) * 128],
                                                      hbuf[s2][:, k * 128:(k + 1) * 128], ident_f[:, :])
                        return ins
                    op("pe", fntr, reads=[hb_[s2], b_const], writes=[bankbuf[tb0], bankbuf[tb1]])
                    op("act", lambda: nc.scalar.copy(out=hT_[:, 0:4, :], in_=psf[:, tb0, :].rearrange("p (k t) -> p k t", k=4)),
                       reads=[bankbuf[tb0]], writes=[hTb_])
                    op("dve", lambda: nc.vector.tensor_copy(out=hT_[:, 4:8, :], in_=psf[:, tb1, :].rearrange("p (k t) -> p k t", k=4)),
                       reads=[bankbuf[tb1]], writes=[hTb_])
                    lb = next_bank()
                    mm_group(psf[:, lb, 0:36], bankbuf[lb],
                             [(hT_[:, k, :], wr[:, k, :]) for k in range(8)], [hTb_, wB])
                    op("dve", lambda: nc.vector.tensor_tensor(out=Lg[:, s, :], in0=psf[:, lb, 0:36], in1=br[:, :], op=ALU.add),
                       reads=[bankbuf[lb], wB], writes=[Lgb])
                pending_route.append(T)
            flush_route()
            cx.barrier()
            if stage == 2:
                op("dve", lambda: nc.vector.tensor_copy(out=zeros[:, 0:32], in_=D1[:, :]), writes=[b_const])
                op("dve", lambda: nc.vector.tensor_copy(out=zeros[:, 32:64], in_=D2[:, :]), writes=[b_const])
                op("dve", lambda: nc.vector.tensor_copy(out=zeros[:, 64:96], in_=RW1[:, :]), writes=[b_const])
                op("dve", lambda: nc.vector.tensor_copy(out=zeros[:, 96:128], in_=RW2[:, :]), writes=[b_const])
                dma("sp", lambda: nc.sync.dma_start(out=dbg_r, in_=zeros[:, 0:128]), reads=[b_const])
                cx.barrier()
                return nc

        with ExitStack() as pc:
            NW = 2
            wgs = [sb(pc, "wgs%d" % i, [128, 8, 512], BF16) for i in range(NW)]
            wus = [sb(pc, "wus%d" % i, [128, 8, 512], BF16) for i in range(NW)]
            wds = [sb(pc, "wds%d" % i, [128, 4, D], BF16) for i in range(NW)]
            wEb = [[Buf(), Buf(), Buf()] for _ in range(NW)]
            wgf = [sb(pc, "wgf%d" % i, [128, 8, 512], F32) for i in range(2)]
            wuf = [sb(pc, "wuf%d" % i, [128, 8, 512], F32) for i in range(2)]
            wdf = [sb(pc, "wdf%d" % i, [128, 4, D], F32) for i in range(2)]
            wFb = [[Buf(), Buf(), Buf()] for _ in range(2)]
            NX = 3
            xgs = [sb(pc, "xgs%d" % i, [128, 3, D], BF16) for i in range(NX)]
            xgb = [Buf() for _ in range(NX)]
            xbT = [sb(pc, "xbT%d" % i, [128, 8, CAP], BF16) for i in range(2)]
            xbTb = [Buf(), Buf()]
            sg = [sb(pc, "sg%d" % i, [128, CAP], F32) for i in range(2)]
            sgb = [Buf(), Buf()]
            hT = [sb(pc, "hT%d" % i, [128, 4, CAP], BF16) for i in range(2)]
            hTb = [Buf(), Buf()]
            ysb = [sb(pc, "ysb%d" % i, [128, D], F32) for i in range(2)]
            ysbb = [Buf(), Buf()]
            xg_v = xg_scr.rearrange("(e j p) d -> e p j d", p=128, j=3)
            ys_v = ys_scr.rearrange("(e j p) d -> e p j d", p=128, j=3)

            def load_w(e):
                fs = e % 2
                dma("sp", lambda: nc.sync.dma_start(out=wgf[fs][:], in_=w_gate[e].rearrange("(k p) f -> p k f", p=128)), writes=[wFb[fs][0]])
                dma("sp", lambda: nc.sync.dma_start(out=wuf[fs][:], in_=w_up[e].rearrange("(k p) f -> p k f", p=128)), writes=[wFb[fs][1]])
                dma("sp", lambda: nc.sync.dma_start(out=wdf[fs][:], in_=w_down[e].rearrange("(k p) f -> p k f", p=128)), writes=[wFb[fs][2]])
                dma("sp", lambda: nc.sync.dma_start(out=xgs[e % NX][:], in_=xg_v[e]), writes=[xgb[e % NX]])

            def cast_w(e):
                fs, sl = e % 2, e % NW
                for k in range(0, 8, 4):
                    op("act", lambda: nc.scalar.copy(out=wgs[sl][:, k:k + 4, :], in_=wgf[fs][:, k:k + 4, :]),
                       reads=[wFb[fs][0]], writes=[wEb[sl][0]])
                for k in range(0, 8, 4):
                    op("act" if k == 0 else "dve",
                       (lambda: nc.scalar.copy(out=wus[sl][:, k:k + 4, :], in_=wuf[fs][:, k:k + 4, :])) if k == 0 else
                       (lambda: nc.vector.tensor_copy(out=wus[sl][:, k:k + 4, :], in_=wuf[fs][:, k:k + 4, :])),
                       reads=[wFb[fs][1]], writes=[wEb[sl][1]])
                for k in range(0, 4, 2):
                    op("dve", lambda: nc.vector.tensor_copy(out=wds[sl][:, k:k + 2, :], in_=wdf[fs][:, k:k + 2, :]),
                       reads=[wFb[fs][2]], writes=[wEb[sl][2]])

            def do_transposes(e):
                sl = e % NX
                xb = xbT[e % 2]
                for j in range(3):
                    def fnt():
                        ins = None
                        for k in range(8):
                            ins = nc.tensor.transpose(psb[:, j % 2, k * 128:(k + 1) * 128], xgs[sl][:, j, k * 128:(k + 1) * 128],
                                                      ident_bf[:, :])
                        return ins
                    op("pe", fnt, reads=[xgb[sl], b_const], writes=[pbbuf[j % 2]])
                    evac(xb[:, :, j * 128:(j + 1) * 128], psb[:, j % 2, :].rearrange("p (k t) -> p k t", k=8),
                         [pbbuf[j % 2]], [xbTb[e % 2]])

            def do_gate_up(e):
                sl = e % NW
                xb = xbT[e % 2]
                for f in range(4):
                    s2 = f % 2
                    bg, bu = next_bank(), next_bank()
                    mm_group(psf[:, bg, 0:CAP], bankbuf[bg],
                             [(wgs[sl][:, k, f * 128:(f + 1) * 128], xb[:, k, :]) for k in range(8)], wEb[sl] + [xbTb[e % 2]])
                    mm_group(psf[:, bu, 0:CAP], bankbuf[bu],
                             [(wus[sl][:, k, f * 128:(f + 1) * 128], xb[:, k, :]) for k in range(8)], wEb[sl] + [xbTb[e % 2]])
                    op("act", lambda: nc.scalar.activation(out=sg[s2][:, :], in_=psf[:, bg, 0:CAP], func=AF.Silu),
                       reads=[bankbuf[bg]], writes=[sgb[s2]])
                    op("dve", lambda: nc.vector.tensor_tensor(out=hT[e % 2][:, f, :], in0=sg[s2][:, :], in1=psf[:, bu, 0:CAP], op=ALU.mult),
                       reads=[sgb[s2], bankbuf[bu]], writes=[hTb[e % 2]])

            def do_down(e):
                sl = e % NW
                for j in range(3):
                    s2 = j % 2
                    for half in range(2):
                        bk = next_bank()
                        mm_group(psf[:, bk, :], bankbuf[bk],
                                 [(hT[e % 2][:, f, j * 128:(j + 1) * 128], wds[sl][:, f, half * 512:(half + 1) * 512]) for f in range(4)],
                                 wEb[sl] + [hTb[e % 2]])
                        op("act" if half == 0 else "dve",
                           (lambda: nc.scalar.copy(out=ysb[s2][:, 0:512], in_=psf[:, bk, :])) if half == 0 else
                           (lambda: nc.vector.tensor_copy(out=ysb[s2][:, 512:1024], in_=psf[:, bk, :])),
                           reads=[bankbuf[bk]], writes=[ysbb[s2]])
                    dma("sp", lambda: nc.sync.dma_start(out=ys_v[e][:, j, :], in_=ysb[s2][:, :]), reads=[ysbb[s2]])

            load_w(0)
            load_w(1)
            cast_w(0)
            do_transposes(0)
            for e in range(32):
                if e + 2 < 32:
                    load_w(e + 2)
                do_gate_up(e)
                if e + 1 < 32:
                    do_transposes(e + 1)
                    cast_w(e + 1)
                do_down(e)
            cx.barrier()

        if stage == 3:
            return nc
        with ExitStack() as pd:
            g2 = sb(pd, "g2", [128, D], F32)
            b2 = sb(pd, "b2", [128, D], F32)
            wD = Buf()
            dma("sp", lambda: nc.sync.dma_start(out=g2[:], in_=lnp[2].partition_broadcast(128)), writes=[wD])
            dma("sp", lambda: nc.sync.dma_start(out=b2[:], in_=lnp[3].partition_broadcast(128)), writes=[wD])
            cx.barrier()
            NR = 4
            r1 = [sb(pd, "r1%d" % i, [128, D], F32) for i in range(NR)]
            r2 = [sb(pd, "r2%d" % i, [128, D], F32) for i in range(NR)]
            hh = [sb(pd, "hh%d" % i, [128, D], F32) for i in range(NR)]
            stats = sb(pd, "stats2", [128, 2, 6], F32)
            mv = sb(pd, "mv2", [128, 2], F32)
            rstd = sb(pd, "rstd2", [128, 1], F32)
            stb = Buf()
            r1b, r2b, hhb = [Buf() for _ in range(NR)], [Buf() for _ in range(NR)], [Buf() for _ in range(NR)]
            h1_v = h1_scr.rearrange("(s p) d -> p s d", p=128)
            out_v = out.rearrange("(s p) d -> p s d", p=128)

            def loads_d(gs):
                s2 = gs % NR
                for rr, rrb in ((r1, r1b), (r2, r2b)):
                    for half in range(2):
                        op("act", lambda: nc.scalar.copy(out=rr[s2][:, half * 512:(half + 1) * 512], in_=zeros[:, :]),
                           reads=[b_const], writes=[rrb[s2]])
                dma("pool", lambda: nc.gpsimd.indirect_dma_start(
                    out=r1[s2][:, :], out_offset=None, in_=ys_scr,
                    in_offset=bass.IndirectOffsetOnAxis(ap=D1[:, gs:gs + 1], axis=0),
                    bounds_check=bc_reg, oob_is_err=False), writes=[r1b[s2]])
                dma("pool", lambda: nc.gpsimd.indirect_dma_start(
                    out=r2[s2][:, :], out_offset=None, in_=ys_scr,
                    in_offset=bass.IndirectOffsetOnAxis(ap=D2[:, gs:gs + 1], axis=0),
                    bounds_check=bc_reg, oob_is_err=False), writes=[r2b[s2]])
                dma("sp", lambda: nc.sync.dma_start(out=hh[s2][:, :], in_=h1_v[:, gs, :]), writes=[hhb[s2]])

            epsT2 = sb(pd, "epsT2", [128, 1], F32)
            rwb = Buf()
            op("pool", lambda: nc.gpsimd.memset(epsT2[:], EPS / (ALPHA * ALPHA)), writes=[rwb])
            op("dve", lambda: nc.vector.tensor_scalar(out=RW1[:, :], in0=RW1[:, :], scalar1=1.0 / ALPHA, scalar2=None, op0=ALU.mult),
               writes=[rwb])
            op("dve", lambda: nc.vector.tensor_scalar(out=RW2[:, :], in0=RW2[:, :], scalar1=1.0 / ALPHA, scalar2=None, op0=ALU.mult),
               writes=[rwb])
            cx.barrier()
            loads_d(0)
            loads_d(1)
            for gs in range(32):
                s2 = gs % NR
                if gs + 2 < 32:
                    loads_d(gs + 2)
                op("dve", lambda: nc.vector.scalar_tensor_tensor(out=hh[s2][:, :], in0=r1[s2][:, :], scalar=RW1[:, gs:gs + 1],
                                                                 in1=hh[s2][:, :], op0=ALU.mult, op1=ALU.add),
                   reads=[r1b[s2], hhb[s2]], writes=[hhb[s2]])
                op("dve", lambda: nc.vector.scalar_tensor_tensor(out=hh[s2][:, :], in0=r2[s2][:, :], scalar=RW2[:, gs:gs + 1],
                                                                 in1=hh[s2][:, :], op0=ALU.mult, op1=ALU.add),
                   reads=[r2b[s2], hhb[s2]], writes=[hhb[s2]])
                layer_norm(nc, op, hh[s2], hhb[s2], stats, mv, rstd, stb, g2, b2, wD, epsT2)
                dma("sp", lambda: nc.sync.dma_start(out=out_v[:, gs, :], in_=hh[s2][:, :]), reads=[hhb[s2]])
            cx.barrier()
    return nc


def fence(cx, op, nc, rstd_like=None):
    cx.barrier()


def layer_norm(nc, op, h, hb, stats, mv, rstd, stb, g, b, wB, epsT, gmul_on_dve=False):
    for half in range(2):
        op("dve", lambda: nc.vector.bn_stats(out=stats[:, half, :], in_=h[:, half * 512:(half + 1) * 512]),
           reads=[hb], writes=[stb])
    op("dve", lambda: nc.vector.bn_aggr(out=mv[:, :], in_=stats[:, :, :].rearrange("p a b -> p (a b)")), reads=[stb], writes=[stb])
    op("act", lambda: nc.scalar.activation(out=rstd[:, :], in_=mv[:, 1:2], func=AF.Sqrt, bias=epsT[:, 0:1]),
       reads=[stb, wB], writes=[stb])
    op("dve", lambda: nc.vector.reciprocal(out=rstd[:, :], in_=rstd[:, :]), reads=[stb], writes=[stb])
    op("dve", lambda: nc.vector.tensor_scalar(out=h[:, :], in0=h[:, :], scalar1=mv[:, 0:1], scalar2=rstd[:, 0:1],
                                              op0=ALU.subtract, op1=ALU.mult), reads=[stb, hb], writes=[hb])
    if gmul_on_dve:
        op("dve", lambda: nc.vector.tensor_tensor(out=h[:, :], in0=h[:, :], in1=g[:, :], op=ALU.mult), reads=[hb, wB], writes=[hb])
    else:
        op("pool", lambda: nc.gpsimd.tensor_tensor(out=h[:, :], in0=h[:, :], in1=g[:, :], op=ALU.mult), reads=[hb, wB], writes=[hb])
    op("pool", lambda: nc.gpsimd.tensor_tensor(out=h[:, :], in0=h[:, :], in1=b[:, :], op=ALU.add), reads=[hb, wB], writes=[hb])


def routing(nc, op, dma, R, Ind, Lg, Lgb, rb, ustr_bf, ones_bf, b_const, psf, bankbuf, next_bank,
            base, ebase, D1, D2, RW1, RW2, T, wB):
    V_ = nc.vector

    def dv(fn, extra_r=()):
        op("dve", fn, reads=[rb, Lgb] + list(extra_r), writes=[rb])
    lg = Lg[:, :, 0:4]
    le = Lg[:, :, 4:36].rearrange("p s (g e) -> p s g e", g=4)
    dv(lambda: V_.tensor_reduce(out=R["gmax"][:, :], in_=lg, axis=AX.X, op=ALU.max))
    dv(lambda: V_.tensor_tensor(out=R["ohg"][:, :, :], in0=lg, in1=R["gmax"][:, :].unsqueeze(2).to_broadcast([128, 4, 4]),
                                op=ALU.is_equal))
    dv(lambda: V_.tensor_tensor(out=R["eg"][:, :, :], in0=lg, in1=R["gmax"][:, :].unsqueeze(2).to_broadcast([128, 4, 4]),
                                op=ALU.subtract))
    op("act", lambda: nc.scalar.activation(out=R["eg"][:, :, :], in_=R["eg"][:, :, :], func=AF.Exp), reads=[rb], writes=[rb])
    dv(lambda: V_.tensor_reduce(out=R["sumg"][:, :], in_=R["eg"][:, :, :], axis=AX.X, op=ALU.add))
    dv(lambda: V_.reciprocal(out=R["gp"][:, :], in_=R["sumg"][:, :]))
    dv(lambda: V_.tensor_tensor(out=R["prod"][:, :, :, :], in0=le,
                                in1=R["ohg"][:, :, :].unsqueeze(3).to_broadcast([128, 4, 4, 8]), op=ALU.mult))
    dv(lambda: V_.tensor_reduce(out=R["sel"][:, :, :], in_=R["prod"][:, :, :, :].rearrange("p s g e -> p s e g"),
                                axis=AX.X, op=ALU.add))
    dv(lambda: V_.tensor_reduce(out=R["m1"][:, :], in_=R["sel"][:, :, :], axis=AX.X, op=ALU.max))
    dv(lambda: V_.tensor_tensor(out=R["oh1"][:, :, :], in0=R["sel"][:, :, :],
                                in1=R["m1"][:, :].unsqueeze(2).to_broadcast([128, 4, 8]), op=ALU.is_equal))
    dv(lambda: V_.scalar_tensor_tensor(out=R["sel2"][:, :, :], in0=R["oh1"][:, :, :], scalar=-1e30, in1=R["sel"][:, :, :],
                                       op0=ALU.mult, op1=ALU.add))
    dv(lambda: V_.tensor_reduce(out=R["m2"][:, :], in_=R["sel2"][:, :, :], axis=AX.X, op=ALU.max))
    dv(lambda: V_.tensor_tensor(out=R["oh2"][:, :, :], in0=R["sel2"][:, :, :],
                                in1=R["m2"][:, :].unsqueeze(2).to_broadcast([128, 4, 8]), op=ALU.is_equal))
    dv(lambda: V_.tensor_tensor(out=R["dm"][:, :], in0=R["m1"][:, :], in1=R["m2"][:, :], op=ALU.subtract))
    op("act", lambda: nc.scalar.activation(out=R["w1"][:, :], in_=R["dm"][:, :], func=AF.Sigmoid), reads=[rb], writes=[rb])
    dv(lambda: V_.tensor_scalar(out=R["w2"][:, :], in0=R["w1"][:, :], scalar1=-1.0, scalar2=1.0, op0=ALU.mult, op1=ALU.add))
    dv(lambda: V_.tensor_tensor(out=RW1[:, 4 * T:4 * T + 4], in0=R["w1"][:, :], in1=R["gp"][:, :], op=ALU.mult))
    dv(lambda: V_.tensor_tensor(out=RW2[:, 4 * T:4 * T + 4], in0=R["w2"][:, :], in1=R["gp"][:, :], op=ALU.mult))
    ohg_b = R["ohg"][:, :, :].unsqueeze(3).to_broadcast([128, 4, 4, 8])
    for nm, src in (("OH1", "oh1"), ("OH2", "oh2")):
        dv(lambda: V_.tensor_tensor(out=R[nm][:, :, :].rearrange("p s (g e) -> p s g e", g=4), in0=ohg_b,
                                    in1=R[src][:, :, :].unsqueeze(2).to_broadcast([128, 4, 4, 8]), op=ALU.mult))
    dv(lambda: V_.tensor_tensor(out=Ind[:, :, :], in0=R["OH1"][:, :, :], in1=R["OH2"][:, :, :], op=ALU.add))
    bR, bT = next_bank(), next_bank()
    ind2 = Ind[:, :, :].rearrange("p s e -> p (s e)")
    op("pe", lambda: nc.tensor.matmul(psf[:, bR, 0:128], ustr_bf[:, :], ind2, start=True, stop=True),
       reads=[rb, b_const], writes=[bankbuf[bR]])
    op("pe", lambda: nc.tensor.matmul(psf[:, bT, 0:128], ones_bf[:, :], ind2, start=True, stop=True),
       reads=[rb, b_const], writes=[bankbuf[bT]])
    for s in range(4):
        dv(lambda: V_.tensor_tensor(out=R["Rk"][:, s, :], in0=psf[:, bR, s * 32:(s + 1) * 32], in1=base[:, :], op=ALU.add),
           extra_r=[bankbuf[bR], wB])
        op("dve", lambda: V_.tensor_tensor(out=base[:, :], in0=base[:, :], in1=psf[:, bT, s * 32:(s + 1) * 32], op=ALU.add),
           reads=[rb, wB, bankbuf[bT]], writes=[rb, wB])
    dv(lambda: V_.tensor_scalar(out=R["ov"][:, :, :], in0=R["Rk"][:, :, :], scalar1=float(CAP) - 0.5, scalar2=1.0e6,
                                op0=ALU.is_ge, op1=ALU.mult))
    dv(lambda: V_.tensor_tensor(out=R["Rk"][:, :, :], in0=R["Rk"][:, :, :], in1=R["ov"][:, :, :], op=ALU.add))
    dv(lambda: V_.tensor_tensor(out=R["Rk"][:, :, :], in0=R["Rk"][:, :, :],
                                in1=ebase[:, :].unsqueeze(1).to_broadcast([128, 4, 32]), op=ALU.add), extra_r=[b_const])
    for nm, dst, Dk in (("OH1", "d1f", D1), ("OH2", "d2f", D2)):
        dv(lambda: V_.tensor_tensor(out=R[nm][:, :, :], in0=R[nm][:, :, :], in1=R["Rk"][:, :, :], op=ALU.mult))
        dv(lambda: V_.tensor_reduce(out=R[dst][:, :], in_=R[nm][:, :, :], axis=AX.X, op=ALU.add))
        dv(lambda: V_.tensor_copy(out=Dk[:, 4 * T:4 * T + 4], in_=R[dst][:, :]))


_NC_CACHE = {}


def _prep_core(c, x, shared):
    b, p = c // 2, c % 2
    xr = x[b, ::-1, :]
    blocks = [2 * i + p for i in range(NBLK)]
    rows_o = np.concatenate([np.arange(128 * a, 128 * a + 128) for a in blocks])
    xo = xr[rows_o]
    halo = np.zeros((64, D), np.float32)
    for i, a in enumerate(blocks):
        r0 = 128 * (a + 1)
        if r0 < S:
            halo[2 * i] = xr[r0]
            halo[2 * i + 1] = xr[r0 + 1]
    tri = np.where(np.arange(128)[None, :] <= np.arange(128)[:, None], MASKV, 0.0).astype(np.float32)
    if p == 0:
        mask = np.concatenate([tri, np.zeros((128, 128), np.float32)], axis=1)
    else:
        mask = np.concatenate([np.full((128, 128), MASKV, np.float32), tri], axis=1)
    ident = np.eye(128, dtype=np.float32)
    ustr = (np.arange(128)[:, None] < np.arange(128)[None, :]).astype(np.float32)
    ones = np.ones((128, 128), np.float32)
    eb = np.tile((np.arange(32, dtype=np.float32) * CAP)[None, :], (128, 1))
    m01 = (mask == 0.0).astype(np.float32)
    consts = np.ascontiguousarray(np.concatenate([ident, ustr, ones, mask, eb, m01], axis=1))
    m = dict(shared)
    m.update({
        "xT": np.ascontiguousarray(xr.T),
        "xTsh": np.ascontiguousarray(np.concatenate([xr.T[:, 1:], np.zeros((D, 1), np.float32)], axis=1)),
        "xTs": np.ascontiguousarray(xr[0:S:256].T),
        "xTo": np.ascontiguousarray(xo.T),
        "xTsho": np.ascontiguousarray(np.concatenate([xr, np.zeros((1, D), np.float32)], axis=0)[rows_o + 1].T),
        "xTh": np.ascontiguousarray(halo.T),
        "xo": np.ascontiguousarray(xo),
        "consts": consts,
    })
    return m


def _prep_shared(w_in, gate_bias, conv_w, w_branch_a, w_branch_b, w_out, ln1_g, ln1_b,
                 w_router_g, b_router_g, w_router_e, b_router_e, w_gate, w_up, w_down, ln2_g, ln2_b):
    f = lambda a: np.ascontiguousarray(np.asarray(a, dtype=np.float32))
    gb = f(gate_bias[0]).reshape(16, 128).T
    cwp = f(conv_w[0]).reshape(3, 4, 128).transpose(2, 1, 0).reshape(128, 12)
    return {
        "w_in": f(w_in[0]), "gbias": f(gb), "cw": f(cwp),
        "w_ba": f(w_branch_a[0]), "w_bb": f(w_branch_b[0]), "w_out": f(w_out[0]),
        "lnp": f(np.stack([ln1_g[0], ln1_b[0], ln2_g[0], ln2_b[0]], axis=0)),
        "w_r": f(np.concatenate([w_router_g[0], w_router_e[0]], axis=1)),
        "b_r": f(np.concatenate([b_router_g[0], b_router_e[0].reshape(-1)], axis=0)),
        "w_gate": f(w_gate[0]), "w_up": f(w_up[0]), "w_down": f(w_down[0]),
    }


def kernel(x, w_in, gate_bias, conv_w, w_branch_a, w_branch_b, w_out, ln1_g, ln1_b,
           w_router_g, b_router_g, w_router_e, b_router_e, w_gate, w_up, w_down, ln2_g, ln2_b):
    x = np.asarray(x, dtype=np.float32)
    shared = _prep_shared(w_in, gate_bias, conv_w, w_branch_a, w_branch_b, w_out, ln1_g, ln1_b,
                          w_router_g, b_router_g, w_router_e, b_router_e, w_gate, w_up, w_down, ln2_g, ln2_b)
    in_maps = [_prep_core(c, x, shared) for c in range(8)]
    nc = build(4)
    res = run_bass_kernel_spmd(nc, in_maps, core_ids=list(range(8)))
    out = np.zeros((4, S, D), np.float32)
    for c in range(8):
        b, p = c // 2, c % 2
        oc = np.asarray(res.results[c]["out"]).reshape(NOWN, D)
        for i in range(NBLK):
            a = 2 * i + p
            rr = np.arange(128 * a, 128 * a + 128)
            out[b, S - 1 - rr] = oc[128 * i:128 * i + 128]
    return out
```

```python
import os
import numpy as np
from contextlib import ExitStack
import concourse.bass as bass
import concourse.mybir as mybir
from concourse.bass_utils import run_bass_kernel_spmd

F32 = mybir.dt.float32
BF16 = mybir.dt.bfloat16
I32 = mybir.dt.int32
AF = mybir.ActivationFunctionType
ALU = mybir.AluOpType
AX = mybir.AxisListType

S = 8192
D = 1024
NOWN = 4096
NBLK = 32
CAP = 384
NSLOT = 32 * CAP
ALPHA = 2.0 ** 0.25
EPS = 1e-5
MASKV = -30000.0
NDMA = 12


class Buf:
    __slots__ = ("lw", "rd")

    def __init__(self):
        self.lw = {}
        self.rd = {}


class Ctx:
    def __init__(self, nc, es):
        self.nc = nc
        self.eng = {"pe": nc.tensor, "act": nc.scalar, "dve": nc.vector, "pool": nc.gpsimd, "sp": nc.sync}
        self.semobj = {}
        self.cnt = {}
        self.waited = {e: {} for e in self.eng}
        for e in self.eng:
            self.semobj[e] = es.enter_context(nc.semaphore("s_" + e))
            self.cnt[e] = 0
        self.rr = {"sp": 0, "pool": 0}
        for q in ("sp", "pool"):
            for i in range(NDMA):
                k = (q, i)
                self.semobj[k] = es.enter_context(nc.semaphore("d_%s%d" % (q, i)))
                self.cnt[k] = 0

    def _deps(self, reads, writes):
        need = {}
        for b in reads:
            for k, v in b.lw.items():
                if need.get(k, 0) < v:
                    need[k] = v
        for b in writes:
            for k, v in b.lw.items():
                if need.get(k, 0) < v:
                    need[k] = v
            for k, v in b.rd.items():
                if need.get(k, 0) < v:
                    need[k] = v
        return need

    def _wait(self, e, need):
        eng = self.eng[e]
        w = self.waited[e]
        for k, v in need.items():
            if k == e and e == "pe":
                continue
            if w.get(k, 0) >= v:
                continue
            eng.wait_ge(self.semobj[k], v)
            w[k] = v

    def _mark(self, ev, reads, writes):
        for b in writes:
            b.lw[ev[0]] = ev[1]
            b.rd = {}
        for b in reads:
            if b.rd.get(ev[0], 0) < ev[1]:
                b.rd[ev[0]] = ev[1]

    def op(self, e, fn, reads=(), writes=()):
        self._wait(e, self._deps(reads, writes))
        ins = fn()
        self.cnt[e] += 1
        ins.then_inc(self.semobj[e], 1)
        self._mark((e, self.cnt[e]), reads, writes)

    def dma(self, q, fn, reads=(), writes=()):
        need = self._deps(reads, writes)
        i = self.rr[q]
        self.rr[q] = (i + 1) % NDMA
        k = (q, i)
        if self.cnt[k] > 0:
            need[k] = max(need.get(k, 0), self.cnt[k])
        self._wait(q, need)
        ins = fn()
        self.cnt[k] += 16
        ins.then_inc(self.semobj[k], 16)
        self._mark((k, self.cnt[k]), reads, writes)

    def barrier(self):
        allv = {k: v for k, v in self.cnt.items() if v > 0}
        for e in self.eng:
            need = {k: v for k, v in allv.items() if k != e}
            self._wait(e, need)


def build(stage=3):
    nc = bass.Bass("TRN2", target_bir_lowering=False)

    def din(name, shape, dt=F32):
        return nc.dram_tensor(name, list(shape), dt, kind="ExternalInput").ap()

    xT = din("xT", [D, S])
    xTsh = din("xTsh", [D, S])
    xTsho = din("xTsho", [D, NOWN])
    xTs = din("xTs", [D, 32])
    xTo = din("xTo", [D, NOWN])
    xTh = din("xTh", [D, 64])
    xo = din("xo", [NOWN, D])
    w_in = din("w_in", [D, 5120])
    gbias = din("gbias", [128, 16])
    cw = din("cw", [128, 12])
    w_ba = din("w_ba", [512, D])
    w_bb = din("w_bb", [512, D])
    w_out = din("w_out", [D, D])
    lnp = din("lnp", [4, D])
    w_r = din("w_r", [D, 36])
    b_r = din("b_r", [36])
    w_gate = din("w_gate", [32, D, 512])
    w_up = din("w_up", [32, D, 512])
    w_down = din("w_down", [32, 512, D])
    consts = din("consts", [128, 128 * 3 + 256 + 32 + 256])
    out = nc.dram_tensor("out", [NOWN, D], F32, kind="ExternalOutput").ap()
    if stage == 1:
        attn_scr = nc.dram_tensor("attn_scr", [512, NOWN], BF16, kind="ExternalOutput").ap()
    else:
        attn_scr = nc.dram_tensor("attn_scr", [512, NOWN], BF16).ap()
    if stage == 2:
        h1_scr = nc.dram_tensor("h1_scr", [NOWN, D], F32, kind="ExternalOutput").ap()
        dbg_r = nc.dram_tensor("dbg_r", [128, 32 * 4], F32, kind="ExternalOutput").ap()
    else:
        h1_scr = nc.dram_tensor("h1_scr", [NOWN, D], F32).ap()
    xg_scr = nc.dram_tensor("xg_scr", [NSLOT, D], BF16).ap()
    vsh_scr = nc.dram_tensor("vsh_scr", [512, NOWN], BF16).ap()
    ys_scr = nc.dram_tensor("ys_scr", [NSLOT, D], F32).ap()

    w_in_v = w_in.rearrange("(k p) n -> p k n", p=128)
    xT_v = xT.rearrange("(k p) t -> p k t", p=128)
    xTo_v = xTo.rearrange("(k p) t -> p k t", p=128)
    xTh_v = xTh.rearrange("(k p) t -> p k t", p=128)
    xTs_v = xTs.rearrange("(k p) t -> p k t", p=128)
    xTsh_v = xTsh.rearrange("(k p) t -> p k t", p=128)
    xTsho_v = xTsho.rearrange("(k p) t -> p k t", p=128)

    with ExitStack() as es:
        E = es.enter_context
        cx = Ctx(nc, es)
        op, dma = cx.op, cx.dma

        def sb(st, name, shape, dt):
            return st.enter_context(nc.sbuf_tensor(name, list(shape), dt))

        ident_bf = sb(es, "ident_bf", [128, 128], BF16)
        ustr_bf = sb(es, "ustr_bf", [128, 128], BF16)
        ones_bf = sb(es, "ones_bf", [128, 128], BF16)
        mask_bf = sb(es, "mask_bf", [128, 256], BF16)
        ident_f = sb(es, "ident_f", [128, 128], F32)
        ebase = sb(es, "ebase", [128, 32], F32)
        D1 = sb(es, "D1", [128, 32], I32)
        D2 = sb(es, "D2", [128, 32], I32)
        RW1 = sb(es, "RW1", [128, 32], F32)
        RW2 = sb(es, "RW2", [128, 32], F32)
        zeros = sb(es, "zeros", [128, 512], F32)
        zeros_bf = sb(es, "zeros_bf", [128, 512], BF16)
        b_const = Buf()
        bc_reg = nc.gpsimd.alloc_register("bc_reg")
        nc.gpsimd.reg_mov(bc_reg, NSLOT - 1)
        dma("pool", lambda: nc.gpsimd.dma_start(out=ident_bf[:], in_=consts[:, 0:128]), writes=[b_const])
        dma("pool", lambda: nc.gpsimd.dma_start(out=ustr_bf[:], in_=consts[:, 128:256]), writes=[b_const])
        dma("pool", lambda: nc.gpsimd.dma_start(out=ones_bf[:], in_=consts[:, 256:384]), writes=[b_const])
        dma("pool", lambda: nc.gpsimd.dma_start(out=mask_bf[:], in_=consts[:, 384:640]), writes=[b_const])
        dma("sp", lambda: nc.sync.dma_start(out=ident_f[:], in_=consts[:, 0:128]), writes=[b_const])
        dma("sp", lambda: nc.sync.dma_start(out=ebase[:], in_=consts[:, 640:672]), writes=[b_const])
        mask01 = sb(es, "mask01", [128, 256], F32)
        dma("sp", lambda: nc.sync.dma_start(out=mask01[:], in_=consts[:, 672:928]), writes=[b_const])
        op("pool", lambda: nc.gpsimd.memset(zeros[:], 0.0), writes=[b_const])
        op("pool", lambda: nc.gpsimd.memset(zeros_bf[:], 0.0), writes=[b_const])
        ones_f = sb(es, "ones_f", [128, 1], F32)
        op("pool", lambda: nc.gpsimd.memset(ones_f[:], 1.0), writes=[b_const])
        epsT = sb(es, "epsT", [128, 1], F32)
        op("pool", lambda: nc.gpsimd.memset(epsT[:], EPS), writes=[b_const])
        cx.barrier()

        ps_stack = [None]

        def alloc_psum(tag, nf, nb):
            if ps_stack[0] is not None:
                ps_stack[0].close()
            st_ = ExitStack()
            es.callback(st_.close)
            ps_stack[0] = st_
            f_ = st_.enter_context(nc.psum_tensor("psf" + tag, [128, nf, 512], F32))
            b_ = st_.enter_context(nc.psum_tensor("psb" + tag, [128, nb, 1024], BF16))
            return f_, b_

        psf, psb = alloc_psum("A", 6, 2)
        bankbuf = [Buf() for _ in range(6)]
        pbbuf = [Buf(), Buf(), Buf()]
        bank_rr = [0]

        def next_bank(n=6):
            i = bank_rr[0] % n
            bank_rr[0] += 1
            return i

        evac_rr = [0]

        def evac(out_ap, in_ap, reads, writes, scale=None):
            evac_rr[0] += 1
            if evac_rr[0] % 2 == 0:
                if scale is None:
                    op("act", lambda: nc.scalar.copy(out=out_ap, in_=in_ap), reads=reads, writes=writes)
                else:
                    op("act", lambda: nc.scalar.mul(out=out_ap, in_=in_ap, mul=scale), reads=reads, writes=writes)
            else:
                if scale is None:
                    op("dve", lambda: nc.vector.tensor_copy(out=out_ap, in_=in_ap), reads=reads, writes=writes)
                else:
                    op("dve", lambda: nc.vector.tensor_scalar(out=out_ap, in0=in_ap, scalar1=scale, scalar2=None,
                                                              op0=ALU.mult), reads=reads, writes=writes)

        def mm_group(bank_ap, bankb, pairs, reads):
            def fn():
                n = len(pairs)
                ins = None
                for i, (l, r) in enumerate(pairs):
                    ins = nc.tensor.matmul(bank_ap, l, r, start=(i == 0), stop=(i == n - 1))
                return ins
            op("pe", fn, reads=reads, writes=[bankb])

        with ExitStack() as pa:
            KT = sb(pa, "KT", [128, 4, S], BF16)
            V = sb(pa, "dV", [128, 64, 512], BF16)
            VsT = sb(pa, "VsT", [128, 4, 32], F32)
            VsTb = Buf()
            KTb = [[Buf() for _ in range(4)] for _ in range(16)]
            Vb = [Buf() for _ in range(64)]
            QTb = [[Buf() for _ in range(4)] for _ in range(8)]
            with ExitStack() as pa1:
                wqkv = sb(pa1, "wqkv", [128, 8, 1024], BF16)
                wvn = sb(pa1, "wvn", [128, 8, 512], BF16)
                xs = sb(pa1, "xs", [128, 8, 32], BF16)
                xt = [sb(pa1, "xt%d" % i, [128, 8, 512], BF16) for i in range(2)]
                xtsh = [sb(pa1, "xtsh%d" % i, [128, 8, 512], BF16) for i in range(2)]
                xtb = [Buf(), Buf()]
                xtshb = [Buf(), Buf()]
                wb = Buf()
                dma("pool", lambda: nc.gpsimd.dma_start(out=wqkv[:, :, :], in_=w_in_v[:, :, 512:1536]), writes=[wb])
                dma("pool", lambda: nc.gpsimd.dma_start(out=xs[:], in_=xTs_v), writes=[wb])
                op("dve", lambda: nc.vector.tensor_scalar(out=wvn[:, :, :], in0=wqkv[:, :, 512:1024], scalar1=-1.0, scalar2=None,
                                                          op0=ALU.mult), reads=[wb], writes=[wb])
                for j in range(4):
                    bk = next_bank()
                    mm_group(psf[:, bk, 0:32], bankbuf[bk],
                             [(wqkv[:, k, 512 + j * 128:512 + (j + 1) * 128], xs[:, k, :]) for k in range(8)], reads=[wb])
                    op("dve", lambda: nc.vector.tensor_copy(out=VsT[:, j, :], in_=psf[:, bk, 0:32]), reads=[bankbuf[bk]], writes=[VsTb])
                for T in range(16):
                    sl = T % 2
                    dma("pool", lambda: nc.gpsimd.dma_start(out=xt[sl][:], in_=xT_v[:, :, T * 512:(T + 1) * 512]),
                        writes=[xtb[sl]])
                    dma("pool", lambda: nc.gpsimd.dma_start(out=xtsh[sl][:], in_=xTsh_v[:, :, T * 512:(T + 1) * 512]),
                        writes=[xtshb[sl]])
                    for j in range(4):
                        bk = next_bank()
                        mm_group(psf[:, bk, :], bankbuf[bk],
                                 [(wqkv[:, k, j * 128:(j + 1) * 128], xt[sl][:, k, :]) for k in range(8)],
                                 reads=[wb, xtb[sl]])
                        evac(KT[:, j, T * 512:(T + 1) * 512], psf[:, bk, :], [bankbuf[bk]], [KTb[T][j]])
                    for s in range(4):
                        bk = next_bank()
                        mm_group(psf[:, bk, :], bankbuf[bk],
                                 [(xtsh[sl][:, k, s * 128:(s + 1) * 128], wqkv[:, k, 512:1024]) for k in range(8)] +
                                 [(xt[sl][:, k, s * 128:(s + 1) * 128], wvn[:, k, :]) for k in range(8)],
                                 reads=[wb, xtb[sl], xtshb[sl]])
                        evac(V[:, 4 * T + s, :], psf[:, bk, :], [bankbuf[bk]], [Vb[4 * T + s]])
                cx.barrier()
            QT = sb(pa, "QT", [128, 4, NOWN], BF16)
            with ExitStack() as paq:
                wq = sb(paq, "wq", [128, 8, 512], BF16)
                wvq = sb(paq, "wvq", [128, 8, 512], BF16)
                xt = [sb(paq, "xtq%d" % i, [128, 8, 512], BF16) for i in range(1)] * 2
                xtso = [sb(paq, "xtso%d" % i, [128, 8, 512], BF16) for i in range(1)] * 2
                vstg = [sb(paq, "vstg%d" % i, [128, 512], BF16) for i in range(2)]
                xtb = [Buf()] * 2
                xtsob = [Buf()] * 2
                vstgb = [Buf(), Buf()]
                wb = Buf()
                vsh_w = vsh_scr.rearrange("(j p) t -> p j t", p=128)
                dma("pool", lambda: nc.gpsimd.dma_start(out=wq[:, :, :], in_=w_in_v[:, :, 0:512]), writes=[wb])
                dma("pool", lambda: nc.gpsimd.dma_start(out=wvq[:, :, :], in_=w_in_v[:, :, 1024:1536]), writes=[wb])
                for T in range(8):
                    sl = T % 2
                    dma("pool", lambda: nc.gpsimd.dma_start(out=xt[sl][:], in_=xTo_v[:, :, T * 512:(T + 1) * 512]),
                        writes=[xtb[sl]])
                    for j in range(4):
                        bk = next_bank()
                        mm_group(psf[:, bk, :], bankbuf[bk],
                                 [(wq[:, k, j * 128:(j + 1) * 128], xt[sl][:, k, :]) for k in range(8)],
                                 reads=[wb, xtb[sl]])
                        evac(QT[:, j, T * 512:(T + 1) * 512], psf[:, bk, :], [bankbuf[bk]], [QTb[T][j]], scale=0.125)
                    dma("pool", lambda: nc.gpsimd.dma_start(out=xtso[sl][:], in_=xTsho_v[:, :, T * 512:(T + 1) * 512]),
                        writes=[xtsob[sl]])
                    for j in range(4):
                        bk = next_bank()
                        vs_ = (T * 4 + j) % 2
                        mm_group(psf[:, bk, :], bankbuf[bk],
                                 [(wvq[:, k, j * 128:(j + 1) * 128], xtso[sl][:, k, :]) for k in range(8)],
                                 reads=[wb, xtsob[sl]])
                        evac(vstg[vs_][:, :], psf[:, bk, :], [bankbuf[bk]], [vstgb[vs_]])
                        dma("sp", lambda: nc.sync.dma_start(out=vsh_w[:, j, T * 512:(T + 1) * 512], in_=vstg[vs_][:, :]),
                            reads=[vstgb[vs_]])
                cx.barrier()

            if stage == 0:
                return nc
            psf, psb = alloc_psum("T", 5, 3)
            with ExitStack() as pa2:
                NS = 4
                Sb = [sb(pa2, "Sb%d" % i, [128, 512], F32) for i in range(NS)]
                Wb = [sb(pa2, "Wb%d" % i, [128, 512], BF16) for i in range(NS)]
                WTs = [sb(pa2, "WTs%d" % i, [128, 512], BF16) for i in range(NS)]
                carry = sb(pa2, "carry", [128, 8], F32)
                Obf = [sb(pa2, "Obf%d" % i, [128, 512], BF16) for i in range(2)]
                Sbb = [Buf() for _ in range(NS)]
                Wbb = [Buf() for _ in range(NS)]
                WTsb = [Buf() for _ in range(NS)]
                carryb = [Buf() for _ in range(8)]
                Obfb = [Buf(), Buf()]
                NZ = 3
                Ob = [Buf(), Buf()]
                attn_v = attn_scr.rearrange("(j p) t -> p j t", p=128)

                items = []
                for i in range(NBLK):
                    chunks = []
                    s0 = 2 * i
                    while s0 < 64:
                        e0 = min(64, (s0 // 4 + 1) * 4)
                        chunks.append((s0, e0))
                        s0 = e0
                    for c, (s0, e0) in enumerate(chunks):
                        for h in range(8):
                            items.append((i, c, len(chunks), s0, e0, h))
                NIT = len(items)

                QTz = [sb(pa2, "QTz%d" % i_, [128, 8, 128], BF16) for i_ in range(2)]
                QTzb = [Buf(), Buf()]
                Osb = [sb(pa2, "Osb%d" % i_, [128, 512], BF16) for i_ in range(2)]
                Osbb = [Buf(), Buf()]
                for i_ in range(2):
                    op("dve", lambda: nc.vector.memset(QTz[i_][:], 0.0), writes=[QTzb[i_]])

                def st_qk(t):
                    i, c, nch, s0, e0, h = items[t]
                    if c == 0 and h == 0:
                        dma("sp", lambda: nc.sync.dma_start(out=vsh[i % 2][:], in_=vsh_r[:, :, i * 128:(i + 1) * 128]),
                            writes=[vshb[i % 2]])
                        qz = QTz[i % 2]
                        for jj in range(4):
                            op("act", lambda: nc.scalar.copy(out=qz[0:64, 2 * jj, :], in_=QT[0:64, jj, i * 128:(i + 1) * 128]),
                               reads=[QTb[i // 4][jj]], writes=[QTzb[i % 2]])
                            op("act", lambda: nc.scalar.copy(out=qz[64:128, 2 * jj + 1, :], in_=QT[64:128, jj, i * 128:(i + 1) * 128]),
                               reads=[QTb[i // 4][jj]], writes=[QTzb[i % 2]])
                    j, hb = h // 2, (h % 2) * 64
                    n = (e0 - s0) * 128
                    zb = t % NZ
                    rd = [QTzb[i % 2]] + [KTb[ss // 4][j] for ss in range(s0, e0, 4)] + [b_const]

                    def fn():
                        ins = nc.tensor.matmul(psf[:, zb, 0:n], QTz[i % 2][:, h, :],
                                               KT[:, j, s0 * 128:e0 * 128], start=True, stop=(c != 0))
                        if c == 0:
                            ins = nc.tensor.matmul(psf[:, zb, 0:256], ident_bf[:, :], mask_bf[:, :],
                                                   start=False, stop=True)
                        return ins
                    op("pe", fn, reads=rd, writes=[bankbuf[zb]])

                def st_sig(t):
                    i, c, nch, s0, e0, h = items[t]
                    n = (e0 - s0) * 128
                    zb = t % NZ
                    sl = t % NS
                    op("act", lambda: nc.scalar.activation(out=Sb[sl][:, 0:n], in_=psf[:, zb, 0:n], func=AF.Sigmoid,
                                                           scale=-1.0),
                       reads=[bankbuf[zb]], writes=[Sbb[sl]])

                if os.environ.get('ATT_C'):
                    Cb = [sb(pa2, "Cb%d" % i_, [128, 8], F32) for i_ in range(NS)]
                    Cbb = [Buf() for _ in range(NS)]

                if os.environ.get('ATT_E'):
                    Cf = [sb(pa2, "Cf%d" % i_, [128, 512], F32) for i_ in range(NS)]
                    Cfb = [Buf() for _ in range(NS)]

                def st_scan_old(t):
                    i, c, nch, s0, e0, h = items[t]
                    n = (e0 - s0) * 128
                    sl = t % NS
                    if c == 0:
                        op("pool", lambda: nc.gpsimd.memset(Cb[sl][:, 0:1], 1.0), writes=[Cbb[sl]])
                    else:
                        op("pool", lambda: nc.gpsimd.tensor_copy(out=Cb[sl][:, 0:1], in_=carry[:, h:h + 1]),
                           reads=[carryb[h]], writes=[Cbb[sl]])
                    op("dve", lambda: nc.vector.tensor_tensor_scan(out=Cb[sl][:, 1:n + 1], data0=Sb[sl][:, 0:n],
                                                                   data1=zeros[:, 0:n], initial=Cb[sl][:, 0:1],
                                                                   op0=ALU.mult, op1=ALU.add),
                       reads=[Sbb[sl], Cbb[sl]], writes=[Cbb[sl]])
                    if c != nch - 1:
                        op("pool", lambda: nc.gpsimd.tensor_copy(out=carry[:, h:h + 1], in_=Cb[sl][:, n:n + 1]),
                           reads=[Cbb[sl]], writes=[carryb[h]])
                    op("dve", lambda: nc.vector.tensor_tensor(out=Wb[sl][:, 0:n], in0=Cb[sl][:, 0:n],
                                                              in1=Cb[sl][:, 1:n + 1], op=ALU.subtract),
                       reads=[Cbb[sl]], writes=[Wbb[sl]])

                Cb = [sb(pa2, "Cb%d" % i_, [128, 8], F32) for i_ in range(NS)]
                Cbb = [Buf() for _ in range(NS)]
                vsh = [sb(pa2, "vsh%d" % i_, [128, 4, 128], BF16) for i_ in range(2)]
                vshb = [Buf(), Buf()]
                vsh_r = vsh_scr.rearrange("(j p) t -> p j t", p=128)

                def st_pre(t):
                    i, c, nch, s0, e0, h = items[t]
                    sl = t % NS
                    if c == 0:
                        op("pool", lambda: nc.gpsimd.memset(Cb[sl][:, 0:1], 1.0), writes=[Cbb[sl]])
                    else:
                        op("pool", lambda: nc.gpsimd.tensor_copy(out=Cb[sl][:, 0:1], in_=carry[:, h:h + 1]),
                           reads=[carryb[h]], writes=[Cbb[sl]])

                def st_scan_f(t):
                    i, c, nch, s0, e0, h = items[t]
                    n = (e0 - s0) * 128
                    sl = t % NS
                    op("dve", lambda: nc.vector.tensor_tensor_scan(out=Wb[sl][:, 0:n], data0=Sb[sl][:, 0:n],
                                                                   data1=zeros[:, 0:n], initial=Cb[sl][:, 0:1],
                                                                   op0=ALU.mult, op1=ALU.add),
                       reads=[Sbb[sl], Cbb[sl]], writes=[Wbb[sl]])
                    if c != nch - 1:
                        op("pool", lambda: nc.gpsimd.tensor_copy(out=carry[:, h:h + 1], in_=Wb[sl][:, n - 1:n]),
                           reads=[Wbb[sl]], writes=[carryb[h]])
                    if c == 0:
                        op("pool", lambda: nc.gpsimd.tensor_tensor(out=Wb[sl][:, 0:256], in0=Wb[sl][:, 0:256],
                                                                   in1=mask01[:, :], op=ALU.mult),
                           reads=[Wbb[sl], b_const], writes=[Wbb[sl]])

                def st_scan(t):
                    if True:
                        return st_scan_f(t)
                    if os.environ.get('ATT_C'):
                        return st_scan_old(t)
                    i, c, nch, s0, e0, h = items[t]
                    n = (e0 - s0) * 128
                    sl = t % NS
                    init = ones_f[:, 0:1] if c == 0 else carry[:, h:h + 1]
                    rd = [Sbb[sl]] + ([] if c == 0 else [carryb[h]])
                    if os.environ.get('ATT_E'):
                        op("dve", lambda: nc.vector.tensor_tensor_scan(out=Cf[sl][:, 0:n], data0=Sb[sl][:, 0:n],
                                                                       data1=zeros[:, 0:n], initial=init,
                                                                       op0=ALU.mult, op1=ALU.add),
                           reads=rd, writes=[Cfb[sl]])
                        op("act", lambda: nc.scalar.copy(out=Wb[sl][:, 0:n], in_=Cf[sl][:, 0:n]), reads=[Cfb[sl]], writes=[Wbb[sl]])
                    else:
                      op("dve", lambda: nc.vector.tensor_tensor_scan(out=Wb[sl][:, 0:n], data0=Sb[sl][:, 0:n],
                                                                   data1=zeros[:, 0:n], initial=init,
                                                                   op0=ALU.mult, op1=ALU.add),
                       reads=rd, writes=[Wbb[sl]])
                    if c != nch - 1:
                        ce = "dve" if os.environ.get('ATT_D3') else "pool"
                        cpe = nc.vector if os.environ.get('ATT_D3') else nc.gpsimd
                        op(ce, lambda: cpe.tensor_copy(out=carry[:, h:h + 1], in_=Wb[sl][:, n - 1:n]),
                           reads=[Wbb[sl]], writes=[carryb[h]])

                def st_tr(t):
                    i, c, nch, s0, e0, h = items[t]
                    nsb = e0 - s0
                    n = nsb * 128
                    sl = t % NS
                    hf = t % 2

                    def fn():
                        ins = None
                        for q in range(nsb):
                            ins = nc.tensor.transpose(psb[:, hf, q * 128:(q + 1) * 128],
                                                      Wb[sl][:, q * 128:(q + 1) * 128], ident_bf[:, :])
                        return ins
                    op("pe", fn, reads=[Wbb[sl], b_const], writes=[pbbuf[hf]])
                    if False:
                        op("dve", lambda: nc.vector.tensor_copy(out=WTs[sl][:, 0:n], in_=psb[:, hf, 0:n]),
                           reads=[pbbuf[hf]], writes=[WTsb[sl]])
                    else:
                        op("act", lambda: nc.scalar.copy(out=WTs[sl][:, 0:n], in_=psb[:, hf, 0:n]),
                           reads=[pbbuf[hf]], writes=[WTsb[sl]])

                def st_av(t):
                    i, c, nch, s0, e0, h = items[t]
                    nsb = e0 - s0
                    sl = t % NS
                    ob = i % 2

                    def fn():
                        ins = None
                        if c == 0 and h == 0:
                            nc.tensor.matmul(psf[:, 3 + ob, :], zeros_bf[:, 0:128], zeros_bf[:, :], start=True, stop=False,
                                             skip_group_check=True)
                        for q in range(nsb):
                            ins = nc.tensor.matmul(psf[:, 3 + ob, h * 64:(h + 1) * 64],
                                                   WTs[sl][:, q * 128:(q + 1) * 128],
                                                   V[:, s0 + q, h * 64:(h + 1) * 64],
                                                   start=False, stop=(c == nch - 1 and h == 7 and q == nsb - 1),
                                                   skip_group_check=True)
                        return ins
                    op("pe", fn, reads=[WTsb[sl]] + [Vb[ss] for ss in range(s0, e0)], writes=[Ob[ob]])
                    if c == nch - 1 and h == 7:
                        op("act", lambda: nc.scalar.copy(out=Osb[ob][:, :], in_=psf[:, 3 + ob, :]), reads=[Ob[ob]], writes=[Osbb[ob]])

                        def fnt():
                            ins = None
                            for jj in range(4):
                                ins = nc.tensor.transpose(psb[:, 2, jj * 128:(jj + 1) * 128], Osb[ob][:, jj * 128:(jj + 1) * 128],
                                                          ident_bf[:, :])
                            return ins
                        op("pe", fnt, reads=[Osbb[ob], b_const], writes=[pbbuf[2]])
                        op("dve", lambda: nc.vector.tensor_tensor(
                            out=Obf[ob][:, :].rearrange("p (j t) -> p j t", j=4),
                            in0=psb[:, 2, 0:512].rearrange("p (j t) -> p j t", j=4),
                            in1=vsh[ob][:, :, :], op=ALU.add),
                           reads=[pbbuf[2], vshb[ob]], writes=[Obfb[ob]])
                        dma("sp", lambda: nc.sync.dma_start(out=attn_v[:, :, i * 128:(i + 1) * 128],
                                                            in_=Obf[ob][:, :].rearrange("p (j t) -> p j t", j=4)),
                            reads=[Obfb[ob]])

                L1, L2, L3 = 1, 3, 4
                for t in range(NIT + L3 + 1):
                    if t < NIT:
                        st_qk(t)
                        st_sig(t)
                        st_pre(t)
                    if 0 <= t - L1 < NIT:
                        st_scan(t - L1)
                    if 0 <= t - L2 < NIT:
                        st_tr(t - L2)
                    if 0 <= t - L3 < NIT:
                        st_av(t - L3)
                cx.barrier()
        if stage == 1:
            cx.barrier()
            return nc
        psf, psb = alloc_psum("B", 6, 2)

        with ExitStack() as pb:
            wc = sb(pb, "wc", [128, 8, 1536], BF16)
            wg = sb(pb, "wg", [128, 8, 2048], BF16)
            wba = sb(pb, "wba", [128, 4, D], BF16)
            wbb = sb(pb, "wbb", [128, 4, D], BF16)
            wo = sb(pb, "wo", [128, 8, D], BF16)
            wr = sb(pb, "wr", [128, 8, 36], F32)
            gb_sb = sb(pb, "gb_sb", [128, 16], F32)
            cw_sb = sb(pb, "cw_sb", [128, 12], F32)
            g1 = sb(pb, "g1", [128, D], F32)
            b1 = sb(pb, "b1", [128, D], F32)
            br = sb(pb, "br", [128, 36], F32)
            base = sb(pb, "base", [128, 32], F32)
            wB = Buf()
            for c0 in range(0, 1536, 512):
                dma("pool", lambda: nc.gpsimd.dma_start(out=wc[:, :, c0:c0 + 512], in_=w_in_v[:, :, 1536 + c0:1536 + c0 + 512]), writes=[wB])
            for c0 in range(0, 2048, 512):
                dma("pool", lambda: nc.gpsimd.dma_start(out=wg[:, :, c0:c0 + 512], in_=w_in_v[:, :, 3072 + c0:3072 + c0 + 512]), writes=[wB])
            dma("pool", lambda: nc.gpsimd.dma_start(out=wba[:], in_=w_ba.rearrange("(k p) n -> p k n", p=128)), writes=[wB])
            dma("pool", lambda: nc.gpsimd.dma_start(out=wbb[:], in_=w_bb.rearrange("(k p) n -> p k n", p=128)), writes=[wB])
            dma("pool", lambda: nc.gpsimd.dma_start(out=wo[:], in_=w_out.rearrange("(k p) n -> p k n", p=128)), writes=[wB])
            dma("sp", lambda: nc.sync.dma_start(out=wr[:], in_=w_r.rearrange("(k p) n -> p k n", p=128)), writes=[wB])
            dma("sp", lambda: nc.sync.dma_start(out=gb_sb[:], in_=gbias), writes=[wB])
            dma("sp", lambda: nc.sync.dma_start(out=cw_sb[:], in_=cw), writes=[wB])
            dma("sp", lambda: nc.sync.dma_start(out=g1[:], in_=lnp[0].partition_broadcast(128)), writes=[wB])
            dma("sp", lambda: nc.sync.dma_start(out=b1[:], in_=lnp[1].partition_broadcast(128)), writes=[wB])
            dma("sp", lambda: nc.sync.dma_start(out=br[:], in_=b_r.partition_broadcast(128)), writes=[wB])
            op("pool", lambda: nc.gpsimd.memset(base[:], 0.0), writes=[wB])
            cx.barrier()

            xt = [sb(pb, "xtB%d" % i, [128, 8, 512], BF16) for i in range(2)]
            xh = [sb(pb, "xh%d" % i, [128, 8, 8], BF16) for i in range(2)]
            at = [sb(pb, "at%d" % i, [128, 4, 512], BF16) for i in range(2)]
            xtok = [sb(pb, "xtok%d" % i, [128, 4, D], F32) for i in range(1)]
            inb = [Buf(), Buf()]
            xtokb = Buf()
            dummy = sb(pb, "dummyB", [128, 1], F32)
            ccs = sb(pb, "ccs", [128, 512], F32)
            cchs = sb(pb, "cchs", [128, 8], F32)
            U = sb(pb, "U", [128, 4, 130], F32)
            Y = sb(pb, "Y", [128, 4, 128], F32)
            Bin = sb(pb, "Bin", [128, 4, 512], BF16)
            sa = [sb(pb, "sa%d" % i, [128, 512], F32) for i in range(1)] * 2
            sbg = [sb(pb, "sbg%d" % i, [128, 512], F32) for i in range(1)] * 2
            t1 = [sb(pb, "t1%d" % i, [128, 512], F32) for i in range(1)] * 2
            t2 = [sb(pb, "t2%d" % i, [128, 512], F32) for i in range(1)] * 2
            GT = sb(pb, "GT", [128, 8, 512], BF16)
            hbuf = [sb(pb, "hbuf%d" % i, [128, D], F32) for i in range(4)]
            h1b = [sb(pb, "h1b%d" % i, [128, D], BF16) for i in range(4)]
            h1T = [sb(pb, "h1T%d" % i, [128, 8, 128], F32) for i in range(2)]
            stats = [sb(pb, "stB%d" % i, [128, 2, 6], F32) for i in range(4)]
            mv = [sb(pb, "mvB%d" % i, [128, 2], F32) for i in range(4)]
            rstd = [sb(pb, "rstdB%d" % i, [128, 1], F32) for i in range(4)]
            Lg = sb(pb, "Lg", [128, 4, 36], F32)
            ccsb, Ub, Yb, Binb, GTb = Buf(), Buf(), Buf(), Buf(), Buf()
            sab = [Buf()] * 2
            t1b = [Buf()] * 2
            hb_ = [Buf() for _ in range(4)]
            h1bb = [Buf() for _ in range(4)]
            h1Tb, stb, Lgb = [Buf(), Buf()], [Buf() for _ in range(4)], Buf()
            R = {}
            for nm, shp in (("gmax", [128, 4]), ("ohg", [128, 4, 4]), ("eg", [128, 4, 4]), ("sumg", [128, 4]),
                            ("gp", [128, 4]), ("prod", [128, 4, 4, 8]), ("sel", [128, 4, 8]), ("m1", [128, 4]),
                            ("oh1", [128, 4, 8]), ("sel2", [128, 4, 8]), ("m2", [128, 4]), ("oh2", [128, 4, 8]),
                            ("dm", [128, 4]), ("w1", [128, 4]), ("w2", [128, 4]), ("ind8", [128, 4, 8]),
                            ("OH1", [128, 4, 32]), ("OH2", [128, 4, 32]), ("Rk", [128, 4, 32]),
                            ("ov", [128, 4, 32]), ("d1f", [128, 4]), ("d2f", [128, 4])):
                R[nm] = sb(pb, "r_" + nm, shp, F32)
            Ind = sb(pb, "Ind", [128, 4, 32], BF16)
            rb = Buf()
            xT_B = xTo_v
            attn_v = attn_scr.rearrange("(j p) t -> p j t", p=128)
            xo_v = xo.rearrange("(s p) d -> p s d", p=128)
            h1_v = h1_scr.rearrange("(s p) d -> p s d", p=128)

            def load_tile(T_):
                sl_ = T_ % 2
                dma("pool", lambda: nc.gpsimd.dma_start(out=xt[sl_][:], in_=xT_B[:, :, T_ * 512:(T_ + 1) * 512]), writes=[inb[sl_]])
                dma("pool", lambda: nc.gpsimd.dma_start(out=xh[sl_][:], in_=xTh_v[:, :, T_ * 8:(T_ + 1) * 8]), writes=[inb[sl_]])
                dma("sp", lambda: nc.sync.dma_start(out=at[sl_][:], in_=attn_v[:, :, T_ * 512:(T_ + 1) * 512]), writes=[inb[sl_]])

            pending_route = []

            def flush_route():
                while pending_route:
                    Tr = pending_route.pop(0)
                    routing(nc, op, dma, R, Ind, Lg, Lgb, rb, ustr_bf, ones_bf, b_const, psf, bankbuf, next_bank,
                            base, ebase, D1, D2, RW1, RW2, Tr, wB)
                    for s_ in range(4):
                        gs_ = 4 * Tr + s_
                        for Dk in (D1, D2):
                            dma("pool", lambda: nc.gpsimd.indirect_dma_start(
                                out=xg_scr, out_offset=bass.IndirectOffsetOnAxis(ap=Dk[:, gs_:gs_ + 1], axis=0),
                                in_=h1b[s_][:, :], in_offset=None, bounds_check=bc_reg, oob_is_err=False),
                                reads=[h1bb[s_], rb])

            load_tile(0)
            dma("sp", lambda: nc.sync.dma_start(out=xtok[0][:], in_=xo_v[:, 0:4, :]), writes=[xtokb])
            for T in range(8):
                sl = T % 2
                if T + 1 < 8:
                    load_tile(T + 1)
                for m in range(4):
                    bcc, bch, bcb, bh = next_bank(), next_bank(), next_bank(), next_bank()
                    mm_group(psf[:, bcc, :], bankbuf[bcc],
                             [(wc[:, k, 512 + m * 128:512 + (m + 1) * 128], xt[sl][:, k, :]) for k in range(8)], [wB, inb[sl]])
                    mm_group(psf[:, bch, :], bankbuf[bch],
                             [(wc[:, k, 1024 + m * 128:1024 + (m + 1) * 128], xt[sl][:, k, :]) for k in range(8)], [wB, inb[sl]])
                    mm_group(psf[:, bcb, :], bankbuf[bcb],
                             [(wc[:, k, m * 128:(m + 1) * 128], xt[sl][:, k, :]) for k in range(8)], [wB, inb[sl]])

                    def fnh():
                        ins = None
                        for k in range(8):
                            ins = nc.tensor.matmul(psf[:, bh, 0:8], wc[:, k, 512 + m * 128:512 + (m + 1) * 128],
                                                   xh[sl][:, k, :], start=(k == 0), stop=(k == 7))
                        for k in range(8):
                            ins = nc.tensor.matmul(psf[:, bh, 8:16], wc[:, k, 1024 + m * 128:1024 + (m + 1) * 128],
                                                   xh[sl][:, k, :], start=(k == 0), stop=(k == 7))
                        return ins
                    op("pe", fnh, reads=[wB, inb[sl]], writes=[bankbuf[bh]])
                    op("act", lambda: nc.scalar.copy(out=ccs[:, :], in_=psf[:, bcc, :]), reads=[bankbuf[bcc]], writes=[ccsb])
                    op("act", lambda: nc.scalar.copy(out=cchs[:, :], in_=psf[:, bh, 0:8]), reads=[bankbuf[bh]], writes=[ccsb])
                    op("dve", lambda: nc.vector.tensor_tensor(out=U[:, :, 0:128],
                                                              in0=ccs[:, :].rearrange("p (b t) -> p b t", b=4),
                                                              in1=psf[:, bch, :].rearrange("p (b t) -> p b t", b=4),
                                                              op=ALU.mult), reads=[ccsb, bankbuf[bch]], writes=[Ub])
                    op("dve", lambda: nc.vector.tensor_tensor(out=U[:, :, 128:130],
                                                              in0=cchs[:, :].rearrange("p (b t) -> p b t", b=4),
                                                              in1=psf[:, bh, 8:16].rearrange("p (b t) -> p b t", b=4),
                                                              op=ALU.mult), reads=[ccsb, bankbuf[bh]], writes=[Ub])
                    op("dve", lambda: nc.vector.tensor_scalar(out=Y[:, :, :], in0=U[:, :, 0:128],
                                                              scalar1=cw_sb[:, m * 3 + 2:m * 3 + 3], scalar2=None,
                                                              op0=ALU.mult), reads=[Ub, wB], writes=[Yb])
                    op("dve", lambda: nc.vector.scalar_tensor_tensor(out=Y[:, :, :], in0=U[:, :, 1:129],
                                                                     scalar=cw_sb[:, m * 3 + 1:m * 3 + 2], in1=Y[:, :, :],
                                                                     op0=ALU.mult, op1=ALU.add), reads=[Ub, Yb], writes=[Yb])
                    op("dve", lambda: nc.vector.scalar_tensor_tensor(out=Y[:, :, :], in0=U[:, :, 2:130],
                                                                     scalar=cw_sb[:, m * 3:m * 3 + 1], in1=Y[:, :, :],
                                                                     op0=ALU.mult, op1=ALU.add), reads=[Ub, Yb], writes=[Yb])
                    op("dve", lambda: nc.vector.tensor_tensor(out=Bin[:, m, :], in0=Y[:, :, :].rearrange("p b t -> p (b t)"),
                                                              in1=psf[:, bcb, :], op=ALU.mult),
                       reads=[Yb, bankbuf[bcb]], writes=[Binb])
                flush_route()
                for m in range(8):
                    s2 = m % 2
                    bA, bB, bga, bgb = next_bank(), next_bank(), next_bank(), next_bank()
                    mm_group(psf[:, bA, :], bankbuf[bA],
                             [(wba[:, k, m * 128:(m + 1) * 128], at[sl][:, k, :]) for k in range(4)], [wB, inb[sl]])
                    mm_group(psf[:, bB, :], bankbuf[bB],
                             [(wbb[:, k, m * 128:(m + 1) * 128], Bin[:, k, :]) for k in range(4)], [wB, Binb])
                    mm_group(psf[:, bga, :], bankbuf[bga],
                             [(wg[:, k, m * 128:(m + 1) * 128], xt[sl][:, k, :]) for k in range(8)], [wB, inb[sl]])
                    mm_group(psf[:, bgb, :], bankbuf[bgb],
                             [(wg[:, k, 1024 + m * 128:1024 + (m + 1) * 128], xt[sl][:, k, :]) for k in range(8)], [wB, inb[sl]])
                    op("act", lambda: nc.scalar.activation(out=sa[s2][:, :], in_=psf[:, bga, :], func=AF.Sigmoid,
                                                           bias=gb_sb[:, m:m + 1]), reads=[bankbuf[bga], wB], writes=[sab[s2]])
                    op("act", lambda: nc.scalar.activation(out=sbg[s2][:, :], in_=psf[:, bgb, :], func=AF.Sigmoid,
                                                           bias=gb_sb[:, 8 + m:9 + m]), reads=[bankbuf[bgb], wB], writes=[sab[s2]])
                    op("dve", lambda: nc.vector.tensor_tensor(out=t1[s2][:, :], in0=sa[s2][:, :], in1=psf[:, bA, :], op=ALU.mult),
                       reads=[sab[s2], bankbuf[bA]], writes=[t1b[s2]])
                    op("dve", lambda: nc.vector.tensor_tensor(out=t2[s2][:, :], in0=sbg[s2][:, :], in1=psf[:, bB, :], op=ALU.mult),
                       reads=[sab[s2], bankbuf[bB]], writes=[t1b[s2]])
                    op("pool", lambda: nc.gpsimd.tensor_tensor(out=GT[:, m, :], in0=t1[s2][:, :], in1=t2[s2][:, :], op=ALU.add),
                       reads=[t1b[s2]], writes=[GTb])
                for s in range(4):
                    s2 = s
                    bk0, bk1 = next_bank(), next_bank()
                    for half, bk in ((0, bk0), (1, bk1)):
                        mm_group(psf[:, bk, :], bankbuf[bk],
                                 [(GT[:, k, s * 128:(s + 1) * 128], wo[:, k, half * 512:(half + 1) * 512]) for k in range(8)],
                                 [wB, GTb])
                        op("dve", lambda: nc.vector.scalar_tensor_tensor(out=hbuf[s2][:, half * 512:(half + 1) * 512],
                                                                         in0=xtok[0][:, s, half * 512:(half + 1) * 512],
                                                                         scalar=ALPHA, in1=psf[:, bk, :],
                                                                         op0=ALU.mult, op1=ALU.add),
                           reads=[xtokb, bankbuf[bk]], writes=[hb_[s2]])
                if T + 1 < 8:
                    dma("sp", lambda: nc.sync.dma_start(out=xtok[0][:], in_=xo_v[:, 4 * (T + 1):4 * (T + 1) + 4, :]), writes=[xtokb])
                for s in range(4):
                    for half in range(2):
                        op("dve", lambda: nc.vector.bn_stats(out=stats[s][:, half, :], in_=hbuf[s][:, half * 512:(half + 1) * 512]),
                           reads=[hb_[s]], writes=[stb[s]])
                    op("dve", lambda: nc.vector.bn_aggr(out=mv[s][:, :], in_=stats[s][:, :, :].rearrange("p a b -> p (a b)")),
                       reads=[stb[s]], writes=[stb[s]])
                for s in range(4):
                    op("act", lambda: nc.scalar.activation(out=rstd[s][:, :], in_=mv[s][:, 1:2], func=AF.Sqrt, bias=epsT[:, 0:1]),
                       reads=[stb[s], wB], writes=[stb[s]])
                for s in range(4):
                    op("dve", lambda: nc.vector.reciprocal(out=rstd[s][:, :], in_=rstd[s][:, :]), reads=[stb[s]], writes=[stb[s]])
                    op("dve", lambda: nc.vector.tensor_scalar(out=hbuf[s][:, :], in0=hbuf[s][:, :], scalar1=mv[s][:, 0:1],
                                                              scalar2=rstd[s][:, 0:1], op0=ALU.subtract, op1=ALU.mult),
                       reads=[stb[s], hb_[s]], writes=[hb_[s]])
                    op("dve", lambda: nc.vector.tensor_tensor(out=hbuf[s][:, :], in0=hbuf[s][:, :], in1=g1[:, :], op=ALU.mult),
                       reads=[hb_[s], wB], writes=[hb_[s]])
                    op("pool", lambda: nc.gpsimd.tensor_tensor(out=hbuf[s][:, :], in0=hbuf[s][:, :], in1=b1[:, :], op=ALU.add),
                       reads=[hb_[s], wB], writes=[hb_[s]])
                for s in range(4):
                    gs = 4 * T + s
                    dma("sp", lambda: nc.sync.dma_start(out=h1_v[:, gs, :], in_=hbuf[s][:, :]), reads=[hb_[s]])
                    op("act", lambda: nc.scalar.copy(out=h1b[s][:, :], in_=hbuf[s][:, :]), reads=[hb_[s]], writes=[h1bb[s]])
                for s in range(4):
                    s2 = s
                    hT_ = h1T[s % 2]
                    hTb_ = h1Tb[s % 2]
                    tb0, tb1 = next_bank(), next_bank()

                    def fntr():
                        ins = None
                        for k in range(8):
                            tb = tb0 if k < 4 else tb1
                            ins = nc.tensor.transpose(psf[:, tb, (k % 4) * 128:(k % 4 + 1) * 128],
                                                      hbuf[s2][:, k * 128:(k + 1) * 128], ident_f[:, :])
                        return ins
                    op("pe", fntr, reads=[hb_[s2], b_const], writes=[bankbuf[tb0], bankbuf[tb1]])
                    op("act", lambda: nc.scalar.copy(out=hT_[:, 0:4, :], in_=psf[:, tb0, :].rearrange("p (k t) -> p k t", k=4)),
                       reads=[bankbuf[tb0]], writes=[hTb_])
                    op("dve", lambda: nc.vector.tensor_copy(out=hT_[:, 4:8, :], in_=psf[:, tb1, :].rearrange("p (k t) -> p k t", k=4)),
                       reads=[bankbuf[tb1]], writes=[hTb_])
                    lb = next_bank()
                    mm_group(psf[:, lb, 0:36], bankbuf[lb],
                             [(hT_[:, k, :], wr[:, k, :]) for k in range(8)], [hTb_, wB])
                    op("dve", lambda: nc.vector.tensor_tensor(out=Lg[:, s, :], in0=psf[:, lb, 0:36], in1=br[:, :], op=ALU.add),
                       reads=[bankbuf[lb], wB], writes=[Lgb])
                pending_route.append(T)
            flush_route()
            cx.barrier()
            if stage == 2:
                op("dve", lambda: nc.vector.tensor_copy(out=zeros[:, 0:32], in_=D1[:, :]), writes=[b_const])
                op("dve", lambda: nc.vector.tensor_copy(out=zeros[:, 32:64], in_=D2[:, :]), writes=[b_const])
                op("dve", lambda: nc.vector.tensor_copy(out=zeros[:, 64:96], in_=RW1[:, :]), writes=[b_const])
                op("dve", lambda: nc.vector.tensor_copy(out=zeros[:, 96:128], in_=RW2[:, :]), writes=[b_const])
                dma("sp", lambda: nc.sync.dma_start(out=dbg_r, in_=zeros[:, 0:128]), reads=[b_const])
                cx.barrier()
                return nc

        with ExitStack() as pc:
            NW = 3
            wgs = [sb(pc, "wgs%d" % i, [128, 8, 512], BF16) for i in range(NW)]
            wus = [sb(pc, "wus%d" % i, [128, 8, 512], BF16) for i in range(NW)]
            wds = [sb(pc, "wds%d" % i, [128, 4, D], BF16) for i in range(NW)]
            wEb = [[Buf(), Buf(), Buf()] for _ in range(NW)]
            xgs = [sb(pc, "xgs%d" % i, [128, 3, D], BF16) for i in range(NW)]
            xgb = [Buf() for _ in range(NW)]
            xbT = [sb(pc, "xbT%d" % i, [128, 8, CAP], BF16) for i in range(2)]
            xbTb = [Buf(), Buf()]
            sg = [sb(pc, "sg%d" % i, [128, CAP], F32) for i in range(2)]
            sgb = [Buf(), Buf()]
            hT = [sb(pc, "hT%d" % i, [128, 4, CAP], BF16) for i in range(2)]
            hTb = [Buf(), Buf()]
            ysb = [sb(pc, "ysb%d" % i, [128, D], F32) for i in range(2)]
            ysbb = [Buf(), Buf()]
            xg_v = xg_scr.rearrange("(e j p) d -> e p j d", p=128, j=3)
            ys_v = ys_scr.rearrange("(e j p) d -> e p j d", p=128, j=3)

            def load_w(e):
                sl = e % NW
                dma("pool", lambda: nc.gpsimd.dma_start(out=wgs[sl][:], in_=w_gate[e].rearrange("(k p) f -> p k f", p=128)), writes=[wEb[sl][0]])
                dma("pool", lambda: nc.gpsimd.dma_start(out=wus[sl][:], in_=w_up[e].rearrange("(k p) f -> p k f", p=128)), writes=[wEb[sl][1]])
                dma("pool", lambda: nc.gpsimd.dma_start(out=wds[sl][:], in_=w_down[e].rearrange("(k p) f -> p k f", p=128)), writes=[wEb[sl][2]])
                dma("sp", lambda: nc.sync.dma_start(out=xgs[sl][:], in_=xg_v[e]), writes=[xgb[sl]])

            def do_transposes(e):
                sl = e % NW
                xb = xbT[e % 2]
                for j in range(3):
                    def fnt():
                        ins = None
                        for k in range(8):
                            ins = nc.tensor.transpose(psb[:, j % 2, k * 128:(k + 1) * 128], xgs[sl][:, j, k * 128:(k + 1) * 128],
                                                      ident_bf[:, :])
                        return ins
                    op("pe", fnt, reads=[xgb[sl], b_const], writes=[pbbuf[j % 2]])
                    evac(xb[:, :, j * 128:(j + 1) * 128], psb[:, j % 2, :].rearrange("p (k t) -> p k t", k=8),
                         [pbbuf[j % 2]], [xbTb[e % 2]])

            def do_gate_up(e):
                sl = e % NW
                xb = xbT[e % 2]
                for f in range(4):
                    s2 = f % 2
                    bg, bu = next_bank(), next_bank()
                    mm_group(psf[:, bg, 0:CAP], bankbuf[bg],
                             [(wgs[sl][:, k, f * 128:(f + 1) * 128], xb[:, k, :]) for k in range(8)], wEb[sl] + [xbTb[e % 2]])
                    mm_group(psf[:, bu, 0:CAP], bankbuf[bu],
                             [(wus[sl][:, k, f * 128:(f + 1) * 128], xb[:, k, :]) for k in range(8)], wEb[sl] + [xbTb[e % 2]])
                    op("act", lambda: nc.scalar.activation(out=sg[s2][:, :], in_=psf[:, bg, 0:CAP], func=AF.Silu),
                       reads=[bankbuf[bg]], writes=[sgb[s2]])
                    op("dve", lambda: nc.vector.tensor_tensor(out=hT[e % 2][:, f, :], in0=sg[s2][:, :], in1=psf[:, bu, 0:CAP], op=ALU.mult),
                       reads=[sgb[s2], bankbuf[bu]], writes=[hTb[e % 2]])

            def do_down(e):
                sl = e % NW
                for j in range(3):
                    s2 = j % 2
                    for half in range(2):
                        bk = next_bank()
                        mm_group(psf[:, bk, :], bankbuf[bk],
                                 [(hT[e % 2][:, f, j * 128:(j + 1) * 128], wds[sl][:, f, half * 512:(half + 1) * 512]) for f in range(4)],
                                 wEb[sl] + [hTb[e % 2]])
                        op("act" if half == 0 else "dve",
                           (lambda: nc.scalar.copy(out=ysb[s2][:, 0:512], in_=psf[:, bk, :])) if half == 0 else
                           (lambda: nc.vector.tensor_copy(out=ysb[s2][:, 512:1024], in_=psf[:, bk, :])),
                           reads=[bankbuf[bk]], writes=[ysbb[s2]])
                    dma("sp", lambda: nc.sync.dma_start(out=ys_v[e][:, j, :], in_=ysb[s2][:, :]), reads=[ysbb[s2]])

            load_w(0)
            load_w(1)
            do_transposes(0)
            for e in range(32):
                if e + 2 < 32:
                    load_w(e + 2)
                do_gate_up(e)
                if e + 1 < 32:
                    do_transposes(e + 1)
                do_down(e)
            cx.barrier()

        if stage == 3:
            return nc
        with ExitStack() as pd:
            g2 = sb(pd, "g2", [128, D], F32)
            b2 = sb(pd, "b2", [128, D], F32)
            wD = Buf()
            dma("sp", lambda: nc.sync.dma_start(out=g2[:], in_=lnp[2].partition_broadcast(128)), writes=[wD])
            dma("sp", lambda: nc.sync.dma_start(out=b2[:], in_=lnp[3].partition_broadcast(128)), writes=[wD])
            cx.barrier()
            NR = 4
            r1 = [sb(pd, "r1%d" % i, [128, D], F32) for i in range(NR)]
            r2 = [sb(pd, "r2%d" % i, [128, D], F32) for i in range(NR)]
            hh = [sb(pd, "hh%d" % i, [128, D], F32) for i in range(NR)]
            stats = sb(pd, "stats2", [128, 2, 6], F32)
            mv = sb(pd, "mv2", [128, 2], F32)
            rstd = sb(pd, "rstd2", [128, 1], F32)
            stb = Buf()
            r1b, r2b, hhb = [Buf() for _ in range(NR)], [Buf() for _ in range(NR)], [Buf() for _ in range(NR)]
            h1_v = h1_scr.rearrange("(s p) d -> p s d", p=128)
            out_v = out.rearrange("(s p) d -> p s d", p=128)

            def loads_d(gs):
                s2 = gs % NR
                for rr, rrb in ((r1, r1b), (r2, r2b)):
                    for half in range(2):
                        op("act", lambda: nc.scalar.copy(out=rr[s2][:, half * 512:(half + 1) * 512], in_=zeros[:, :]),
                           reads=[b_const], writes=[rrb[s2]])
                dma("pool", lambda: nc.gpsimd.indirect_dma_start(
                    out=r1[s2][:, :], out_offset=None, in_=ys_scr,
                    in_offset=bass.IndirectOffsetOnAxis(ap=D1[:, gs:gs + 1], axis=0),
                    bounds_check=bc_reg, oob_is_err=False), writes=[r1b[s2]])
                dma("pool", lambda: nc.gpsimd.indirect_dma_start(
                    out=r2[s2][:, :], out_offset=None, in_=ys_scr,
                    in_offset=bass.IndirectOffsetOnAxis(ap=D2[:, gs:gs + 1], axis=0),
                    bounds_check=bc_reg, oob_is_err=False), writes=[r2b[s2]])
                dma("sp", lambda: nc.sync.dma_start(out=hh[s2][:, :], in_=h1_v[:, gs, :]), writes=[hhb[s2]])

            epsT2 = sb(pd, "epsT2", [128, 1], F32)
            rwb = Buf()
            op("pool", lambda: nc.gpsimd.memset(epsT2[:], EPS / (ALPHA * ALPHA)), writes=[rwb])
            op("dve", lambda: nc.vector.tensor_scalar(out=RW1[:, :], in0=RW1[:, :], scalar1=1.0 / ALPHA, scalar2=None, op0=ALU.mult),
               writes=[rwb])
            op("dve", lambda: nc.vector.tensor_scalar(out=RW2[:, :], in0=RW2[:, :], scalar1=1.0 / ALPHA, scalar2=None, op0=ALU.mult),
               writes=[rwb])
            cx.barrier()
            loads_d(0)
            loads_d(1)
            for gs in range(32):
                s2 = gs % NR
                if gs + 2 < 32:
                    loads_d(gs + 2)
                op("dve", lambda: nc.vector.scalar_tensor_tensor(out=hh[s2][:, :], in0=r1[s2][:, :], scalar=RW1[:, gs:gs + 1],
                                                                 in1=hh[s2][:, :], op0=ALU.mult, op1=ALU.add),
                   reads=[r1b[s2], hhb[s2]], writes=[hhb[s2]])
                op("dve", lambda: nc.vector.scalar_tensor_tensor(out=hh[s2][:, :], in0=r2[s2][:, :], scalar=RW2[:, gs:gs + 1],
                                                                 in1=hh[s2][:, :], op0=ALU.mult, op1=ALU.add),
                   reads=[r2b[s2], hhb[s2]], writes=[hhb[s2]])
                layer_norm(nc, op, hh[s2], hhb[s2], stats, mv, rstd, stb, g2, b2, wD, epsT2)
                dma("sp", lambda: nc.sync.dma_start(out=out_v[:, gs, :], in_=hh[s2][:, :]), reads=[hhb[s2]])
            cx.barrier()
    return nc


def fence(cx, op, nc, rstd_like=None):
    cx.barrier()


def layer_norm(nc, op, h, hb, stats, mv, rstd, stb, g, b, wB, epsT, gmul_on_dve=False):
    for half in range(2):
        op("dve", lambda: nc.vector.bn_stats(out=stats[:, half, :], in_=h[:, half * 512:(half + 1) * 512]),
           reads=[hb], writes=[stb])
    op("dve", lambda: nc.vector.bn_aggr(out=mv[:, :], in_=stats[:, :, :].rearrange("p a b -> p (a b)")), reads=[stb], writes=[stb])
    op("act", lambda: nc.scalar.activation(out=rstd[:, :], in_=mv[:, 1:2], func=AF.Sqrt, bias=epsT[:, 0:1]),
       reads=[stb, wB], writes=[stb])
    op("dve", lambda: nc.vector.reciprocal(out=rstd[:, :], in_=rstd[:, :]), reads=[stb], writes=[stb])
    op("dve", lambda: nc.vector.tensor_scalar(out=h[:, :], in0=h[:, :], scalar1=mv[:, 0:1], scalar2=rstd[:, 0:1],
                                              op0=ALU.subtract, op1=ALU.mult), reads=[stb, hb], writes=[hb])
    if gmul_on_dve:
        op("dve", lambda: nc.vector.tensor_tensor(out=h[:, :], in0=h[:, :], in1=g[:, :], op=ALU.mult), reads=[hb, wB], writes=[hb])
    else:
        op("pool", lambda: nc.gpsimd.tensor_tensor(out=h[:, :], in0=h[:, :], in1=g[:, :], op=ALU.mult), reads=[hb, wB], writes=[hb])
    op("pool", lambda: nc.gpsimd.tensor_tensor(out=h[:, :], in0=h[:, :], in1=b[:, :], op=ALU.add), reads=[hb, wB], writes=[hb])


def routing(nc, op, dma, R, Ind, Lg, Lgb, rb, ustr_bf, ones_bf, b_const, psf, bankbuf, next_bank,
            base, ebase, D1, D2, RW1, RW2, T, wB):
    V_ = nc.vector

    def dv(fn, extra_r=()):
        op("dve", fn, reads=[rb, Lgb] + list(extra_r), writes=[rb])
    lg = Lg[:, :, 0:4]
    le = Lg[:, :, 4:36].rearrange("p s (g e) -> p s g e", g=4)
    dv(lambda: V_.tensor_reduce(out=R["gmax"][:, :], in_=lg, axis=AX.X, op=ALU.max))
    dv(lambda: V_.tensor_tensor(out=R["ohg"][:, :, :], in0=lg, in1=R["gmax"][:, :].unsqueeze(2).to_broadcast([128, 4, 4]),
                                op=ALU.is_equal))
    dv(lambda: V_.tensor_tensor(out=R["eg"][:, :, :], in0=lg, in1=R["gmax"][:, :].unsqueeze(2).to_broadcast([128, 4, 4]),
                                op=ALU.subtract))
    op("act", lambda: nc.scalar.activation(out=R["eg"][:, :, :], in_=R["eg"][:, :, :], func=AF.Exp), reads=[rb], writes=[rb])
    dv(lambda: V_.tensor_reduce(out=R["sumg"][:, :], in_=R["eg"][:, :, :], axis=AX.X, op=ALU.add))
    dv(lambda: V_.reciprocal(out=R["gp"][:, :], in_=R["sumg"][:, :]))
    dv(lambda: V_.tensor_tensor(out=R["prod"][:, :, :, :], in0=le,
                                in1=R["ohg"][:, :, :].unsqueeze(3).to_broadcast([128, 4, 4, 8]), op=ALU.mult))
    dv(lambda: V_.tensor_reduce(out=R["sel"][:, :, :], in_=R["prod"][:, :, :, :].rearrange("p s g e -> p s e g"),
                                axis=AX.X, op=ALU.add))
    dv(lambda: V_.tensor_reduce(out=R["m1"][:, :], in_=R["sel"][:, :, :], axis=AX.X, op=ALU.max))
    dv(lambda: V_.tensor_tensor(out=R["oh1"][:, :, :], in0=R["sel"][:, :, :],
                                in1=R["m1"][:, :].unsqueeze(2).to_broadcast([128, 4, 8]), op=ALU.is_equal))
    dv(lambda: V_.scalar_tensor_tensor(out=R["sel2"][:, :, :], in0=R["oh1"][:, :, :], scalar=-1e30, in1=R["sel"][:, :, :],
                                       op0=ALU.mult, op1=ALU.add))
    dv(lambda: V_.tensor_reduce(out=R["m2"][:, :], in_=R["sel2"][:, :, :], axis=AX.X, op=ALU.max))
    dv(lambda: V_.tensor_tensor(out=R["oh2"][:, :, :], in0=R["sel2"][:, :, :],
                                in1=R["m2"][:, :].unsqueeze(2).to_broadcast([128, 4, 8]), op=ALU.is_equal))
    dv(lambda: V_.tensor_tensor(out=R["dm"][:, :], in0=R["m1"][:, :], in1=R["m2"][:, :], op=ALU.subtract))
    op("act", lambda: nc.scalar.activation(out=R["w1"][:, :], in_=R["dm"][:, :], func=AF.Sigmoid), reads=[rb], writes=[rb])
    dv(lambda: V_.tensor_scalar(out=R["w2"][:, :], in0=R["w1"][:, :], scalar1=-1.0, scalar2=1.0, op0=ALU.mult, op1=ALU.add))
    dv(lambda: V_.tensor_tensor(out=RW1[:, 4 * T:4 * T + 4], in0=R["w1"][:, :], in1=R["gp"][:, :], op=ALU.mult))
    dv(lambda: V_.tensor_tensor(out=RW2[:, 4 * T:4 * T + 4], in0=R["w2"][:, :], in1=R["gp"][:, :], op=ALU.mult))
    ohg_b = R["ohg"][:, :, :].unsqueeze(3).to_broadcast([128, 4, 4, 8])
    for nm, src in (("OH1", "oh1"), ("OH2", "oh2")):
        dv(lambda: V_.tensor_tensor(out=R[nm][:, :, :].rearrange("p s (g e) -> p s g e", g=4), in0=ohg_b,
                                    in1=R[src][:, :, :].unsqueeze(2).to_broadcast([128, 4, 4, 8]), op=ALU.mult))
    dv(lambda: V_.tensor_tensor(out=Ind[:, :, :], in0=R["OH1"][:, :, :], in1=R["OH2"][:, :, :], op=ALU.add))
    bR, bT = next_bank(), next_bank()
    ind2 = Ind[:, :, :].rearrange("p s e -> p (s e)")
    op("pe", lambda: nc.tensor.matmul(psf[:, bR, 0:128], ustr_bf[:, :], ind2, start=True, stop=True),
       reads=[rb, b_const], writes=[bankbuf[bR]])
    op("pe", lambda: nc.tensor.matmul(psf[:, bT, 0:128], ones_bf[:, :], ind2, start=True, stop=True),
       reads=[rb, b_const], writes=[bankbuf[bT]])
    for s in range(4):
        dv(lambda: V_.tensor_tensor(out=R["Rk"][:, s, :], in0=psf[:, bR, s * 32:(s + 1) * 32], in1=base[:, :], op=ALU.add),
           extra_r=[bankbuf[bR], wB])
        op("dve", lambda: V_.tensor_tensor(out=base[:, :], in0=base[:, :], in1=psf[:, bT, s * 32:(s + 1) * 32], op=ALU.add),
           reads=[rb, wB, bankbuf[bT]], writes=[rb, wB])
    dv(lambda: V_.tensor_scalar(out=R["ov"][:, :, :], in0=R["Rk"][:, :, :], scalar1=float(CAP) - 0.5, scalar2=1.0e6,
                                op0=ALU.is_ge, op1=ALU.mult))
    dv(lambda: V_.tensor_tensor(out=R["Rk"][:, :, :], in0=R["Rk"][:, :, :], in1=R["ov"][:, :, :], op=ALU.add))
    dv(lambda: V_.tensor_tensor(out=R["Rk"][:, :, :], in0=R["Rk"][:, :, :],
                                in1=ebase[:, :].unsqueeze(1).to_broadcast([128, 4, 32]), op=ALU.add), extra_r=[b_const])
    for nm, dst, Dk in (("OH1", "d1f", D1), ("OH2", "d2f", D2)):
        dv(lambda: V_.tensor_tensor(out=R[nm][:, :, :], in0=R[nm][:, :, :], in1=R["Rk"][:, :, :], op=ALU.mult))
        dv(lambda: V_.tensor_reduce(out=R[dst][:, :], in_=R[nm][:, :, :], axis=AX.X, op=ALU.add))
        dv(lambda: V_.tensor_copy(out=Dk[:, 4 * T:4 * T + 4], in_=R[dst][:, :]))


_NC_CACHE = {}


def _prep_core(c, x, shared):
    b, p = c // 2, c % 2
    xr = x[b, ::-1, :]
    blocks = [2 * i + p for i in range(NBLK)]
    rows_o = np.concatenate([np.arange(128 * a, 128 * a + 128) for a in blocks])
    xo = xr[rows_o]
    halo = np.zeros((64, D), np.float32)
    for i, a in enumerate(blocks):
        r0 = 128 * (a + 1)
        if r0 < S:
            halo[2 * i] = xr[r0]
            halo[2 * i + 1] = xr[r0 + 1]
    tri = np.where(np.arange(128)[None, :] <= np.arange(128)[:, None], MASKV, 0.0).astype(np.float32)
    if p == 0:
        mask = np.concatenate([tri, np.zeros((128, 128), np.float32)], axis=1)
    else:
        mask = np.concatenate([np.full((128, 128), MASKV, np.float32), tri], axis=1)
    ident = np.eye(128, dtype=np.float32)
    ustr = (np.arange(128)[:, None] < np.arange(128)[None, :]).astype(np.float32)
    ones = np.ones((128, 128), np.float32)
    eb = np.tile((np.arange(32, dtype=np.float32) * CAP)[None, :], (128, 1))
    m01 = (mask == 0.0).astype(np.float32)
    consts = np.ascontiguousarray(np.concatenate([ident, ustr, ones, mask, eb, m01], axis=1))
    m = dict(shared)
    m.update({
        "xT": np.ascontiguousarray(xr.T),
        "xTsh": np.ascontiguousarray(np.concatenate([xr.T[:, 1:], np.zeros((D, 1), np.float32)], axis=1)),
        "xTs": np.ascontiguousarray(xr[0:S:256].T),
        "xTo": np.ascontiguousarray(xo.T),
        "xTsho": np.ascontiguousarray(np.concatenate([xr, np.zeros((1, D), np.float32)], axis=0)[rows_o + 1].T),
        "xTh": np.ascontiguousarray(halo.T),
        "xo": np.ascontiguousarray(xo),
        "consts": consts,
    })
    return m


def _prep_shared(w_in, gate_bias, conv_w, w_branch_a, w_branch_b, w_out, ln1_g, ln1_b,
                 w_router_g, b_router_g, w_router_e, b_router_e, w_gate, w_up, w_down, ln2_g, ln2_b):
    f = lambda a: np.ascontiguousarray(np.asarray(a, dtype=np.float32))
    gb = f(gate_bias[0]).reshape(16, 128).T
    cwp = f(conv_w[0]).reshape(3, 4, 128).transpose(2, 1, 0).reshape(128, 12)
    return {
        "w_in": f(w_in[0]), "gbias": f(gb), "cw": f(cwp),
        "w_ba": f(w_branch_a[0]), "w_bb": f(w_branch_b[0]), "w_out": f(w_out[0]),
        "lnp": f(np.stack([ln1_g[0], ln1_b[0], ln2_g[0], ln2_b[0]], axis=0)),
        "w_r": f(np.concatenate([w_router_g[0], w_router_e[0]], axis=1)),
        "b_r": f(np.concatenate([b_router_g[0], b_router_e[0].reshape(-1)], axis=0)),
        "w_gate": f(w_gate[0]), "w_up": f(w_up[0]), "w_down": f(w_down[0]),
    }


def kernel(x, w_in, gate_bias, conv_w, w_branch_a, w_branch_b, w_out, ln1_g, ln1_b,
           w_router_g, b_router_g, w_router_e, b_router_e, w_gate, w_up, w_down, ln2_g, ln2_b):
    x = np.asarray(x, dtype=np.float32)
    shared = _prep_shared(w_in, gate_bias, conv_w, w_branch_a, w_branch_b, w_out, ln1_g, ln1_b,
                          w_router_g, b_router_g, w_router_e, b_router_e, w_gate, w_up, w_down, ln2_g, ln2_b)
    in_maps = [_prep_core(c, x, shared) for c in range(8)]
    nc = build(4)
    res = run_bass_kernel_spmd(nc, in_maps, core_ids=list(range(8)))
    out = np.zeros((4, S, D), np.float32)
    for c in range(8):
        b, p = c // 2, c % 2
        oc = np.asarray(res.results[c]["out"]).reshape(NOWN, D)
        for i in range(NBLK):
            a = 2 * i + p
            rr = np.arange(128 * a, 128 * a + 128)
            out[b, S - 1 - rr] = oc[128 * i:128 * i + 128]
    return out
```

```python
import os
import numpy as np
from contextlib import ExitStack
import concourse.bass as bass
import concourse.mybir as mybir
from concourse.bass_utils import run_bass_kernel_spmd

F32 = mybir.dt.float32
BF16 = mybir.dt.bfloat16
I32 = mybir.dt.int32
AF = mybir.ActivationFunctionType
ALU = mybir.AluOpType
AX = mybir.AxisListType

S = 8192
D = 1024
NOWN = 4096
NBLK = 32
CAP = 384
NSLOT = 32 * CAP
ALPHA = 2.0 ** 0.25
EPS = 1e-5
MASKV = -30000.0
NDMA = 12


class Buf:
    __slots__ = ("lw", "rd")

    def __init__(self):
        self.lw = {}
        self.rd = {}


class Ctx:
    def __init__(self, nc, es):
        self.nc = nc
        self.eng = {"pe": nc.tensor, "act": nc.scalar, "dve": nc.vector, "pool": nc.gpsimd, "sp": nc.sync}
        self.semobj = {}
        self.cnt = {}
        self.waited = {e: {} for e in self.eng}
        for e in self.eng:
            self.semobj[e] = es.enter_context(nc.semaphore("s_" + e))
            self.cnt[e] = 0
        self.rr = {"sp": 0, "pool": 0}
        for q in ("sp", "pool"):
            for i in range(NDMA):
                k = (q, i)
                self.semobj[k] = es.enter_context(nc.semaphore("d_%s%d" % (q, i)))
                self.cnt[k] = 0

    def _deps(self, reads, writes):
        need = {}
        for b in reads:
            for k, v in b.lw.items():
                if need.get(k, 0) < v:
                    need[k] = v
        for b in writes:
            for k, v in b.lw.items():
                if need.get(k, 0) < v:
                    need[k] = v
            for k, v in b.rd.items():
                if need.get(k, 0) < v:
                    need[k] = v
        return need

    def _wait(self, e, need):
        eng = self.eng[e]
        w = self.waited[e]
        for k, v in need.items():
            if k == e and e == "pe":
                continue
            if w.get(k, 0) >= v:
                continue
            eng.wait_ge(self.semobj[k], v)
            w[k] = v

    def _mark(self, ev, reads, writes):
        for b in writes:
            b.lw[ev[0]] = ev[1]
            b.rd = {}
        for b in reads:
            if b.rd.get(ev[0], 0) < ev[1]:
                b.rd[ev[0]] = ev[1]

    def op(self, e, fn, reads=(), writes=()):
        self._wait(e, self._deps(reads, writes))
        ins = fn()
        self.cnt[e] += 1
        ins.then_inc(self.semobj[e], 1)
        self._mark((e, self.cnt[e]), reads, writes)

    def dma(self, q, fn, reads=(), writes=()):
        need = self._deps(reads, writes)
        i = self.rr[q]
        self.rr[q] = (i + 1) % NDMA
        k = (q, i)
        if self.cnt[k] > 0:
            need[k] = max(need.get(k, 0), self.cnt[k])
        self._wait(q, need)
        ins = fn()
        self.cnt[k] += 16
        ins.then_inc(self.semobj[k], 16)
        self._mark((k, self.cnt[k]), reads, writes)

    def barrier(self):
        allv = {k: v for k, v in self.cnt.items() if v > 0}
        for e in self.eng:
            need = {k: v for k, v in allv.items() if k != e}
            self._wait(e, need)


def build(stage=3):
    nc = bass.Bass("TRN2", target_bir_lowering=False)

    def din(name, shape, dt=F32):
        return nc.dram_tensor(name, list(shape), dt, kind="ExternalInput").ap()

    xT = din("xT", [D, S])
    xTsh = din("xTsh", [D, S])
    xTsho = din("xTsho", [D, NOWN])
    xTs = din("xTs", [D, 32])
    xTo = din("xTo", [D, NOWN])
    xTh = din("xTh", [D, 64])
    xo = din("xo", [NOWN, D])
    w_in = din("w_in", [D, 5120])
    gbias = din("gbias", [128, 16])
    cw = din("cw", [128, 12])
    w_ba = din("w_ba", [512, D])
    w_bb = din("w_bb", [512, D])
    w_out = din("w_out", [D, D])
    lnp = din("lnp", [4, D])
    w_r = din("w_r", [D, 36])
    b_r = din("b_r", [36])
    w_gate = din("w_gate", [32, D, 512])
    w_up = din("w_up", [32, D, 512])
    w_down = din("w_down", [32, 512, D])
    consts = din("consts", [128, 128 * 3 + 256 + 32 + 256])
    out = nc.dram_tensor("out", [NOWN, D], F32, kind="ExternalOutput").ap()
    if stage == 1:
        attn_scr = nc.dram_tensor("attn_scr", [512, NOWN], BF16, kind="ExternalOutput").ap()
    else:
        attn_scr = nc.dram_tensor("attn_scr", [512, NOWN], BF16).ap()
    if stage == 2:
        h1_scr = nc.dram_tensor("h1_scr", [NOWN, D], F32, kind="ExternalOutput").ap()
        dbg_r = nc.dram_tensor("dbg_r", [128, 32 * 4], F32, kind="ExternalOutput").ap()
    else:
        h1_scr = nc.dram_tensor("h1_scr", [NOWN, D], F32).ap()
    xg_scr = nc.dram_tensor("xg_scr", [NSLOT, D], BF16).ap()
    vsh_scr = nc.dram_tensor("vsh_scr", [512, NOWN], BF16).ap()
    ys_scr = nc.dram_tensor("ys_scr", [NSLOT, D], F32).ap()

    w_in_v = w_in.rearrange("(k p) n -> p k n", p=128)
    xT_v = xT.rearrange("(k p) t -> p k t", p=128)
    xTo_v = xTo.rearrange("(k p) t -> p k t", p=128)
    xTh_v = xTh.rearrange("(k p) t -> p k t", p=128)
    xTs_v = xTs.rearrange("(k p) t -> p k t", p=128)
    xTsh_v = xTsh.rearrange("(k p) t -> p k t", p=128)
    xTsho_v = xTsho.rearrange("(k p) t -> p k t", p=128)

    with ExitStack() as es:
        E = es.enter_context
        cx = Ctx(nc, es)
        op, dma = cx.op, cx.dma

        def sb(st, name, shape, dt):
            return st.enter_context(nc.sbuf_tensor(name, list(shape), dt))

        ident_bf = sb(es, "ident_bf", [128, 128], BF16)
        ustr_bf = sb(es, "ustr_bf", [128, 128], BF16)
        ones_bf = sb(es, "ones_bf", [128, 128], BF16)
        mask_bf = sb(es, "mask_bf", [128, 256], BF16)
        ident_f = sb(es, "ident_f", [128, 128], F32)
        ebase = sb(es, "ebase", [128, 32], F32)
        D1 = sb(es, "D1", [128, 32], I32)
        D2 = sb(es, "D2", [128, 32], I32)
        RW1 = sb(es, "RW1", [128, 32], F32)
        RW2 = sb(es, "RW2", [128, 32], F32)
        zeros = sb(es, "zeros", [128, 512], F32)
        zeros_bf = sb(es, "zeros_bf", [128, 512], BF16)
        b_const = Buf()
        bc_reg = nc.gpsimd.alloc_register("bc_reg")
        nc.gpsimd.reg_mov(bc_reg, NSLOT - 1)
        dma("pool", lambda: nc.gpsimd.dma_start(out=ident_bf[:], in_=consts[:, 0:128]), writes=[b_const])
        dma("pool", lambda: nc.gpsimd.dma_start(out=ustr_bf[:], in_=consts[:, 128:256]), writes=[b_const])
        dma("pool", lambda: nc.gpsimd.dma_start(out=ones_bf[:], in_=consts[:, 256:384]), writes=[b_const])
        dma("pool", lambda: nc.gpsimd.dma_start(out=mask_bf[:], in_=consts[:, 384:640]), writes=[b_const])
        dma("sp", lambda: nc.sync.dma_start(out=ident_f[:], in_=consts[:, 0:128]), writes=[b_const])
        dma("sp", lambda: nc.sync.dma_start(out=ebase[:], in_=consts[:, 640:672]), writes=[b_const])
        mask01 = sb(es, "mask01", [128, 256], F32)
        dma("sp", lambda: nc.sync.dma_start(out=mask01[:], in_=consts[:, 672:928]), writes=[b_const])
        op("pool", lambda: nc.gpsimd.memset(zeros[:], 0.0), writes=[b_const])
        op("pool", lambda: nc.gpsimd.memset(zeros_bf[:], 0.0), writes=[b_const])
        ones_f = sb(es, "ones_f", [128, 1], F32)
        op("pool", lambda: nc.gpsimd.memset(ones_f[:], 1.0), writes=[b_const])
        epsT = sb(es, "epsT", [128, 1], F32)
        op("pool", lambda: nc.gpsimd.memset(epsT[:], EPS), writes=[b_const])
        cx.barrier()

        ps_stack = [None]

        def alloc_psum(tag, nf, nb):
            if ps_stack[0] is not None:
                ps_stack[0].close()
            st_ = ExitStack()
            es.callback(st_.close)
            ps_stack[0] = st_
            f_ = st_.enter_context(nc.psum_tensor("psf" + tag, [128, nf, 512], F32))
            b_ = st_.enter_context(nc.psum_tensor("psb" + tag, [128, nb, 1024], BF16))
            return f_, b_

        psf, psb = alloc_psum("A", 6, 2)
        bankbuf = [Buf() for _ in range(6)]
        pbbuf = [Buf(), Buf(), Buf()]
        bank_rr = [0]

        def next_bank(n=6):
            i = bank_rr[0] % n
            bank_rr[0] += 1
            return i

        evac_rr = [0]

        def evac(out_ap, in_ap, reads, writes, scale=None):
            evac_rr[0] += 1
            if evac_rr[0] % 2 == 0:
                if scale is None:
                    op("act", lambda: nc.scalar.copy(out=out_ap, in_=in_ap), reads=reads, writes=writes)
                else:
                    op("act", lambda: nc.scalar.mul(out=out_ap, in_=in_ap, mul=scale), reads=reads, writes=writes)
            else:
                if scale is None:
                    op("dve", lambda: nc.vector.tensor_copy(out=out_ap, in_=in_ap), reads=reads, writes=writes)
                else:
                    op("dve", lambda: nc.vector.tensor_scalar(out=out_ap, in0=in_ap, scalar1=scale, scalar2=None,
                                                              op0=ALU.mult), reads=reads, writes=writes)

        def mm_group(bank_ap, bankb, pairs, reads):
            def fn():
                n = len(pairs)
                ins = None
                for i, (l, r) in enumerate(pairs):
                    ins = nc.tensor.matmul(bank_ap, l, r, start=(i == 0), stop=(i == n - 1))
                return ins
            op("pe", fn, reads=reads, writes=[bankb])

        with ExitStack() as pa:
            KT = sb(pa, "KT", [128, 4, S], BF16)
            V = sb(pa, "dV", [128, 64, 512], BF16)
            VsT = sb(pa, "VsT", [128, 4, 32], F32)
            VsTb = Buf()
            KTb = [[Buf() for _ in range(4)] for _ in range(16)]
            Vb = [Buf() for _ in range(64)]
            QTb = [[Buf() for _ in range(4)] for _ in range(8)]
            with ExitStack() as pa1:
                wqkv = sb(pa1, "wqkv", [128, 8, 1024], BF16)
                wvn = sb(pa1, "wvn", [128, 8, 512], BF16)
                xs = sb(pa1, "xs", [128, 8, 32], BF16)
                xt = [sb(pa1, "xt%d" % i, [128, 8, 512], BF16) for i in range(2)]
                xtsh = [sb(pa1, "xtsh%d" % i, [128, 8, 512], BF16) for i in range(2)]
                xtb = [Buf(), Buf()]
                xtshb = [Buf(), Buf()]
                wb = Buf()
                dma("pool", lambda: nc.gpsimd.dma_start(out=wqkv[:, :, :], in_=w_in_v[:, :, 512:1536]), writes=[wb])
                dma("pool", lambda: nc.gpsimd.dma_start(out=xs[:], in_=xTs_v), writes=[wb])
                op("dve", lambda: nc.vector.tensor_scalar(out=wvn[:, :, :], in0=wqkv[:, :, 512:1024], scalar1=-1.0, scalar2=None,
                                                          op0=ALU.mult), reads=[wb], writes=[wb])
                for j in range(4):
                    bk = next_bank()
                    mm_group(psf[:, bk, 0:32], bankbuf[bk],
                             [(wqkv[:, k, 512 + j * 128:512 + (j + 1) * 128], xs[:, k, :]) for k in range(8)], reads=[wb])
                    op("dve", lambda: nc.vector.tensor_copy(out=VsT[:, j, :], in_=psf[:, bk, 0:32]), reads=[bankbuf[bk]], writes=[VsTb])
                for T in range(16):
                    sl = T % 2
                    dma("pool", lambda: nc.gpsimd.dma_start(out=xt[sl][:], in_=xT_v[:, :, T * 512:(T + 1) * 512]),
                        writes=[xtb[sl]])
                    dma("pool", lambda: nc.gpsimd.dma_start(out=xtsh[sl][:], in_=xTsh_v[:, :, T * 512:(T + 1) * 512]),
                        writes=[xtshb[sl]])
                    for j in range(4):
                        bk = next_bank()
                        mm_group(psf[:, bk, :], bankbuf[bk],
                                 [(wqkv[:, k, j * 128:(j + 1) * 128], xt[sl][:, k, :]) for k in range(8)],
                                 reads=[wb, xtb[sl]])
                        evac(KT[:, j, T * 512:(T + 1) * 512], psf[:, bk, :], [bankbuf[bk]], [KTb[T][j]])
                    for s in range(4):
                        bk = next_bank()
                        mm_group(psf[:, bk, :], bankbuf[bk],
                                 [(xtsh[sl][:, k, s * 128:(s + 1) * 128], wqkv[:, k, 512:1024]) for k in range(8)] +
                                 [(xt[sl][:, k, s * 128:(s + 1) * 128], wvn[:, k, :]) for k in range(8)],
                                 reads=[wb, xtb[sl], xtshb[sl]])
                        evac(V[:, 4 * T + s, :], psf[:, bk, :], [bankbuf[bk]], [Vb[4 * T + s]])
                cx.barrier()
            QT = sb(pa, "QT", [128, 4, NOWN], BF16)
            with ExitStack() as paq:
                wq = sb(paq, "wq", [128, 8, 512], BF16)
                wvq = sb(paq, "wvq", [128, 8, 512], BF16)
                xt = [sb(paq, "xtq%d" % i, [128, 8, 512], BF16) for i in range(1)] * 2
                xtso = [sb(paq, "xtso%d" % i, [128, 8, 512], BF16) for i in range(1)] * 2
                vstg = [sb(paq, "vstg%d" % i, [128, 512], BF16) for i in range(2)]
                xtb = [Buf()] * 2
                xtsob = [Buf()] * 2
                vstgb = [Buf(), Buf()]
                wb = Buf()
                vsh_w = vsh_scr.rearrange("(j p) t -> p j t", p=128)
                dma("pool", lambda: nc.gpsimd.dma_start(out=wq[:, :, :], in_=w_in_v[:, :, 0:512]), writes=[wb])
                dma("pool", lambda: nc.gpsimd.dma_start(out=wvq[:, :, :], in_=w_in_v[:, :, 1024:1536]), writes=[wb])
                for T in range(8):
                    sl = T % 2
                    dma("pool", lambda: nc.gpsimd.dma_start(out=xt[sl][:], in_=xTo_v[:, :, T * 512:(T + 1) * 512]),
                        writes=[xtb[sl]])
                    for j in range(4):
                        bk = next_bank()
                        mm_group(psf[:, bk, :], bankbuf[bk],
                                 [(wq[:, k, j * 128:(j + 1) * 128], xt[sl][:, k, :]) for k in range(8)],
                                 reads=[wb, xtb[sl]])
                        evac(QT[:, j, T * 512:(T + 1) * 512], psf[:, bk, :], [bankbuf[bk]], [QTb[T][j]], scale=0.125)
                    dma("pool", lambda: nc.gpsimd.dma_start(out=xtso[sl][:], in_=xTsho_v[:, :, T * 512:(T + 1) * 512]),
                        writes=[xtsob[sl]])
                    for j in range(4):
                        bk = next_bank()
                        vs_ = (T * 4 + j) % 2
                        mm_group(psf[:, bk, :], bankbuf[bk],
                                 [(wvq[:, k, j * 128:(j + 1) * 128], xtso[sl][:, k, :]) for k in range(8)],
                                 reads=[wb, xtsob[sl]])
                        evac(vstg[vs_][:, :], psf[:, bk, :], [bankbuf[bk]], [vstgb[vs_]])
                        dma("sp", lambda: nc.sync.dma_start(out=vsh_w[:, j, T * 512:(T + 1) * 512], in_=vstg[vs_][:, :]),
                            reads=[vstgb[vs_]])
                cx.barrier()

            if stage == 0:
                return nc
            psf, psb = alloc_psum("T", 5, 3)
            with ExitStack() as pa2:
                NS = 4
                Sb = [sb(pa2, "Sb%d" % i, [128, 512], F32) for i in range(NS)]
                Wb = [sb(pa2, "Wb%d" % i, [128, 512], BF16) for i in range(NS)]
                WTs = [sb(pa2, "WTs%d" % i, [128, 512], BF16) for i in range(NS)]
                carry = sb(pa2, "carry", [128, 8], F32)
                Obf = [sb(pa2, "Obf%d" % i, [128, 512], BF16) for i in range(2)]
                Sbb = [Buf() for _ in range(NS)]
                Wbb = [Buf() for _ in range(NS)]
                WTsb = [Buf() for _ in range(NS)]
                carryb = [Buf() for _ in range(8)]
                Obfb = [Buf(), Buf()]
                NZ = 3
                Ob = [Buf(), Buf()]
                attn_v = attn_scr.rearrange("(j p) t -> p j t", p=128)

                items = []
                for i in range(NBLK):
                    chunks = []
                    s0 = 2 * i
                    while s0 < 64:
                        e0 = min(64, (s0 // 4 + 1) * 4)
                        chunks.append((s0, e0))
                        s0 = e0
                    for c, (s0, e0) in enumerate(chunks):
                        for h in range(8):
                            items.append((i, c, len(chunks), s0, e0, h))
                NIT = len(items)

                QTz = [sb(pa2, "QTz%d" % i_, [128, 8, 128], BF16) for i_ in range(2)]
                QTzb = [Buf(), Buf()]
                Osb = [sb(pa2, "Osb%d" % i_, [128, 512], BF16) for i_ in range(2)]
                Osbb = [Buf(), Buf()]
                for i_ in range(2):
                    op("dve", lambda: nc.vector.memset(QTz[i_][:], 0.0), writes=[QTzb[i_]])

                def st_qk(t):
                    i, c, nch, s0, e0, h = items[t]
                    if c == 0 and h == 0:
                        dma("sp", lambda: nc.sync.dma_start(out=vsh[i % 2][:], in_=vsh_r[:, :, i * 128:(i + 1) * 128]),
                            writes=[vshb[i % 2]])
                        qz = QTz[i % 2]
                        for jj in range(4):
                            op("act", lambda: nc.scalar.copy(out=qz[0:64, 2 * jj, :], in_=QT[0:64, jj, i * 128:(i + 1) * 128]),
                               reads=[QTb[i // 4][jj]], writes=[QTzb[i % 2]])
                            op("act", lambda: nc.scalar.copy(out=qz[64:128, 2 * jj + 1, :], in_=QT[64:128, jj, i * 128:(i + 1) * 128]),
                               reads=[QTb[i // 4][jj]], writes=[QTzb[i % 2]])
                    j, hb = h // 2, (h % 2) * 64
                    n = (e0 - s0) * 128
                    zb = t % NZ
                    rd = [QTzb[i % 2]] + [KTb[ss // 4][j] for ss in range(s0, e0, 4)] + [b_const]

                    def fn():
                        ins = nc.tensor.matmul(psf[:, zb, 0:n], QTz[i % 2][:, h, :],
                                               KT[:, j, s0 * 128:e0 * 128], start=True, stop=(c != 0))
                        if c == 0:
                            ins = nc.tensor.matmul(psf[:, zb, 0:256], ident_bf[:, :], mask_bf[:, :],
                                                   start=False, stop=True)
                        return ins
                    op("pe", fn, reads=rd, writes=[bankbuf[zb]])

                def st_sig(t):
                    i, c, nch, s0, e0, h = items[t]
                    n = (e0 - s0) * 128
                    zb = t % NZ
                    sl = t % NS
                    op("act", lambda: nc.scalar.activation(out=Sb[sl][:, 0:n], in_=psf[:, zb, 0:n], func=AF.Sigmoid,
                                                           scale=-1.0),
                       reads=[bankbuf[zb]], writes=[Sbb[sl]])

                if os.environ.get('ATT_C'):
                    Cb = [sb(pa2, "Cb%d" % i_, [128, 8], F32) for i_ in range(NS)]
                    Cbb = [Buf() for _ in range(NS)]

                if os.environ.get('ATT_E'):
                    Cf = [sb(pa2, "Cf%d" % i_, [128, 512], F32) for i_ in range(NS)]
                    Cfb = [Buf() for _ in range(NS)]

                def st_scan_old(t):
                    i, c, nch, s0, e0, h = items[t]
                    n = (e0 - s0) * 128
                    sl = t % NS
                    if c == 0:
                        op("pool", lambda: nc.gpsimd.memset(Cb[sl][:, 0:1], 1.0), writes=[Cbb[sl]])
                    else:
                        op("pool", lambda: nc.gpsimd.tensor_copy(out=Cb[sl][:, 0:1], in_=carry[:, h:h + 1]),
                           reads=[carryb[h]], writes=[Cbb[sl]])
                    op("dve", lambda: nc.vector.tensor_tensor_scan(out=Cb[sl][:, 1:n + 1], data0=Sb[sl][:, 0:n],
                                                                   data1=zeros[:, 0:n], initial=Cb[sl][:, 0:1],
                                                                   op0=ALU.mult, op1=ALU.add),
                       reads=[Sbb[sl], Cbb[sl]], writes=[Cbb[sl]])
                    if c != nch - 1:
                        op("pool", lambda: nc.gpsimd.tensor_copy(out=carry[:, h:h + 1], in_=Cb[sl][:, n:n + 1]),
                           reads=[Cbb[sl]], writes=[carryb[h]])
                    op("dve", lambda: nc.vector.tensor_tensor(out=Wb[sl][:, 0:n], in0=Cb[sl][:, 0:n],
                                                              in1=Cb[sl][:, 1:n + 1], op=ALU.subtract),
                       reads=[Cbb[sl]], writes=[Wbb[sl]])

                Cb = [sb(pa2, "Cb%d" % i_, [128, 8], F32) for i_ in range(NS)]
                Cbb = [Buf() for _ in range(NS)]
                vsh = [sb(pa2, "vsh%d" % i_, [128, 4, 128], BF16) for i_ in range(2)]
                vshb = [Buf(), Buf()]
                vsh_r = vsh_scr.rearrange("(j p) t -> p j t", p=128)

                def st_pre(t):
                    i, c, nch, s0, e0, h = items[t]
                    sl = t % NS
                    if c == 0:
                        op("pool", lambda: nc.gpsimd.memset(Cb[sl][:, 0:1], 1.0), writes=[Cbb[sl]])
                    else:
                        op("pool", lambda: nc.gpsimd.tensor_copy(out=Cb[sl][:, 0:1], in_=carry[:, h:h + 1]),
                           reads=[carryb[h]], writes=[Cbb[sl]])

                def st_scan_f(t):
                    i, c, nch, s0, e0, h = items[t]
                    n = (e0 - s0) * 128
                    sl = t % NS
                    op("dve", lambda: nc.vector.tensor_tensor_scan(out=Wb[sl][:, 0:n], data0=Sb[sl][:, 0:n],
                                                                   data1=zeros[:, 0:n], initial=Cb[sl][:, 0:1],
                                                                   op0=ALU.mult, op1=ALU.add),
                       reads=[Sbb[sl], Cbb[sl]], writes=[Wbb[sl]])
                    if c != nch - 1:
                        op("pool", lambda: nc.gpsimd.tensor_copy(out=carry[:, h:h + 1], in_=Wb[sl][:, n - 1:n]),
                           reads=[Wbb[sl]], writes=[carryb[h]])
                    if c == 0:
                        op("pool", lambda: nc.gpsimd.tensor_tensor(out=Wb[sl][:, 0:256], in0=Wb[sl][:, 0:256],
                                                                   in1=mask01[:, :], op=ALU.mult),
                           reads=[Wbb[sl], b_const], writes=[Wbb[sl]])

                def st_scan(t):
                    if True:
                        return st_scan_f(t)
                    if os.environ.get('ATT_C'):
                        return st_scan_old(t)
                    i, c, nch, s0, e0, h = items[t]
                    n = (e0 - s0) * 128
                    sl = t % NS
                    init = ones_f[:, 0:1] if c == 0 else carry[:, h:h + 1]
                    rd = [Sbb[sl]] + ([] if c == 0 else [carryb[h]])
                    if os.environ.get('ATT_E'):
                        op("dve", lambda: nc.vector.tensor_tensor_scan(out=Cf[sl][:, 0:n], data0=Sb[sl][:, 0:n],
                                                                       data1=zeros[:, 0:n], initial=init,
                                                                       op0=ALU.mult, op1=ALU.add),
                           reads=rd, writes=[Cfb[sl]])
                        op("act", lambda: nc.scalar.copy(out=Wb[sl][:, 0:n], in_=Cf[sl][:, 0:n]), reads=[Cfb[sl]], writes=[Wbb[sl]])
                    else:
                      op("dve", lambda: nc.vector.tensor_tensor_scan(out=Wb[sl][:, 0:n], data0=Sb[sl][:, 0:n],
                                                                   data1=zeros[:, 0:n], initial=init,
                                                                   op0=ALU.mult, op1=ALU.add),
                       reads=rd, writes=[Wbb[sl]])
                    if c != nch - 1:
                        ce = "dve" if os.environ.get('ATT_D3') else "pool"
                        cpe = nc.vector if os.environ.get('ATT_D3') else nc.gpsimd
                        op(ce, lambda: cpe.tensor_copy(out=carry[:, h:h + 1], in_=Wb[sl][:, n - 1:n]),
                           reads=[Wbb[sl]], writes=[carryb[h]])

                def st_tr(t):
                    i, c, nch, s0, e0, h = items[t]
                    nsb = e0 - s0
                    n = nsb * 128
                    sl = t % NS
                    hf = t % 2

                    def fn():
                        ins = None
                        for q in range(nsb):
                            ins = nc.tensor.transpose(psb[:, hf, q * 128:(q + 1) * 128],
                                                      Wb[sl][:, q * 128:(q + 1) * 128], ident_bf[:, :])
                        return ins
                    op("pe", fn, reads=[Wbb[sl], b_const], writes=[pbbuf[hf]])
                    if False:
                        op("dve", lambda: nc.vector.tensor_copy(out=WTs[sl][:, 0:n], in_=psb[:, hf, 0:n]),
                           reads=[pbbuf[hf]], writes=[WTsb[sl]])
                    else:
                        op("act", lambda: nc.scalar.copy(out=WTs[sl][:, 0:n], in_=psb[:, hf, 0:n]),
                           reads=[pbbuf[hf]], writes=[WTsb[sl]])

                def st_av(t):
                    i, c, nch, s0, e0, h = items[t]
                    nsb = e0 - s0
                    sl = t % NS
                    ob = i % 2

                    def fn():
                        ins = None
                        if c == 0 and h == 0:
                            nc.tensor.matmul(psf[:, 3 + ob, :], zeros_bf[:, 0:128], zeros_bf[:, :], start=True, stop=False,
                                             skip_group_check=True)
                        for q in range(nsb):
                            ins = nc.tensor.matmul(psf[:, 3 + ob, h * 64:(h + 1) * 64],
                                                   WTs[sl][:, q * 128:(q + 1) * 128],
                                                   V[:, s0 + q, h * 64:(h + 1) * 64],
                                                   start=False, stop=(c == nch - 1 and h == 7 and q == nsb - 1),
                                                   skip_group_check=True)
                        return ins
                    op("pe", fn, reads=[WTsb[sl]] + [Vb[ss] for ss in range(s0, e0)], writes=[Ob[ob]])
                    if c == nch - 1 and h == 7:
                        op("act", lambda: nc.scalar.copy(out=Osb[ob][:, :], in_=psf[:, 3 + ob, :]), reads=[Ob[ob]], writes=[Osbb[ob]])

                        def fnt():
                            ins = None
                            for jj in range(4):
                                ins = nc.tensor.transpose(psb[:, 2, jj * 128:(jj + 1) * 128], Osb[ob][:, jj * 128:(jj + 1) * 128],
                                                          ident_bf[:, :])
                            return ins
                        op("pe", fnt, reads=[Osbb[ob], b_const], writes=[pbbuf[2]])
                        op("dve", lambda: nc.vector.tensor_tensor(
                            out=Obf[ob][:, :].rearrange("p (j t) -> p j t", j=4),
                            in0=psb[:, 2, 0:512].rearrange("p (j t) -> p j t", j=4),
                            in1=vsh[ob][:, :, :], op=ALU.add),
                           reads=[pbbuf[2], vshb[ob]], writes=[Obfb[ob]])
                        dma("sp", lambda: nc.sync.dma_start(out=attn_v[:, :, i * 128:(i + 1) * 128],
                                                            in_=Obf[ob][:, :].rearrange("p (j t) -> p j t", j=4)),
                            reads=[Obfb[ob]])

                L1, L2, L3 = 2, 4, 5
                for t in range(NIT + L3 + 1):
                    if t < NIT:
                        st_qk(t)
                        st_sig(t)
                        st_pre(t)
                    if 0 <= t - L1 < NIT:
                        st_scan(t - L1)
                    if 0 <= t - L2 < NIT:
                        st_tr(t - L2)
                    if 0 <= t - L3 < NIT:
                        st_av(t - L3)
                cx.barrier()
        if stage == 1:
            cx.barrier()
            return nc
        psf, psb = alloc_psum("B", 6, 2)

        with ExitStack() as pb:
            wc = sb(pb, "wc", [128, 8, 1536], BF16)
            wg = sb(pb, "wg", [128, 8, 2048], BF16)
            wba = sb(pb, "wba", [128, 4, D], BF16)
            wbb = sb(pb, "wbb", [128, 4, D], BF16)
            wo = sb(pb, "wo", [128, 8, D], BF16)
            wr = sb(pb, "wr", [128, 8, 36], F32)
            gb_sb = sb(pb, "gb_sb", [128, 16], F32)
            cw_sb = sb(pb, "cw_sb", [128, 12], F32)
            g1 = sb(pb, "g1", [128, D], F32)
            b1 = sb(pb, "b1", [128, D], F32)
            br = sb(pb, "br", [128, 36], F32)
            base = sb(pb, "base", [128, 32], F32)
            wB = Buf()
            for c0 in range(0, 1536, 512):
                dma("pool", lambda: nc.gpsimd.dma_start(out=wc[:, :, c0:c0 + 512], in_=w_in_v[:, :, 1536 + c0:1536 + c0 + 512]), writes=[wB])
            for c0 in range(0, 2048, 512):
                dma("pool", lambda: nc.gpsimd.dma_start(out=wg[:, :, c0:c0 + 512], in_=w_in_v[:, :, 3072 + c0:3072 + c0 + 512]), writes=[wB])
            dma("pool", lambda: nc.gpsimd.dma_start(out=wba[:], in_=w_ba.rearrange("(k p) n -> p k n", p=128)), writes=[wB])
            dma("pool", lambda: nc.gpsimd.dma_start(out=wbb[:], in_=w_bb.rearrange("(k p) n -> p k n", p=128)), writes=[wB])
            dma("pool", lambda: nc.gpsimd.dma_start(out=wo[:], in_=w_out.rearrange("(k p) n -> p k n", p=128)), writes=[wB])
            dma("sp", lambda: nc.sync.dma_start(out=wr[:], in_=w_r.rearrange("(k p) n -> p k n", p=128)), writes=[wB])
            dma("sp", lambda: nc.sync.dma_start(out=gb_sb[:], in_=gbias), writes=[wB])
            dma("sp", lambda: nc.sync.dma_start(out=cw_sb[:], in_=cw), writes=[wB])
            dma("sp", lambda: nc.sync.dma_start(out=g1[:], in_=lnp[0].partition_broadcast(128)), writes=[wB])
            dma("sp", lambda: nc.sync.dma_start(out=b1[:], in_=lnp[1].partition_broadcast(128)), writes=[wB])
            dma("sp", lambda: nc.sync.dma_start(out=br[:], in_=b_r.partition_broadcast(128)), writes=[wB])
            op("pool", lambda: nc.gpsimd.memset(base[:], 0.0), writes=[wB])
            cx.barrier()

            xt = [sb(pb, "xtB%d" % i, [128, 8, 512], BF16) for i in range(2)]
            xh = [sb(pb, "xh%d" % i, [128, 8, 8], BF16) for i in range(2)]
            at = [sb(pb, "at%d" % i, [128, 4, 512], BF16) for i in range(2)]
            xtok = [sb(pb, "xtok%d" % i, [128, 4, D], F32) for i in range(1)]
            inb = [Buf(), Buf()]
            xtokb = Buf()
            dummy = sb(pb, "dummyB", [128, 1], F32)
            ccs = sb(pb, "ccs", [128, 512], F32)
            cchs = sb(pb, "cchs", [128, 8], F32)
            U = sb(pb, "U", [128, 4, 130], F32)
            Y = sb(pb, "Y", [128, 4, 128], F32)
            Bin = sb(pb, "Bin", [128, 4, 512], BF16)
            sa = [sb(pb, "sa%d" % i, [128, 512], F32) for i in range(1)] * 2
            sbg = [sb(pb, "sbg%d" % i, [128, 512], F32) for i in range(1)] * 2
            t1 = [sb(pb, "t1%d" % i, [128, 512], F32) for i in range(1)] * 2
            t2 = [sb(pb, "t2%d" % i, [128, 512], F32) for i in range(1)] * 2
            GT = sb(pb, "GT", [128, 8, 512], BF16)
            hbuf = [sb(pb, "hbuf%d" % i, [128, D], F32) for i in range(4)]
            h1b = [sb(pb, "h1b%d" % i, [128, D], BF16) for i in range(4)]
            h1T = [sb(pb, "h1T%d" % i, [128, 8, 128], F32) for i in range(2)]
            stats = [sb(pb, "stB%d" % i, [128, 2, 6], F32) for i in range(4)]
            mv = [sb(pb, "mvB%d" % i, [128, 2], F32) for i in range(4)]
            rstd = [sb(pb, "rstdB%d" % i, [128, 1], F32) for i in range(4)]
            Lg = sb(pb, "Lg", [128, 4, 36], F32)
            ccsb, Ub, Yb, Binb, GTb = Buf(), Buf(), Buf(), Buf(), Buf()
            sab = [Buf()] * 2
            t1b = [Buf()] * 2
            hb_ = [Buf() for _ in range(4)]
            h1bb = [Buf() for _ in range(4)]
            h1Tb, stb, Lgb = [Buf(), Buf()], [Buf() for _ in range(4)], Buf()
            R = {}
            for nm, shp in (("gmax", [128, 4]), ("ohg", [128, 4, 4]), ("eg", [128, 4, 4]), ("sumg", [128, 4]),
                            ("gp", [128, 4]), ("prod", [128, 4, 4, 8]), ("sel", [128, 4, 8]), ("m1", [128, 4]),
                            ("oh1", [128, 4, 8]), ("sel2", [128, 4, 8]), ("m2", [128, 4]), ("oh2", [128, 4, 8]),
                            ("dm", [128, 4]), ("w1", [128, 4]), ("w2", [128, 4]), ("ind8", [128, 4, 8]),
                            ("OH1", [128, 4, 32]), ("OH2", [128, 4, 32]), ("Rk", [128, 4, 32]),
                            ("ov", [128, 4, 32]), ("d1f", [128, 4]), ("d2f", [128, 4])):
                R[nm] = sb(pb, "r_" + nm, shp, F32)
            Ind = sb(pb, "Ind", [128, 4, 32], BF16)
            rb = Buf()
            xT_B = xTo_v
            attn_v = attn_scr.rearrange("(j p) t -> p j t", p=128)
            xo_v = xo.rearrange("(s p) d -> p s d", p=128)
            h1_v = h1_scr.rearrange("(s p) d -> p s d", p=128)

            def load_tile(T_):
                sl_ = T_ % 2
                dma("pool", lambda: nc.gpsimd.dma_start(out=xt[sl_][:], in_=xT_B[:, :, T_ * 512:(T_ + 1) * 512]), writes=[inb[sl_]])
                dma("pool", lambda: nc.gpsimd.dma_start(out=xh[sl_][:], in_=xTh_v[:, :, T_ * 8:(T_ + 1) * 8]), writes=[inb[sl_]])
                dma("sp", lambda: nc.sync.dma_start(out=at[sl_][:], in_=attn_v[:, :, T_ * 512:(T_ + 1) * 512]), writes=[inb[sl_]])

            pending_route = []

            def flush_route():
                while pending_route:
                    Tr = pending_route.pop(0)
                    routing(nc, op, dma, R, Ind, Lg, Lgb, rb, ustr_bf, ones_bf, b_const, psf, bankbuf, next_bank,
                            base, ebase, D1, D2, RW1, RW2, Tr, wB)
                    for s_ in range(4):
                        gs_ = 4 * Tr + s_
                        for Dk in (D1, D2):
                            dma("pool", lambda: nc.gpsimd.indirect_dma_start(
                                out=xg_scr, out_offset=bass.IndirectOffsetOnAxis(ap=Dk[:, gs_:gs_ + 1], axis=0),
                                in_=h1b[s_][:, :], in_offset=None, bounds_check=bc_reg, oob_is_err=False),
                                reads=[h1bb[s_], rb])

            load_tile(0)
            dma("sp", lambda: nc.sync.dma_start(out=xtok[0][:], in_=xo_v[:, 0:4, :]), writes=[xtokb])
            for T in range(8):
                sl = T % 2
                if T + 1 < 8:
                    load_tile(T + 1)
                for m in range(4):
                    bcc, bch, bcb, bh = next_bank(), next_bank(), next_bank(), next_bank()
                    mm_group(psf[:, bcc, :], bankbuf[bcc],
                             [(wc[:, k, 512 + m * 128:512 + (m + 1) * 128], xt[sl][:, k, :]) for k in range(8)], [wB, inb[sl]])
                    mm_group(psf[:, bch, :], bankbuf[bch],
                             [(wc[:, k, 1024 + m * 128:1024 + (m + 1) * 128], xt[sl][:, k, :]) for k in range(8)], [wB, inb[sl]])
                    mm_group(psf[:, bcb, :], bankbuf[bcb],
                             [(wc[:, k, m * 128:(m + 1) * 128], xt[sl][:, k, :]) for k in range(8)], [wB, inb[sl]])

                    def fnh():
                        ins = None
                        for k in range(8):
                            ins = nc.tensor.matmul(psf[:, bh, 0:8], wc[:, k, 512 + m * 128:512 + (m + 1) * 128],
                                                   xh[sl][:, k, :], start=(k == 0), stop=(k == 7))
                        for k in range(8):
                            ins = nc.tensor.matmul(psf[:, bh, 8:16], wc[:, k, 1024 + m * 128:1024 + (m + 1) * 128],
                                                   xh[sl][:, k, :], start=(k == 0), stop=(k == 7))
                        return ins
                    op("pe", fnh, reads=[wB, inb[sl]], writes=[bankbuf[bh]])
                    op("act", lambda: nc.scalar.copy(out=ccs[:, :], in_=psf[:, bcc, :]), reads=[bankbuf[bcc]], writes=[ccsb])
                    op("act", lambda: nc.scalar.copy(out=cchs[:, :], in_=psf[:, bh, 0:8]), reads=[bankbuf[bh]], writes=[ccsb])
                    op("dve", lambda: nc.vector.tensor_tensor(out=U[:, :, 0:128],
                                                              in0=ccs[:, :].rearrange("p (b t) -> p b t", b=4),
                                                              in1=psf[:, bch, :].rearrange("p (b t) -> p b t", b=4),
                                                              op=ALU.mult), reads=[ccsb, bankbuf[bch]], writes=[Ub])
                    op("dve", lambda: nc.vector.tensor_tensor(out=U[:, :, 128:130],
                                                              in0=cchs[:, :].rearrange("p (b t) -> p b t", b=4),
                                                              in1=psf[:, bh, 8:16].rearrange("p (b t) -> p b t", b=4),
                                                              op=ALU.mult), reads=[ccsb, bankbuf[bh]], writes=[Ub])
                    op("dve", lambda: nc.vector.tensor_scalar(out=Y[:, :, :], in0=U[:, :, 0:128],
                                                              scalar1=cw_sb[:, m * 3 + 2:m * 3 + 3], scalar2=None,
                                                              op0=ALU.mult), reads=[Ub, wB], writes=[Yb])
                    op("dve", lambda: nc.vector.scalar_tensor_tensor(out=Y[:, :, :], in0=U[:, :, 1:129],
                                                                     scalar=cw_sb[:, m * 3 + 1:m * 3 + 2], in1=Y[:, :, :],
                                                                     op0=ALU.mult, op1=ALU.add), reads=[Ub, Yb], writes=[Yb])
                    op("dve", lambda: nc.vector.scalar_tensor_tensor(out=Y[:, :, :], in0=U[:, :, 2:130],
                                                                     scalar=cw_sb[:, m * 3:m * 3 + 1], in1=Y[:, :, :],
                                                                     op0=ALU.mult, op1=ALU.add), reads=[Ub, Yb], writes=[Yb])
                    op("dve", lambda: nc.vector.tensor_tensor(out=Bin[:, m, :], in0=Y[:, :, :].rearrange("p b t -> p (b t)"),
                                                              in1=psf[:, bcb, :], op=ALU.mult),
                       reads=[Yb, bankbuf[bcb]], writes=[Binb])
                flush_route()
                for m in range(8):
                    s2 = m % 2
                    bA, bB, bga, bgb = next_bank(), next_bank(), next_bank(), next_bank()
                    mm_group(psf[:, bA, :], bankbuf[bA],
                             [(wba[:, k, m * 128:(m + 1) * 128], at[sl][:, k, :]) for k in range(4)], [wB, inb[sl]])
                    mm_group(psf[:, bB, :], bankbuf[bB],
                             [(wbb[:, k, m * 128:(m + 1) * 128], Bin[:, k, :]) for k in range(4)], [wB, Binb])
                    mm_group(psf[:, bga, :], bankbuf[bga],
                             [(wg[:, k, m * 128:(m + 1) * 128], xt[sl][:, k, :]) for k in range(8)], [wB, inb[sl]])
                    mm_group(psf[:, bgb, :], bankbuf[bgb],
                             [(wg[:, k, 1024 + m * 128:1024 + (m + 1) * 128], xt[sl][:, k, :]) for k in range(8)], [wB, inb[sl]])
                    op("act", lambda: nc.scalar.activation(out=sa[s2][:, :], in_=psf[:, bga, :], func=AF.Sigmoid,
                                                           bias=gb_sb[:, m:m + 1]), reads=[bankbuf[bga], wB], writes=[sab[s2]])
                    op("act", lambda: nc.scalar.activation(out=sbg[s2][:, :], in_=psf[:, bgb, :], func=AF.Sigmoid,
                                                           bias=gb_sb[:, 8 + m:9 + m]), reads=[bankbuf[bgb], wB], writes=[sab[s2]])
                    op("dve", lambda: nc.vector.tensor_tensor(out=t1[s2][:, :], in0=sa[s2][:, :], in1=psf[:, bA, :], op=ALU.mult),
                       reads=[sab[s2], bankbuf[bA]], writes=[t1b[s2]])
                    op("dve", lambda: nc.vector.tensor_tensor(out=t2[s2][:, :], in0=sbg[s2][:, :], in1=psf[:, bB, :], op=ALU.mult),
                       reads=[sab[s2], bankbuf[bB]], writes=[t1b[s2]])
                    op("pool", lambda: nc.gpsimd.tensor_tensor(out=GT[:, m, :], in0=t1[s2][:, :], in1=t2[s2][:, :], op=ALU.add),
                       reads=[t1b[s2]], writes=[GTb])
                for s in range(4):
                    s2 = s
                    bk0, bk1 = next_bank(), next_bank()
                    for half, bk in ((0, bk0), (1, bk1)):
                        mm_group(psf[:, bk, :], bankbuf[bk],
                                 [(GT[:, k, s * 128:(s + 1) * 128], wo[:, k, half * 512:(half + 1) * 512]) for k in range(8)],
                                 [wB, GTb])
                        op("dve", lambda: nc.vector.scalar_tensor_tensor(out=hbuf[s2][:, half * 512:(half + 1) * 512],
                                                                         in0=xtok[0][:, s, half * 512:(half + 1) * 512],
                                                                         scalar=ALPHA, in1=psf[:, bk, :],
                                                                         op0=ALU.mult, op1=ALU.add),
                           reads=[xtokb, bankbuf[bk]], writes=[hb_[s2]])
                if T + 1 < 8:
                    dma("sp", lambda: nc.sync.dma_start(out=xtok[0][:], in_=xo_v[:, 4 * (T + 1):4 * (T + 1) + 4, :]), writes=[xtokb])
                for s in range(4):
                    for half in range(2):
                        op("dve", lambda: nc.vector.bn_stats(out=stats[s][:, half, :], in_=hbuf[s][:, half * 512:(half + 1) * 512]),
                           reads=[hb_[s]], writes=[stb[s]])
                    op("dve", lambda: nc.vector.bn_aggr(out=mv[s][:, :], in_=stats[s][:, :, :].rearrange("p a b -> p (a b)")),
                       reads=[stb[s]], writes=[stb[s]])
                for s in range(4):
                    op("act", lambda: nc.scalar.activation(out=rstd[s][:, :], in_=mv[s][:, 1:2], func=AF.Sqrt, bias=epsT[:, 0:1]),
                       reads=[stb[s], wB], writes=[stb[s]])
                for s in range(4):
                    op("dve", lambda: nc.vector.reciprocal(out=rstd[s][:, :], in_=rstd[s][:, :]), reads=[stb[s]], writes=[stb[s]])
                    op("dve", lambda: nc.vector.tensor_scalar(out=hbuf[s][:, :], in0=hbuf[s][:, :], scalar1=mv[s][:, 0:1],
                                                              scalar2=rstd[s][:, 0:1], op0=ALU.subtract, op1=ALU.mult),
                       reads=[stb[s], hb_[s]], writes=[hb_[s]])
                    op("dve", lambda: nc.vector.tensor_tensor(out=hbuf[s][:, :], in0=hbuf[s][:, :], in1=g1[:, :], op=ALU.mult),
                       reads=[hb_[s], wB], writes=[hb_[s]])
                    op("pool", lambda: nc.gpsimd.tensor_tensor(out=hbuf[s][:, :], in0=hbuf[s][:, :], in1=b1[:, :], op=ALU.add),
                       reads=[hb_[s], wB], writes=[hb_[s]])
                for s in range(4):
                    gs = 4 * T + s
                    dma("sp", lambda: nc.sync.dma_start(out=h1_v[:, gs, :], in_=hbuf[s][:, :]), reads=[hb_[s]])
                    op("act", lambda: nc.scalar.copy(out=h1b[s][:, :], in_=hbuf[s][:, :]), reads=[hb_[s]], writes=[h1bb[s]])
                for s in range(4):
                    s2 = s
                    hT_ = h1T[s % 2]
                    hTb_ = h1Tb[s % 2]
                    tb0, tb1 = next_bank(), next_bank()

                    def fntr():
                        ins = None
                        for k in range(8):
                            tb = tb0 if k < 4 else tb1
                            ins = nc.tensor.transpose(psf[:, tb, (k % 4) * 128:(k % 4 + 1) * 128],
                                                      hbuf[s2][:, k * 128:(k + 1) * 128], ident_f[:, :])
                        return ins
                    op("pe", fntr, reads=[hb_[s2], b_const], writes=[bankbuf[tb0], bankbuf[tb1]])
                    op("act", lambda: nc.scalar.copy(out=hT_[:, 0:4, :], in_=psf[:, tb0, :].rearrange("p (k t) -> p k t", k=4)),
                       reads=[bankbuf[tb0]], writes=[hTb_])
                    op("dve", lambda: nc.vector.tensor_copy(out=hT_[:, 4:8, :], in_=psf[:, tb1, :].rearrange("p (k t) -> p k t", k=4)),
                       reads=[bankbuf[tb1]], writes=[hTb_])
                    lb = next_bank()
                    mm_group(psf[:, lb, 0:36], bankbuf[lb],
                             [(hT_[:, k, :], wr[:, k, :]) for k in range(8)], [hTb_, wB])
                    op("dve", lambda: nc.vector.tensor_tensor(out=Lg[:, s, :], in0=psf[:, lb, 0:36], in1=br[:, :], op=ALU.add),
                       reads=[bankbuf[lb], wB], writes=[Lgb])
                pending_route.append(T)
            flush_route()
            cx.barrier()
            if stage == 2:
                op("dve", lambda: nc.vector.tensor_copy(out=zeros[:, 0:32], in_=D1[:, :]), writes=[b_const])
                op("dve", lambda: nc.vector.tensor_copy(out=zeros[:, 32:64], in_=D2[:, :]), writes=[b_const])
                op("dve", lambda: nc.vector.tensor_copy(out=zeros[:, 64:96], in_=RW1[:, :]), writes=[b_const])
                op("dve", lambda: nc.vector.tensor_copy(out=zeros[:, 96:128], in_=RW2[:, :]), writes=[b_const])
                dma("sp", lambda: nc.sync.dma_start(out=dbg_r, in_=zeros[:, 0:128]), reads=[b_const])
                cx.barrier()
                return nc

        with ExitStack() as pc:
            NW = 3
            wgs = [sb(pc, "wgs%d" % i, [128, 8, 512], BF16) for i in range(NW)]
            wus = [sb(pc, "wus%d" % i, [128, 8, 512], BF16) for i in range(NW)]
            wds = [sb(pc, "wds%d" % i, [128, 4, D], BF16) for i in range(NW)]
            wEb = [[Buf(), Buf(), Buf()] for _ in range(NW)]
            xgs = [sb(pc, "xgs%d" % i, [128, 3, D], BF16) for i in range(NW)]
            xgb = [Buf() for _ in range(NW)]
            xbT = [sb(pc, "xbT%d" % i, [128, 8, CAP], BF16) for i in range(2)]
            xbTb = [Buf(), Buf()]
            sg = [sb(pc, "sg%d" % i, [128, CAP], F32) for i in range(2)]
            sgb = [Buf(), Buf()]
            hT = [sb(pc, "hT%d" % i, [128, 4, CAP], BF16) for i in range(2)]
            hTb = [Buf(), Buf()]
            ysb = [sb(pc, "ysb%d" % i, [128, D], F32) for i in range(2)]
            ysbb = [Buf(), Buf()]
            xg_v = xg_scr.rearrange("(e j p) d -> e p j d", p=128, j=3)
            ys_v = ys_scr.rearrange("(e j p) d -> e p j d", p=128, j=3)

            def load_w(e):
                sl = e % NW
                dma("pool", lambda: nc.gpsimd.dma_start(out=wgs[sl][:], in_=w_gate[e].rearrange("(k p) f -> p k f", p=128)), writes=[wEb[sl][0]])
                dma("pool", lambda: nc.gpsimd.dma_start(out=wus[sl][:], in_=w_up[e].rearrange("(k p) f -> p k f", p=128)), writes=[wEb[sl][1]])
                dma("pool", lambda: nc.gpsimd.dma_start(out=wds[sl][:], in_=w_down[e].rearrange("(k p) f -> p k f", p=128)), writes=[wEb[sl][2]])
                dma("sp", lambda: nc.sync.dma_start(out=xgs[sl][:], in_=xg_v[e]), writes=[xgb[sl]])

            def do_transposes(e):
                sl = e % NW
                xb = xbT[e % 2]
                for j in range(3):
                    def fnt():
                        ins = None
                        for k in range(8):
                            ins = nc.tensor.transpose(psb[:, j % 2, k * 128:(k + 1) * 128], xgs[sl][:, j, k * 128:(k + 1) * 128],
                                                      ident_bf[:, :])
                        return ins
                    op("pe", fnt, reads=[xgb[sl], b_const], writes=[pbbuf[j % 2]])
                    evac(xb[:, :, j * 128:(j + 1) * 128], psb[:, j % 2, :].rearrange("p (k t) -> p k t", k=8),
                         [pbbuf[j % 2]], [xbTb[e % 2]])

            def do_gate_up(e):
                sl = e % NW
                xb = xbT[e % 2]
                for f in range(4):
                    s2 = f % 2
                    bg, bu = next_bank(), next_bank()
                    mm_group(psf[:, bg, 0:CAP], bankbuf[bg],
                             [(wgs[sl][:, k, f * 128:(f + 1) * 128], xb[:, k, :]) for k in range(8)], wEb[sl] + [xbTb[e % 2]])
                    mm_group(psf[:, bu, 0:CAP], bankbuf[bu],
                             [(wus[sl][:, k, f * 128:(f + 1) * 128], xb[:, k, :]) for k in range(8)], wEb[sl] + [xbTb[e % 2]])
                    op("act", lambda: nc.scalar.activation(out=sg[s2][:, :], in_=psf[:, bg, 0:CAP], func=AF.Silu),
                       reads=[bankbuf[bg]], writes=[sgb[s2]])
                    op("dve", lambda: nc.vector.tensor_tensor(out=hT[e % 2][:, f, :], in0=sg[s2][:, :], in1=psf[:, bu, 0:CAP], op=ALU.mult),
                       reads=[sgb[s2], bankbuf[bu]], writes=[hTb[e % 2]])

            def do_down(e):
                sl = e % NW
                for j in range(3):
                    s2 = j % 2
                    for half in range(2):
                        bk = next_bank()
                        mm_group(psf[:, bk, :], bankbuf[bk],
                                 [(hT[e % 2][:, f, j * 128:(j + 1) * 128], wds[sl][:, f, half * 512:(half + 1) * 512]) for f in range(4)],
                                 wEb[sl] + [hTb[e % 2]])
                        op("act" if half == 0 else "dve",
                           (lambda: nc.scalar.copy(out=ysb[s2][:, 0:512], in_=psf[:, bk, :])) if half == 0 else
                           (lambda: nc.vector.tensor_copy(out=ysb[s2][:, 512:1024], in_=psf[:, bk, :])),
                           reads=[bankbuf[bk]], writes=[ysbb[s2]])
                    dma("sp", lambda: nc.sync.dma_start(out=ys_v[e][:, j, :], in_=ysb[s2][:, :]), reads=[ysbb[s2]])

            load_w(0)
            load_w(1)
            do_transposes(0)
            for e in range(32):
                if e + 2 < 32:
                    load_w(e + 2)
                do_gate_up(e)
                if e + 1 < 32:
                    do_transposes(e + 1)
                do_down(e)
            cx.barrier()

        if stage == 3:
            return nc
        with ExitStack() as pd:
            g2 = sb(pd, "g2", [128, D], F32)
            b2 = sb(pd, "b2", [128, D], F32)
            wD = Buf()
            dma("sp", lambda: nc.sync.dma_start(out=g2[:], in_=lnp[2].partition_broadcast(128)), writes=[wD])
            dma("sp", lambda: nc.sync.dma_start(out=b2[:], in_=lnp[3].partition_broadcast(128)), writes=[wD])
            cx.barrier()
            NR = 4
            r1 = [sb(pd, "r1%d" % i, [128, D], F32) for i in range(NR)]
            r2 = [sb(pd, "r2%d" % i, [128, D], F32) for i in range(NR)]
            hh = [sb(pd, "hh%d" % i, [128, D], F32) for i in range(NR)]
            stats = sb(pd, "stats2", [128, 2, 6], F32)
            mv = sb(pd, "mv2", [128, 2], F32)
            rstd = sb(pd, "rstd2", [128, 1], F32)
            stb = Buf()
            r1b, r2b, hhb = [Buf() for _ in range(NR)], [Buf() for _ in range(NR)], [Buf() for _ in range(NR)]
            h1_v = h1_scr.rearrange("(s p) d -> p s d", p=128)
            out_v = out.rearrange("(s p) d -> p s d", p=128)

            def loads_d(gs):
                s2 = gs % NR
                for rr, rrb in ((r1, r1b), (r2, r2b)):
                    for half in range(2):
                        op("act", lambda: nc.scalar.copy(out=rr[s2][:, half * 512:(half + 1) * 512], in_=zeros[:, :]),
                           reads=[b_const], writes=[rrb[s2]])
                dma("pool", lambda: nc.gpsimd.indirect_dma_start(
                    out=r1[s2][:, :], out_offset=None, in_=ys_scr,
                    in_offset=bass.IndirectOffsetOnAxis(ap=D1[:, gs:gs + 1], axis=0),
                    bounds_check=bc_reg, oob_is_err=False), writes=[r1b[s2]])
                dma("pool", lambda: nc.gpsimd.indirect_dma_start(
                    out=r2[s2][:, :], out_offset=None, in_=ys_scr,
                    in_offset=bass.IndirectOffsetOnAxis(ap=D2[:, gs:gs + 1], axis=0),
                    bounds_check=bc_reg, oob_is_err=False), writes=[r2b[s2]])
                dma("sp", lambda: nc.sync.dma_start(out=hh[s2][:, :], in_=h1_v[:, gs, :]), writes=[hhb[s2]])

            epsT2 = sb(pd, "epsT2", [128, 1], F32)
            rwb = Buf()
            op("pool", lambda: nc.gpsimd.memset(epsT2[:], EPS / (ALPHA * ALPHA)), writes=[rwb])
            op("dve", lambda: nc.vector.tensor_scalar(out=RW1[:, :], in0=RW1[:, :], scalar1=1.0 / ALPHA, scalar2=None, op0=ALU.mult),
               writes=[rwb])
            op("dve", lambda: nc.vector.tensor_scalar(out=RW2[:, :], in0=RW2[:, :], scalar1=1.0 / ALPHA, scalar2=None, op0=ALU.mult),
               writes=[rwb])
            cx.barrier()
            loads_d(0)
            loads_d(1)
            for gs in range(32):
                s2 = gs % NR
                if gs + 2 < 32:
                    loads_d(gs + 2)
                op("dve", lambda: nc.vector.scalar_tensor_tensor(out=hh[s2][:, :], in0=r1[s2][:, :], scalar=RW1[:, gs:gs + 1],
                                                                 in1=hh[s2][:, :], op0=ALU.mult, op1=ALU.add),
                   reads=[r1b[s2], hhb[s2]], writes=[hhb[s2]])
                op("dve", lambda: nc.vector.scalar_tensor_tensor(out=hh[s2][:, :], in0=r2[s2][:, :], scalar=RW2[:, gs:gs + 1],
                                                                 in1=hh[s2][:, :], op0=ALU.mult, op1=ALU.add),
                   reads=[r2b[s2], hhb[s2]], writes=[hhb[s2]])
                layer_norm(nc, op, hh[s2], hhb[s2], stats, mv, rstd, stb, g2, b2, wD, epsT2)
                dma("sp", lambda: nc.sync.dma_start(out=out_v[:, gs, :], in_=hh[s2][:, :]), reads=[hhb[s2]])
            cx.barrier()
    return nc


def fence(cx, op, nc, rstd_like=None):
    cx.barrier()


def layer_norm(nc, op, h, hb, stats, mv, rstd, stb, g, b, wB, epsT, gmul_on_dve=False):
    for half in range(2):
        op("dve", lambda: nc.vector.bn_stats(out=stats[:, half, :], in_=h[:, half * 512:(half + 1) * 512]),
           reads=[hb], writes=[stb])
    op("dve", lambda: nc.vector.bn_aggr(out=mv[:, :], in_=stats[:, :, :].rearrange("p a b -> p (a b)")), reads=[stb], writes=[stb])
    op("act", lambda: nc.scalar.activation(out=rstd[:, :], in_=mv[:, 1:2], func=AF.Sqrt, bias=epsT[:, 0:1]),
       reads=[stb, wB], writes=[stb])
    op("dve", lambda: nc.vector.reciprocal(out=rstd[:, :], in_=rstd[:, :]), reads=[stb], writes=[stb])
    op("dve", lambda: nc.vector.tensor_scalar(out=h[:, :], in0=h[:, :], scalar1=mv[:, 0:1], scalar2=rstd[:, 0:1],
                                              op0=ALU.subtract, op1=ALU.mult), reads=[stb, hb], writes=[hb])
    if gmul_on_dve:
        op("dve", lambda: nc.vector.tensor_tensor(out=h[:, :], in0=h[:, :], in1=g[:, :], op=ALU.mult), reads=[hb, wB], writes=[hb])
    else:
        op("pool", lambda: nc.gpsimd.tensor_tensor(out=h[:, :], in0=h[:, :], in1=g[:, :], op=ALU.mult), reads=[hb, wB], writes=[hb])
    op("pool", lambda: nc.gpsimd.tensor_tensor(out=h[:, :], in0=h[:, :], in1=b[:, :], op=ALU.add), reads=[hb, wB], writes=[hb])


def routing(nc, op, dma, R, Ind, Lg, Lgb, rb, ustr_bf, ones_bf, b_const, psf, bankbuf, next_bank,
            base, ebase, D1, D2, RW1, RW2, T, wB):
    V_ = nc.vector

    def dv(fn, extra_r=()):
        op("dve", fn, reads=[rb, Lgb] + list(extra_r), writes=[rb])
    lg = Lg[:, :, 0:4]
    le = Lg[:, :, 4:36].rearrange("p s (g e) -> p s g e", g=4)
    dv(lambda: V_.tensor_reduce(out=R["gmax"][:, :], in_=lg, axis=AX.X, op=ALU.max))
    dv(lambda: V_.tensor_tensor(out=R["ohg"][:, :, :], in0=lg, in1=R["gmax"][:, :].unsqueeze(2).to_broadcast([128, 4, 4]),
                                op=ALU.is_equal))
    dv(lambda: V_.tensor_tensor(out=R["eg"][:, :, :], in0=lg, in1=R["gmax"][:, :].unsqueeze(2).to_broadcast([128, 4, 4]),
                                op=ALU.subtract))
    op("act", lambda: nc.scalar.activation(out=R["eg"][:, :, :], in_=R["eg"][:, :, :], func=AF.Exp), reads=[rb], writes=[rb])
    dv(lambda: V_.tensor_reduce(out=R["sumg"][:, :], in_=R["eg"][:, :, :], axis=AX.X, op=ALU.add))
    dv(lambda: V_.reciprocal(out=R["gp"][:, :], in_=R["sumg"][:, :]))
    dv(lambda: V_.tensor_tensor(out=R["prod"][:, :, :, :], in0=le,
                                in1=R["ohg"][:, :, :].unsqueeze(3).to_broadcast([128, 4, 4, 8]), op=ALU.mult))
    dv(lambda: V_.tensor_reduce(out=R["sel"][:, :, :], in_=R["prod"][:, :, :, :].rearrange("p s g e -> p s e g"),
                                axis=AX.X, op=ALU.add))
    dv(lambda: V_.tensor_reduce(out=R["m1"][:, :], in_=R["sel"][:, :, :], axis=AX.X, op=ALU.max))
    dv(lambda: V_.tensor_tensor(out=R["oh1"][:, :, :], in0=R["sel"][:, :, :],
                                in1=R["m1"][:, :].unsqueeze(2).to_broadcast([128, 4, 8]), op=ALU.is_equal))
    dv(lambda: V_.scalar_tensor_tensor(out=R["sel2"][:, :, :], in0=R["oh1"][:, :, :], scalar=-1e30, in1=R["sel"][:, :, :],
                                       op0=ALU.mult, op1=ALU.add))
    dv(lambda: V_.tensor_reduce(out=R["m2"][:, :], in_=R["sel2"][:, :, :], axis=AX.X, op=ALU.max))
    dv(lambda: V_.tensor_tensor(out=R["oh2"][:, :, :], in0=R["sel2"][:, :, :],
                                in1=R["m2"][:, :].unsqueeze(2).to_broadcast([128, 4, 8]), op=ALU.is_equal))
    dv(lambda: V_.tensor_tensor(out=R["dm"][:, :], in0=R["m1"][:, :], in1=R["m2"][:, :], op=ALU.subtract))
    op("act", lambda: nc.scalar.activation(out=R["w1"][:, :], in_=R["dm"][:, :], func=AF.Sigmoid), reads=[rb], writes=[rb])
    dv(lambda: V_.tensor_scalar(out=R["w2"][:, :], in0=R["w1"][:, :], scalar1=-1.0, scalar2=1.0, op0=ALU.mult, op1=ALU.add))
    dv(lambda: V_.tensor_tensor(out=RW1[:, 4 * T:4 * T + 4], in0=R["w1"][:, :], in1=R["gp"][:, :], op=ALU.mult))
    dv(lambda: V_.tensor_tensor(out=RW2[:, 4 * T:4 * T + 4], in0=R["w2"][:, :], in1=R["gp"][:, :], op=ALU.mult))
    ohg_b = R["ohg"][:, :, :].unsqueeze(3).to_broadcast([128, 4, 4, 8])
    for nm, src in (("OH1", "oh1"), ("OH2", "oh2")):
        dv(lambda: V_.tensor_tensor(out=R[nm][:, :, :].rearrange("p s (g e) -> p s g e", g=4), in0=ohg_b,
                                    in1=R[src][:, :, :].unsqueeze(2).to_broadcast([128, 4, 4, 8]), op=ALU.mult))
    dv(lambda: V_.tensor_tensor(out=Ind[:, :, :], in0=R["OH1"][:, :, :], in1=R["OH2"][:, :, :], op=ALU.add))
    bR, bT = next_bank(), next_bank()
    ind2 = Ind[:, :, :].rearrange("p s e -> p (s e)")
    op("pe", lambda: nc.tensor.matmul(psf[:, bR, 0:128], ustr_bf[:, :], ind2, start=True, stop=True),
       reads=[rb, b_const], writes=[bankbuf[bR]])
    op("pe", lambda: nc.tensor.matmul(psf[:, bT, 0:128], ones_bf[:, :], ind2, start=True, stop=True),
       reads=[rb, b_const], writes=[bankbuf[bT]])
    for s in range(4):
        dv(lambda: V_.tensor_tensor(out=R["Rk"][:, s, :], in0=psf[:, bR, s * 32:(s + 1) * 32], in1=base[:, :], op=ALU.add),
           extra_r=[bankbuf[bR], wB])
        op("dve", lambda: V_.tensor_tensor(out=base[:, :], in0=base[:, :], in1=psf[:, bT, s * 32:(s + 1) * 32], op=ALU.add),
           reads=[rb, wB, bankbuf[bT]], writes=[rb, wB])
    dv(lambda: V_.tensor_scalar(out=R["ov"][:, :, :], in0=R["Rk"][:, :, :], scalar1=float(CAP) - 0.5, scalar2=1.0e6,
                                op0=ALU.is_ge, op1=ALU.mult))
    dv(lambda: V_.tensor_tensor(out=R["Rk"][:, :, :], in0=R["Rk"][:, :, :], in1=R["ov"][:, :, :], op=ALU.add))
    dv(lambda: V_.tensor_tensor(out=R["Rk"][:, :, :], in0=R["Rk"][:, :, :],
                                in1=ebase[:, :].unsqueeze(1).to_broadcast([128, 4, 32]), op=ALU.add), extra_r=[b_const])
    for nm, dst, Dk in (("OH1", "d1f", D1), ("OH2", "d2f", D2)):
        dv(lambda: V_.tensor_tensor(out=R[nm][:, :, :], in0=R[nm][:, :, :], in1=R["Rk"][:, :, :], op=ALU.mult))
        dv(lambda: V_.tensor_reduce(out=R[dst][:, :], in_=R[nm][:, :, :], axis=AX.X, op=ALU.add))
        dv(lambda: V_.tensor_copy(out=Dk[:, 4 * T:4 * T + 4], in_=R[dst][:, :]))


_NC_CACHE = {}


def _prep_core(c, x, shared):
    b, p = c // 2, c % 2
    xr = x[b, ::-1, :]
    blocks = [2 * i + p for i in range(NBLK)]
    rows_o = np.concatenate([np.arange(128 * a, 128 * a + 128) for a in blocks])
    xo = xr[rows_o]
    halo = np.zeros((64, D), np.float32)
    for i, a in enumerate(blocks):
        r0 = 128 * (a + 1)
        if r0 < S:
            halo[2 * i] = xr[r0]
            halo[2 * i + 1] = xr[r0 + 1]
    tri = np.where(np.arange(128)[None, :] <= np.arange(128)[:, None], MASKV, 0.0).astype(np.float32)
    if p == 0:
        mask = np.concatenate([tri, np.zeros((128, 128), np.float32)], axis=1)
    else:
        mask = np.concatenate([np.full((128, 128), MASKV, np.float32), tri], axis=1)
    ident = np.eye(128, dtype=np.float32)
    ustr = (np.arange(128)[:, None] < np.arange(128)[None, :]).astype(np.float32)
    ones = np.ones((128, 128), np.float32)
    eb = np.tile((np.arange(32, dtype=np.float32) * CAP)[None, :], (128, 1))
    m01 = (mask == 0.0).astype(np.float32)
    consts = np.ascontiguousarray(np.concatenate([ident, ustr, ones, mask, eb, m01], axis=1))
    m = dict(shared)
    m.update({
        "xT": np.ascontiguousarray(xr.T),
        "xTsh": np.ascontiguousarray(np.concatenate([xr.T[:, 1:], np.zeros((D, 1), np.float32)], axis=1)),
        "xTs": np.ascontiguousarray(xr[0:S:256].T),
        "xTo": np.ascontiguousarray(xo.T),
        "xTsho": np.ascontiguousarray(np.concatenate([xr, np.zeros((1, D), np.float32)], axis=0)[rows_o + 1].T),
        "xTh": np.ascontiguousarray(halo.T),
        "xo": np.ascontiguousarray(xo),
        "consts": consts,
    })
    return m


def _prep_shared(w_in, gate_bias, conv_w, w_branch_a, w_branch_b, w_out, ln1_g, ln1_b,
                 w_router_g, b_router_g, w_router_e, b_router_e, w_gate, w_up, w_down, ln2_g, ln2_b):
    f = lambda a: np.ascontiguousarray(np.asarray(a, dtype=np.float32))
    gb = f(gate_bias[0]).reshape(16, 128).T
    cwp = f(conv_w[0]).reshape(3, 4, 128).transpose(2, 1, 0).reshape(128, 12)
    return {
        "w_in": f(w_in[0]), "gbias": f(gb), "cw": f(cwp),
        "w_ba": f(w_branch_a[0]), "w_bb": f(w_branch_b[0]), "w_out": f(w_out[0]),
        "lnp": f(np.stack([ln1_g[0], ln1_b[0], ln2_g[0], ln2_b[0]], axis=0)),
        "w_r": f(np.concatenate([w_router_g[0], w_router_e[0]], axis=1)),
        "b_r": f(np.concatenate([b_router_g[0], b_router_e[0].reshape(-1)], axis=0)),
        "w_gate": f(w_gate[0]), "w_up": f(w_up[0]), "w_down": f(w_down[0]),
    }


def kernel(x, w_in, gate_bias, conv_w, w_branch_a, w_branch_b, w_out, ln1_g, ln1_b,
           w_router_g, b_router_g, w_router_e, b_router_e, w_gate, w_up, w_down, ln2_g, ln2_b):
    x = np.asarray(x, dtype=np.float32)
    shared = _prep_shared(w_in, gate_bias, conv_w, w_branch_a, w_branch_b, w_out, ln1_g, ln1_b,
                          w_router_g, b_router_g, w_router_e, b_router_e, w_gate, w_up, w_down, ln2_g, ln2_b)
    in_maps = [_prep_core(c, x, shared) for c in range(8)]
    nc = build(4)
    res = run_bass_kernel_spmd(nc, in_maps, core_ids=list(range(8)))
    out = np.zeros((4, S, D), np.float32)
    for c in range(8):
        b, p = c // 2, c % 2
        oc = np.asarray(res.results[c]["out"]).reshape(NOWN, D)
        for i in range(NBLK):
            a = 2 * i + p
            rr = np.arange(128 * a, 128 * a + 128)
            out[b, S - 1 - rr] = oc[128 * i:128 * i + 128]
    return out
```

```python
import numpy as np
from contextlib import ExitStack
import concourse.bass as bass
import concourse.mybir as mybir
from concourse.bass_utils import run_bass_kernel_spmd

F32 = mybir.dt.float32
BF16 = mybir.dt.bfloat16
I32 = mybir.dt.int32
AF = mybir.ActivationFunctionType
ALU = mybir.AluOpType
AX = mybir.AxisListType

S = 8192
D = 1024
NOWN = 4096
NBLK = 32
CAP = 384
NSLOT = 32 * CAP
ALPHA = 2.0 ** 0.25
EPS = 1e-5
MASKV = -30000.0
NDMA = 12


class Buf:
    __slots__ = ("lw", "rd")

    def __init__(self):
        self.lw = {}
        self.rd = {}


class Ctx:
    def __init__(self, nc, es):
        self.nc = nc
        self.eng = {"pe": nc.tensor, "act": nc.scalar, "dve": nc.vector, "pool": nc.gpsimd, "sp": nc.sync}
        self.semobj = {}
        self.cnt = {}
        self.waited = {e: {} for e in self.eng}
        for e in self.eng:
            self.semobj[e] = es.enter_context(nc.semaphore("s_" + e))
            self.cnt[e] = 0
        self.rr = {"sp": 0, "pool": 0}
        for q in ("sp", "pool"):
            for i in range(NDMA):
                k = (q, i)
                self.semobj[k] = es.enter_context(nc.semaphore("d_%s%d" % (q, i)))
                self.cnt[k] = 0

    def _deps(self, reads, writes):
        need = {}
        for b in reads:
            for k, v in b.lw.items():
                if need.get(k, 0) < v:
                    need[k] = v
        for b in writes:
            for k, v in b.lw.items():
                if need.get(k, 0) < v:
                    need[k] = v
            for k, v in b.rd.items():
                if need.get(k, 0) < v:
                    need[k] = v
        return need

    def _wait(self, e, need):
        eng = self.eng[e]
        w = self.waited[e]
        for k, v in need.items():
            if k == e and e == "pe":
                continue
            if w.get(k, 0) >= v:
                continue
            eng.wait_ge(self.semobj[k], v)
            w[k] = v

    def _mark(self, ev, reads, writes):
        for b in writes:
            b.lw[ev[0]] = ev[1]
            b.rd = {}
        for b in reads:
            if b.rd.get(ev[0], 0) < ev[1]:
                b.rd[ev[0]] = ev[1]

    def op(self, e, fn, reads=(), writes=()):
        self._wait(e, self._deps(reads, writes))
        ins = fn()
        self.cnt[e] += 1
        ins.then_inc(self.semobj[e], 1)
        self._mark((e, self.cnt[e]), reads, writes)

    def dma(self, q, fn, reads=(), writes=()):
        need = self._deps(reads, writes)
        i = self.rr[q]
        self.rr[q] = (i + 1) % NDMA
        k = (q, i)
        if self.cnt[k] > 0:
            need[k] = max(need.get(k, 0), self.cnt[k])
        self._wait(q, need)
        ins = fn()
        self.cnt[k] += 16
        ins.then_inc(self.semobj[k], 16)
        self._mark((k, self.cnt[k]), reads, writes)

    def barrier(self):
        allv = {k: v for k, v in self.cnt.items() if v > 0}
        for e in self.eng:
            need = {k: v for k, v in allv.items() if k != e}
            self._wait(e, need)


def build(stage=3):
    nc = bass.Bass("TRN2", target_bir_lowering=False)

    def din(name, shape, dt=F32):
        return nc.dram_tensor(name, list(shape), dt, kind="ExternalInput").ap()

    xT = din("xT", [D, S])
    xTsh = din("xTsh", [D, S])
    xTsho = din("xTsho", [D, NOWN])
    xTs = din("xTs", [D, 32])
    xTo = din("xTo", [D, NOWN])
    xTh = din("xTh", [D, 64])
    xo = din("xo", [NOWN, D])
    w_in = din("w_in", [D, 5120])
    gbias = din("gbias", [128, 16])
    cw = din("cw", [128, 12])
    w_ba = din("w_ba", [512, D])
    w_bb = din("w_bb", [512, D])
    w_out = din("w_out", [D, D])
    lnp = din("lnp", [4, D])
    w_r = din("w_r", [D, 36])
    b_r = din("b_r", [36])
    w_gate = din("w_gate", [32, D, 512])
    w_up = din("w_up", [32, D, 512])
    w_down = din("w_down", [32, 512, D])
    consts = din("consts", [128, 128 * 3 + 256 + 32 + 256])
    out = nc.dram_tensor("out", [NOWN, D], F32, kind="ExternalOutput").ap()
    if stage == 1:
        attn_scr = nc.dram_tensor("attn_scr", [512, NOWN], BF16, kind="ExternalOutput").ap()
    else:
        attn_scr = nc.dram_tensor("attn_scr", [512, NOWN], BF16).ap()
    if stage == 2:
        h1_scr = nc.dram_tensor("h1_scr", [NOWN, D], F32, kind="ExternalOutput").ap()
        dbg_r = nc.dram_tensor("dbg_r", [128, 32 * 4], F32, kind="ExternalOutput").ap()
    else:
        h1_scr = nc.dram_tensor("h1_scr", [NOWN, D], F32).ap()
    xg_scr = nc.dram_tensor("xg_scr", [NSLOT, D], BF16).ap()
    vsh_scr = nc.dram_tensor("vsh_scr", [512, NOWN], BF16).ap()
    ys_scr = nc.dram_tensor("ys_scr", [NSLOT, D], F32).ap()

    w_in_v = w_in.rearrange("(k p) n -> p k n", p=128)
    xT_v = xT.rearrange("(k p) t -> p k t", p=128)
    xTo_v = xTo.rearrange("(k p) t -> p k t", p=128)
    xTh_v = xTh.rearrange("(k p) t -> p k t", p=128)
    xTs_v = xTs.rearrange("(k p) t -> p k t", p=128)
    xTsh_v = xTsh.rearrange("(k p) t -> p k t", p=128)
    xTsho_v = xTsho.rearrange("(k p) t -> p k t", p=128)

    with ExitStack() as es:
        E = es.enter_context
        cx = Ctx(nc, es)
        op, dma = cx.op, cx.dma

        def sb(st, name, shape, dt):
            return st.enter_context(nc.sbuf_tensor(name, list(shape), dt))

        ident_bf = sb(es, "ident_bf", [128, 128], BF16)
        ustr_bf = sb(es, "ustr_bf", [128, 128], BF16)
        ones_bf = sb(es, "ones_bf", [128, 128], BF16)
        mask_bf = sb(es, "mask_bf", [128, 256], BF16)
        ident_f = sb(es, "ident_f", [128, 128], F32)
        ebase = sb(es, "ebase", [128, 32], F32)
        D1 = sb(es, "D1", [128, 32], I32)
        D2 = sb(es, "D2", [128, 32], I32)
        RW1 = sb(es, "RW1", [128, 32], F32)
        RW2 = sb(es, "RW2", [128, 32], F32)
        zeros = sb(es, "zeros", [128, 512], F32)
        zeros_bf = sb(es, "zeros_bf", [128, 512], BF16)
        b_const = Buf()
        bc_reg = nc.gpsimd.alloc_register("bc_reg")
        nc.gpsimd.reg_mov(bc_reg, NSLOT - 1)
        dma("pool", lambda: nc.gpsimd.dma_start(out=ident_bf[:], in_=consts[:, 0:128]), writes=[b_const])
        dma("pool", lambda: nc.gpsimd.dma_start(out=ustr_bf[:], in_=consts[:, 128:256]), writes=[b_const])
        dma("pool", lambda: nc.gpsimd.dma_start(out=ones_bf[:], in_=consts[:, 256:384]), writes=[b_const])
        dma("pool", lambda: nc.gpsimd.dma_start(out=mask_bf[:], in_=consts[:, 384:640]), writes=[b_const])
        dma("sp", lambda: nc.sync.dma_start(out=ident_f[:], in_=consts[:, 0:128]), writes=[b_const])
        dma("sp", lambda: nc.sync.dma_start(out=ebase[:], in_=consts[:, 640:672]), writes=[b_const])
        mask01 = sb(es, "mask01", [128, 256], F32)
        dma("sp", lambda: nc.sync.dma_start(out=mask01[:], in_=consts[:, 672:928]), writes=[b_const])
        op("pool", lambda: nc.gpsimd.memset(zeros[:], 0.0), writes=[b_const])
        op("pool", lambda: nc.gpsimd.memset(zeros_bf[:], 0.0), writes=[b_const])
        ones_f = sb(es, "ones_f", [128, 1], F32)
        op("pool", lambda: nc.gpsimd.memset(ones_f[:], 1.0), writes=[b_const])
        epsT = sb(es, "epsT", [128, 1], F32)
        op("pool", lambda: nc.gpsimd.memset(epsT[:], EPS), writes=[b_const])
        cx.barrier()

        ps_stack = [None]

        def alloc_psum(tag, nf, nb):
            if ps_stack[0] is not None:
                ps_stack[0].close()
            st_ = ExitStack()
            es.callback(st_.close)
            ps_stack[0] = st_
            f_ = st_.enter_context(nc.psum_tensor("psf" + tag, [128, nf, 512], F32))
            b_ = st_.enter_context(nc.psum_tensor("psb" + tag, [128, nb, 1024], BF16))
            return f_, b_

        psf, psb = alloc_psum("A", 6, 2)
        bankbuf = [Buf() for _ in range(6)]
        pbbuf = [Buf(), Buf(), Buf()]
        bank_rr = [0]

        def next_bank(n=6):
            i = bank_rr[0] % n
            bank_rr[0] += 1
            return i

        evac_rr = [0]

        def evac(out_ap, in_ap, reads, writes, scale=None):
            evac_rr[0] += 1
            if evac_rr[0] % 2 == 0:
                if scale is None:
                    op("act", lambda: nc.scalar.copy(out=out_ap, in_=in_ap), reads=reads, writes=writes)
                else:
                    op("act", lambda: nc.scalar.mul(out=out_ap, in_=in_ap, mul=scale), reads=reads, writes=writes)
            else:
                if scale is None:
                    op("dve", lambda: nc.vector.tensor_copy(out=out_ap, in_=in_ap), reads=reads, writes=writes)
                else:
                    op("dve", lambda: nc.vector.tensor_scalar(out=out_ap, in0=in_ap, scalar1=scale, scalar2=None,
                                                              op0=ALU.mult), reads=reads, writes=writes)

        def mm_group(bank_ap, bankb, pairs, reads):
            def fn():
                n = len(pairs)
                ins = None
                for i, (l, r) in enumerate(pairs):
                    ins = nc.tensor.matmul(bank_ap, l, r, start=(i == 0), stop=(i == n - 1))
                return ins
            op("pe", fn, reads=reads, writes=[bankb])

        with ExitStack() as pa:
            KT = sb(pa, "KT", [128, 4, S], BF16)
            V = sb(pa, "dV", [128, 64, 512], BF16)
            VsT = sb(pa, "VsT", [128, 4, 32], F32)
            VsTb = Buf()
            KTb = [[Buf() for _ in range(4)] for _ in range(16)]
            Vb = [Buf() for _ in range(64)]
            QTb = [[Buf() for _ in range(4)] for _ in range(8)]
            with ExitStack() as pa1:
                wqkv = sb(pa1, "wqkv", [128, 8, 1024], BF16)
                wvn = sb(pa1, "wvn", [128, 8, 512], BF16)
                xs = sb(pa1, "xs", [128, 8, 32], BF16)
                xt = [sb(pa1, "xt%d" % i, [128, 8, 512], BF16) for i in range(2)]
                xtsh = [sb(pa1, "xtsh%d" % i, [128, 8, 512], BF16) for i in range(2)]
                xtb = [Buf(), Buf()]
                xtshb = [Buf(), Buf()]
                wb = Buf()
                dma("pool", lambda: nc.gpsimd.dma_start(out=wqkv[:, :, :], in_=w_in_v[:, :, 512:1536]), writes=[wb])
                dma("pool", lambda: nc.gpsimd.dma_start(out=xs[:], in_=xTs_v), writes=[wb])
                op("dve", lambda: nc.vector.tensor_scalar(out=wvn[:, :, :], in0=wqkv[:, :, 512:1024], scalar1=-1.0, scalar2=None,
                                                          op0=ALU.mult), reads=[wb], writes=[wb])
                for j in range(4):
                    bk = next_bank()
                    mm_group(psf[:, bk, 0:32], bankbuf[bk],
                             [(wqkv[:, k, 512 + j * 128:512 + (j + 1) * 128], xs[:, k, :]) for k in range(8)], reads=[wb])
                    op("dve", lambda: nc.vector.tensor_copy(out=VsT[:, j, :], in_=psf[:, bk, 0:32]), reads=[bankbuf[bk]], writes=[VsTb])
                for T in range(16):
                    sl = T % 2
                    dma("pool", lambda: nc.gpsimd.dma_start(out=xt[sl][:], in_=xT_v[:, :, T * 512:(T + 1) * 512]),
                        writes=[xtb[sl]])
                    dma("pool", lambda: nc.gpsimd.dma_start(out=xtsh[sl][:], in_=xTsh_v[:, :, T * 512:(T + 1) * 512]),
                        writes=[xtshb[sl]])
                    for j in range(4):
                        bk = next_bank()
                        mm_group(psf[:, bk, :], bankbuf[bk],
                                 [(wqkv[:, k, j * 128:(j + 1) * 128], xt[sl][:, k, :]) for k in range(8)],
                                 reads=[wb, xtb[sl]])
                        evac(KT[:, j, T * 512:(T + 1) * 512], psf[:, bk, :], [bankbuf[bk]], [KTb[T][j]])
                    for s in range(4):
                        bk = next_bank()
                        mm_group(psf[:, bk, :], bankbuf[bk],
                                 [(xtsh[sl][:, k, s * 128:(s + 1) * 128], wqkv[:, k, 512:1024]) for k in range(8)] +
                                 [(xt[sl][:, k, s * 128:(s + 1) * 128], wvn[:, k, :]) for k in range(8)],
                                 reads=[wb, xtb[sl], xtshb[sl]])
                        evac(V[:, 4 * T + s, :], psf[:, bk, :], [bankbuf[bk]], [Vb[4 * T + s]])
                cx.barrier()
            QT = sb(pa, "QT", [128, 4, NOWN], BF16)
            with ExitStack() as paq:
                wq = sb(paq, "wq", [128, 8, 512], BF16)
                wvq = sb(paq, "wvq", [128, 8, 512], BF16)
                xt = [sb(paq, "xtq%d" % i, [128, 8, 512], BF16) for i in range(1)] * 2
                xtso = [sb(paq, "xtso%d" % i, [128, 8, 512], BF16) for i in range(1)] * 2
                vstg = [sb(paq, "vstg%d" % i, [128, 512], BF16) for i in range(2)]
                xtb = [Buf()] * 2
                xtsob = [Buf()] * 2
                vstgb = [Buf(), Buf()]
                wb = Buf()
                vsh_w = vsh_scr.rearrange("(j p) t -> p j t", p=128)
                dma("pool", lambda: nc.gpsimd.dma_start(out=wq[:, :, :], in_=w_in_v[:, :, 0:512]), writes=[wb])
                dma("pool", lambda: nc.gpsimd.dma_start(out=wvq[:, :, :], in_=w_in_v[:, :, 1024:1536]), writes=[wb])
                for T in range(8):
                    sl = T % 2
                    dma("pool", lambda: nc.gpsimd.dma_start(out=xt[sl][:], in_=xTo_v[:, :, T * 512:(T + 1) * 512]),
                        writes=[xtb[sl]])
                    for j in range(4):
                        bk = next_bank()
                        mm_group(psf[:, bk, :], bankbuf[bk],
                                 [(wq[:, k, j * 128:(j + 1) * 128], xt[sl][:, k, :]) for k in range(8)],
                                 reads=[wb, xtb[sl]])
                        evac(QT[:, j, T * 512:(T + 1) * 512], psf[:, bk, :], [bankbuf[bk]], [QTb[T][j]], scale=0.125)
                    dma("pool", lambda: nc.gpsimd.dma_start(out=xtso[sl][:], in_=xTsho_v[:, :, T * 512:(T + 1) * 512]),
                        writes=[xtsob[sl]])
                    for j in range(4):
                        bk = next_bank()
                        vs_ = (T * 4 + j) % 2
                        mm_group(psf[:, bk, :], bankbuf[bk],
                                 [(wvq[:, k, j * 128:(j + 1) * 128], xtso[sl][:, k, :]) for k in range(8)],
                                 reads=[wb, xtsob[sl]])
                        evac(vstg[vs_][:, :], psf[:, bk, :], [bankbuf[bk]], [vstgb[vs_]])
                        dma("sp", lambda: nc.sync.dma_start(out=vsh_w[:, j, T * 512:(T + 1) * 512], in_=vstg[vs_][:, :]),
                            reads=[vstgb[vs_]])
                cx.barrier()

            if stage == 0:
                return nc
            psf, psb = alloc_psum("T", 5, 3)
            with ExitStack() as pa2:
                NS = 4
                Sb = [sb(pa2, "Sb%d" % i, [128, 512], F32) for i in range(NS)]
                Wb = [sb(pa2, "Wb%d" % i, [128, 512], BF16) for i in range(NS)]
                WTs = [sb(pa2, "WTs%d" % i, [128, 512], BF16) for i in range(NS)]
                carry = sb(pa2, "carry", [128, 8], F32)
                Obf = [sb(pa2, "Obf%d" % i, [128, 512], BF16) for i in range(2)]
                Sbb = [Buf() for _ in range(NS)]
                Wbb = [Buf() for _ in range(NS)]
                WTsb = [Buf() for _ in range(NS)]
                carryb = [Buf() for _ in range(8)]
                Obfb = [Buf(), Buf()]
                NZ = 3
                Ob = [Buf(), Buf()]
                attn_v = attn_scr.rearrange("(j p) t -> p j t", p=128)

                items = []
                for i in range(NBLK):
                    chunks = []
                    s0 = 2 * i
                    while s0 < 64:
                        e0 = min(64, (s0 // 4 + 1) * 4)
                        chunks.append((s0, e0))
                        s0 = e0
                    for c, (s0, e0) in enumerate(chunks):
                        for h in range(8):
                            items.append((i, c, len(chunks), s0, e0, h))
                NIT = len(items)

                QTz = [sb(pa2, "QTz%d" % i_, [128, 8, 128], BF16) for i_ in range(2)]
                QTzb = [Buf(), Buf()]
                Osb = [sb(pa2, "Osb%d" % i_, [128, 512], BF16) for i_ in range(2)]
                Osbb = [Buf(), Buf()]
                for i_ in range(2):
                    op("dve", lambda: nc.vector.memset(QTz[i_][:], 0.0), writes=[QTzb[i_]])

                def st_qk(t):
                    i, c, nch, s0, e0, h = items[t]
                    if c == 0 and h == 0:
                        dma("sp", lambda: nc.sync.dma_start(out=vsh[i % 2][:], in_=vsh_r[:, :, i * 128:(i + 1) * 128]),
                            writes=[vshb[i % 2]])
                        qz = QTz[i % 2]
                        for jj in range(4):
                            op("act", lambda: nc.scalar.copy(out=qz[0:64, 2 * jj, :], in_=QT[0:64, jj, i * 128:(i + 1) * 128]),
                               reads=[QTb[i // 4][jj]], writes=[QTzb[i % 2]])
                            op("act", lambda: nc.scalar.copy(out=qz[64:128, 2 * jj + 1, :], in_=QT[64:128, jj, i * 128:(i + 1) * 128]),
                               reads=[QTb[i // 4][jj]], writes=[QTzb[i % 2]])
                    j, hb = h // 2, (h % 2) * 64
                    n = (e0 - s0) * 128
                    zb = t % NZ
                    rd = [QTzb[i % 2]] + [KTb[ss // 4][j] for ss in range(s0, e0, 4)] + [b_const]

                    def fn():
                        ins = nc.tensor.matmul(psf[:, zb, 0:n], QTz[i % 2][:, h, :],
                                               KT[:, j, s0 * 128:e0 * 128], start=True, stop=(c != 0))
                        if c == 0:
                            ins = nc.tensor.matmul(psf[:, zb, 0:256], ident_bf[:, :], mask_bf[:, :],
                                                   start=False, stop=True)
                        return ins
                    op("pe", fn, reads=rd, writes=[bankbuf[zb]])

                def st_sig(t):
                    i, c, nch, s0, e0, h = items[t]
                    n = (e0 - s0) * 128
                    zb = t % NZ
                    sl = t % NS
                    op("act", lambda: nc.scalar.activation(out=Sb[sl][:, 0:n], in_=psf[:, zb, 0:n], func=AF.Sigmoid,
                                                           scale=-1.0),
                       reads=[bankbuf[zb]], writes=[Sbb[sl]])

                Cb = [sb(pa2, "Cb%d" % i_, [128, 8], F32) for i_ in range(NS)]
                Cbb = [Buf() for _ in range(NS)]
                vsh = [sb(pa2, "vsh%d" % i_, [128, 4, 128], BF16) for i_ in range(2)]
                vshb = [Buf(), Buf()]
                vsh_r = vsh_scr.rearrange("(j p) t -> p j t", p=128)

                def st_pre(t):
                    i, c, nch, s0, e0, h = items[t]
                    sl = t % NS
                    if c == 0:
                        op("pool", lambda: nc.gpsimd.memset(Cb[sl][:, 0:1], 1.0), writes=[Cbb[sl]])
                    else:
                        op("pool", lambda: nc.gpsimd.tensor_copy(out=Cb[sl][:, 0:1], in_=carry[:, h:h + 1]),
                           reads=[carryb[h]], writes=[Cbb[sl]])

                def st_scan_f(t):
                    i, c, nch, s0, e0, h = items[t]
                    n = (e0 - s0) * 128
                    sl = t % NS
                    op("dve", lambda: nc.vector.tensor_tensor_scan(out=Wb[sl][:, 0:n], data0=Sb[sl][:, 0:n],
                                                                   data1=zeros[:, 0:n], initial=Cb[sl][:, 0:1],
                                                                   op0=ALU.mult, op1=ALU.add),
                       reads=[Sbb[sl], Cbb[sl]], writes=[Wbb[sl]])
                    if c != nch - 1:
                        op("pool", lambda: nc.gpsimd.tensor_copy(out=carry[:, h:h + 1], in_=Wb[sl][:, n - 1:n]),
                           reads=[Wbb[sl]], writes=[carryb[h]])
                    if c == 0:
                        op("pool", lambda: nc.gpsimd.tensor_tensor(out=Wb[sl][:, 0:256], in0=Wb[sl][:, 0:256],
                                                                   in1=mask01[:, :], op=ALU.mult),
                           reads=[Wbb[sl], b_const], writes=[Wbb[sl]])

                st_scan = st_scan_f

                def st_tr(t):
                    i, c, nch, s0, e0, h = items[t]
                    nsb = e0 - s0
                    n = nsb * 128
                    sl = t % NS
                    hf = t % 2

                    def fn():
                        ins = None
                        for q in range(nsb):
                            ins = nc.tensor.transpose(psb[:, hf, q * 128:(q + 1) * 128],
                                                      Wb[sl][:, q * 128:(q + 1) * 128], ident_bf[:, :])
                        return ins
                    op("pe", fn, reads=[Wbb[sl], b_const], writes=[pbbuf[hf]])
                    op("act", lambda: nc.scalar.copy(out=WTs[sl][:, 0:n], in_=psb[:, hf, 0:n]),
                       reads=[pbbuf[hf]], writes=[WTsb[sl]])

                def st_av(t):
                    i, c, nch, s0, e0, h = items[t]
                    nsb = e0 - s0
                    sl = t % NS
                    ob = i % 2

                    def fn():
                        ins = None
                        if c == 0 and h == 0:
                            nc.tensor.matmul(psf[:, 3 + ob, :], zeros_bf[:, 0:128], zeros_bf[:, :], start=True, stop=False,
                                             skip_group_check=True)
                        for q in range(nsb):
                            ins = nc.tensor.matmul(psf[:, 3 + ob, h * 64:(h + 1) * 64],
                                                   WTs[sl][:, q * 128:(q + 1) * 128],
                                                   V[:, s0 + q, h * 64:(h + 1) * 64],
                                                   start=False, stop=(c == nch - 1 and h == 7 and q == nsb - 1),
                                                   skip_group_check=True)
                        return ins
                    op("pe", fn, reads=[WTsb[sl]] + [Vb[ss] for ss in range(s0, e0)], writes=[Ob[ob]])
                    if c == nch - 1 and h == 7:
                        op("act", lambda: nc.scalar.copy(out=Osb[ob][:, :], in_=psf[:, 3 + ob, :]), reads=[Ob[ob]], writes=[Osbb[ob]])

                        def fnt():
                            ins = None
                            for jj in range(4):
                                ins = nc.tensor.transpose(psb[:, 2, jj * 128:(jj + 1) * 128], Osb[ob][:, jj * 128:(jj + 1) * 128],
                                                          ident_bf[:, :])
                            return ins
                        op("pe", fnt, reads=[Osbb[ob], b_const], writes=[pbbuf[2]])
                        op("dve", lambda: nc.vector.tensor_tensor(
                            out=Obf[ob][:, :].rearrange("p (j t) -> p j t", j=4),
                            in0=psb[:, 2, 0:512].rearrange("p (j t) -> p j t", j=4),
                            in1=vsh[ob][:, :, :], op=ALU.add),
                           reads=[pbbuf[2], vshb[ob]], writes=[Obfb[ob]])
                        dma("sp", lambda: nc.sync.dma_start(out=attn_v[:, :, i * 128:(i + 1) * 128],
                                                            in_=Obf[ob][:, :].rearrange("p (j t) -> p j t", j=4)),
                            reads=[Obfb[ob]])

                L1, L2, L3 = 2, 4, 5
                for t in range(NIT + L3 + 1):
                    if t < NIT:
                        st_qk(t)
                        st_sig(t)
                        st_pre(t)
                    if 0 <= t - L1 < NIT:
                        st_scan(t - L1)
                    if 0 <= t - L2 < NIT:
                        st_tr(t - L2)
                    if 0 <= t - L3 < NIT:
                        st_av(t - L3)
                cx.barrier()
        if stage == 1:
            cx.barrier()
            return nc
        psf, psb = alloc_psum("B", 6, 2)

        with ExitStack() as pb:
            wc = sb(pb, "wc", [128, 8, 1536], BF16)
            wg = sb(pb, "wg", [128, 8, 2048], BF16)
            wba = sb(pb, "wba", [128, 4, D], BF16)
            wbb = sb(pb, "wbb", [128, 4, D], BF16)
            wo = sb(pb, "wo", [128, 8, D], BF16)
            wr = sb(pb, "wr", [128, 8, 36], F32)
            gb_sb = sb(pb, "gb_sb", [128, 16], F32)
            cw_sb = sb(pb, "cw_sb", [128, 12], F32)
            g1 = sb(pb, "g1", [128, D], F32)
            b1 = sb(pb, "b1", [128, D], F32)
            br = sb(pb, "br", [128, 36], F32)
            base = sb(pb, "base", [128, 32], F32)
            wB = Buf()
            for c0 in range(0, 1536, 512):
                dma("pool", lambda: nc.gpsimd.dma_start(out=wc[:, :, c0:c0 + 512], in_=w_in_v[:, :, 1536 + c0:1536 + c0 + 512]), writes=[wB])
            for c0 in range(0, 2048, 512):
                dma("pool", lambda: nc.gpsimd.dma_start(out=wg[:, :, c0:c0 + 512], in_=w_in_v[:, :, 3072 + c0:3072 + c0 + 512]), writes=[wB])
            dma("pool", lambda: nc.gpsimd.dma_start(out=wba[:], in_=w_ba.rearrange("(k p) n -> p k n", p=128)), writes=[wB])
            dma("pool", lambda: nc.gpsimd.dma_start(out=wbb[:], in_=w_bb.rearrange("(k p) n -> p k n", p=128)), writes=[wB])
            dma("pool", lambda: nc.gpsimd.dma_start(out=wo[:], in_=w_out.rearrange("(k p) n -> p k n", p=128)), writes=[wB])
            dma("sp", lambda: nc.sync.dma_start(out=wr[:], in_=w_r.rearrange("(k p) n -> p k n", p=128)), writes=[wB])
            dma("sp", lambda: nc.sync.dma_start(out=gb_sb[:], in_=gbias), writes=[wB])
            dma("sp", lambda: nc.sync.dma_start(out=cw_sb[:], in_=cw), writes=[wB])
            dma("sp", lambda: nc.sync.dma_start(out=g1[:], in_=lnp[0].partition_broadcast(128)), writes=[wB])
            dma("sp", lambda: nc.sync.dma_start(out=b1[:], in_=lnp[1].partition_broadcast(128)), writes=[wB])
            dma("sp", lambda: nc.sync.dma_start(out=br[:], in_=b_r.partition_broadcast(128)), writes=[wB])
            op("pool", lambda: nc.gpsimd.memset(base[:], 0.0), writes=[wB])
            cx.barrier()

            xt = [sb(pb, "xtB%d" % i, [128, 8, 512], BF16) for i in range(2)]
            xh = [sb(pb, "xh%d" % i, [128, 8, 8], BF16) for i in range(2)]
            at = [sb(pb, "at%d" % i, [128, 4, 512], BF16) for i in range(2)]
            xtok = [sb(pb, "xtok%d" % i, [128, 4, D], F32) for i in range(1)]
            inb = [Buf(), Buf()]
            xtokb = Buf()
            dummy = sb(pb, "dummyB", [128, 1], F32)
            ccs = sb(pb, "ccs", [128, 512], F32)
            cchs = sb(pb, "cchs", [128, 8], F32)
            U = sb(pb, "U", [128, 4, 130], F32)
            Y = sb(pb, "Y", [128, 4, 128], F32)
            Bin = sb(pb, "Bin", [128, 4, 512], BF16)
            sa = [sb(pb, "sa%d" % i, [128, 512], F32) for i in range(1)] * 2
            sbg = [sb(pb, "sbg%d" % i, [128, 512], F32) for i in range(1)] * 2
            t1 = [sb(pb, "t1%d" % i, [128, 512], F32) for i in range(1)] * 2
            t2 = [sb(pb, "t2%d" % i, [128, 512], F32) for i in range(1)] * 2
            GT = sb(pb, "GT", [128, 8, 512], BF16)
            hbuf = [sb(pb, "hbuf%d" % i, [128, D], F32) for i in range(4)]
            h1b = [sb(pb, "h1b%d" % i, [128, D], BF16) for i in range(4)]
            h1T = [sb(pb, "h1T%d" % i, [128, 8, 128], F32) for i in range(2)]
            stats = [sb(pb, "stB%d" % i, [128, 2, 6], F32) for i in range(4)]
            mv = [sb(pb, "mvB%d" % i, [128, 2], F32) for i in range(4)]
            rstd = [sb(pb, "rstdB%d" % i, [128, 1], F32) for i in range(4)]
            Lg = sb(pb, "Lg", [128, 4, 36], F32)
            ccsb, Ub, Yb, Binb, GTb = Buf(), Buf(), Buf(), Buf(), Buf()
            sab = [Buf()] * 2
            t1b = [Buf()] * 2
            hb_ = [Buf() for _ in range(4)]
            h1bb = [Buf() for _ in range(4)]
            h1Tb, stb, Lgb = [Buf(), Buf()], [Buf() for _ in range(4)], Buf()
            R = {}
            for nm, shp in (("gmax", [128, 4]), ("ohg", [128, 4, 4]), ("eg", [128, 4, 4]), ("sumg", [128, 4]),
                            ("gp", [128, 4]), ("prod", [128, 4, 4, 8]), ("sel", [128, 4, 8]), ("m1", [128, 4]),
                            ("oh1", [128, 4, 8]), ("sel2", [128, 4, 8]), ("m2", [128, 4]), ("oh2", [128, 4, 8]),
                            ("dm", [128, 4]), ("w1", [128, 4]), ("w2", [128, 4]), ("ind8", [128, 4, 8]),
                            ("OH1", [128, 4, 32]), ("OH2", [128, 4, 32]), ("Rk", [128, 4, 32]),
                            ("ov", [128, 4, 32]), ("d1f", [128, 4]), ("d2f", [128, 4])):
                R[nm] = sb(pb, "r_" + nm, shp, F32)
            Ind = sb(pb, "Ind", [128, 4, 32], BF16)
            rb = Buf()
            xT_B = xTo_v
            attn_v = attn_scr.rearrange("(j p) t -> p j t", p=128)
            xo_v = xo.rearrange("(s p) d -> p s d", p=128)
            h1_v = h1_scr.rearrange("(s p) d -> p s d", p=128)

            def load_tile(T_):
                sl_ = T_ % 2
                dma("pool", lambda: nc.gpsimd.dma_start(out=xt[sl_][:], in_=xT_B[:, :, T_ * 512:(T_ + 1) * 512]), writes=[inb[sl_]])
                dma("pool", lambda: nc.gpsimd.dma_start(out=xh[sl_][:], in_=xTh_v[:, :, T_ * 8:(T_ + 1) * 8]), writes=[inb[sl_]])
                dma("sp", lambda: nc.sync.dma_start(out=at[sl_][:], in_=attn_v[:, :, T_ * 512:(T_ + 1) * 512]), writes=[inb[sl_]])

            pending_route = []

            def flush_route():
                while pending_route:
                    Tr = pending_route.pop(0)
                    routing(nc, op, dma, R, Ind, Lg, Lgb, rb, ustr_bf, ones_bf, b_const, psf, bankbuf, next_bank,
                            base, ebase, D1, D2, RW1, RW2, Tr, wB)
                    for s_ in range(4):
                        gs_ = 4 * Tr + s_
                        for Dk in (D1, D2):
                            dma("pool", lambda: nc.gpsimd.indirect_dma_start(
                                out=xg_scr, out_offset=bass.IndirectOffsetOnAxis(ap=Dk[:, gs_:gs_ + 1], axis=0),
                                in_=h1b[s_][:, :], in_offset=None, bounds_check=bc_reg, oob_is_err=False),
                                reads=[h1bb[s_], rb])

            load_tile(0)
            dma("sp", lambda: nc.sync.dma_start(out=xtok[0][:], in_=xo_v[:, 0:4, :]), writes=[xtokb])
            for T in range(8):
                sl = T % 2
                if T + 1 < 8:
                    load_tile(T + 1)
                for m in range(4):
                    bcc, bch, bcb, bh = next_bank(), next_bank(), next_bank(), next_bank()
                    mm_group(psf[:, bcc, :], bankbuf[bcc],
                             [(wc[:, k, 512 + m * 128:512 + (m + 1) * 128], xt[sl][:, k, :]) for k in range(8)], [wB, inb[sl]])
                    mm_group(psf[:, bch, :], bankbuf[bch],
                             [(wc[:, k, 1024 + m * 128:1024 + (m + 1) * 128], xt[sl][:, k, :]) for k in range(8)], [wB, inb[sl]])
                    mm_group(psf[:, bcb, :], bankbuf[bcb],
                             [(wc[:, k, m * 128:(m + 1) * 128], xt[sl][:, k, :]) for k in range(8)], [wB, inb[sl]])

                    def fnh():
                        ins = None
                        for k in range(8):
                            ins = nc.tensor.matmul(psf[:, bh, 0:8], wc[:, k, 512 + m * 128:512 + (m + 1) * 128],
                                                   xh[sl][:, k, :], start=(k == 0), stop=(k == 7))
                        for k in range(8):
                            ins = nc.tensor.matmul(psf[:, bh, 8:16], wc[:, k, 1024 + m * 128:1024 + (m + 1) * 128],
                                                   xh[sl][:, k, :], start=(k == 0), stop=(k == 7))
                        return ins
                    op("pe", fnh, reads=[wB, inb[sl]], writes=[bankbuf[bh]])
                    op("act", lambda: nc.scalar.copy(out=ccs[:, :], in_=psf[:, bcc, :]), reads=[bankbuf[bcc]], writes=[ccsb])
                    op("act", lambda: nc.scalar.copy(out=cchs[:, :], in_=psf[:, bh, 0:8]), reads=[bankbuf[bh]], writes=[ccsb])
                    op("dve", lambda: nc.vector.tensor_tensor(out=U[:, :, 0:128],
                                                              in0=ccs[:, :].rearrange("p (b t) -> p b t", b=4),
                                                              in1=psf[:, bch, :].rearrange("p (b t) -> p b t", b=4),
                                                              op=ALU.mult), reads=[ccsb, bankbuf[bch]], writes=[Ub])
                    op("dve", lambda: nc.vector.tensor_tensor(out=U[:, :, 128:130],
                                                              in0=cchs[:, :].rearrange("p (b t) -> p b t", b=4),
                                                              in1=psf[:, bh, 8:16].rearrange("p (b t) -> p b t", b=4),
                                                              op=ALU.mult), reads=[ccsb, bankbuf[bh]], writes=[Ub])
                    op("dve", lambda: nc.vector.tensor_scalar(out=Y[:, :, :], in0=U[:, :, 0:128],
                                                              scalar1=cw_sb[:, m * 3 + 2:m * 3 + 3], scalar2=None,
                                                              op0=ALU.mult), reads=[Ub, wB], writes=[Yb])
                    op("dve", lambda: nc.vector.scalar_tensor_tensor(out=Y[:, :, :], in0=U[:, :, 1:129],
                                                                     scalar=cw_sb[:, m * 3 + 1:m * 3 + 2], in1=Y[:, :, :],
                                                                     op0=ALU.mult, op1=ALU.add), reads=[Ub, Yb], writes=[Yb])
                    op("dve", lambda: nc.vector.scalar_tensor_tensor(out=Y[:, :, :], in0=U[:, :, 2:130],
                                                                     scalar=cw_sb[:, m * 3:m * 3 + 1], in1=Y[:, :, :],
                                                                     op0=ALU.mult, op1=ALU.add), reads=[Ub, Yb], writes=[Yb])
                    op("dve", lambda: nc.vector.tensor_tensor(out=Bin[:, m, :], in0=Y[:, :, :].rearrange("p b t -> p (b t)"),
                                                              in1=psf[:, bcb, :], op=ALU.mult),
                       reads=[Yb, bankbuf[bcb]], writes=[Binb])
                flush_route()
                for m in range(8):
                    s2 = m % 2
                    bA, bB, bga, bgb = next_bank(), next_bank(), next_bank(), next_bank()
                    mm_group(psf[:, bA, :], bankbuf[bA],
                             [(wba[:, k, m * 128:(m + 1) * 128], at[sl][:, k, :]) for k in range(4)], [wB, inb[sl]])
                    mm_group(psf[:, bB, :], bankbuf[bB],
                             [(wbb[:, k, m * 128:(m + 1) * 128], Bin[:, k, :]) for k in range(4)], [wB, Binb])
                    mm_group(psf[:, bga, :], bankbuf[bga],
                             [(wg[:, k, m * 128:(m + 1) * 128], xt[sl][:, k, :]) for k in range(8)], [wB, inb[sl]])
                    mm_group(psf[:, bgb, :], bankbuf[bgb],
                             [(wg[:, k, 1024 + m * 128:1024 + (m + 1) * 128], xt[sl][:, k, :]) for k in range(8)], [wB, inb[sl]])
                    op("act", lambda: nc.scalar.activation(out=sa[s2][:, :], in_=psf[:, bga, :], func=AF.Sigmoid,
                                                           bias=gb_sb[:, m:m + 1]), reads=[bankbuf[bga], wB], writes=[sab[s2]])
                    op("act", lambda: nc.scalar.activation(out=sbg[s2][:, :], in_=psf[:, bgb, :], func=AF.Sigmoid,
                                                           bias=gb_sb[:, 8 + m:9 + m]), reads=[bankbuf[bgb], wB], writes=[sab[s2]])
                    op("dve", lambda: nc.vector.tensor_tensor(out=t1[s2][:, :], in0=sa[s2][:, :], in1=psf[:, bA, :], op=ALU.mult),
                       reads=[sab[s2], bankbuf[bA]], writes=[t1b[s2]])
                    op("dve", lambda: nc.vector.tensor_tensor(out=t2[s2][:, :], in0=sbg[s2][:, :], in1=psf[:, bB, :], op=ALU.mult),
                       reads=[sab[s2], bankbuf[bB]], writes=[t1b[s2]])
                    op("pool", lambda: nc.gpsimd.tensor_tensor(out=GT[:, m, :], in0=t1[s2][:, :], in1=t2[s2][:, :], op=ALU.add),
                       reads=[t1b[s2]], writes=[GTb])
                for s in range(4):
                    s2 = s
                    bk0, bk1 = next_bank(), next_bank()
                    for half, bk in ((0, bk0), (1, bk1)):
                        mm_group(psf[:, bk, :], bankbuf[bk],
                                 [(GT[:, k, s * 128:(s + 1) * 128], wo[:, k, half * 512:(half + 1) * 512]) for k in range(8)],
                                 [wB, GTb])
                        op("dve", lambda: nc.vector.scalar_tensor_tensor(out=hbuf[s2][:, half * 512:(half + 1) * 512],
                                                                         in0=xtok[0][:, s, half * 512:(half + 1) * 512],
                                                                         scalar=ALPHA, in1=psf[:, bk, :],
                                                                         op0=ALU.mult, op1=ALU.add),
                           reads=[xtokb, bankbuf[bk]], writes=[hb_[s2]])
                if T + 1 < 8:
                    dma("sp", lambda: nc.sync.dma_start(out=xtok[0][:], in_=xo_v[:, 4 * (T + 1):4 * (T + 1) + 4, :]), writes=[xtokb])
                for s in range(4):
                    for half in range(2):
                        op("dve", lambda: nc.vector.bn_stats(out=stats[s][:, half, :], in_=hbuf[s][:, half * 512:(half + 1) * 512]),
                           reads=[hb_[s]], writes=[stb[s]])
                    op("dve", lambda: nc.vector.bn_aggr(out=mv[s][:, :], in_=stats[s][:, :, :].rearrange("p a b -> p (a b)")),
                       reads=[stb[s]], writes=[stb[s]])
                for s in range(4):
                    op("act", lambda: nc.scalar.activation(out=rstd[s][:, :], in_=mv[s][:, 1:2], func=AF.Sqrt, bias=epsT[:, 0:1]),
                       reads=[stb[s], wB], writes=[stb[s]])
                for s in range(4):
                    op("dve", lambda: nc.vector.reciprocal(out=rstd[s][:, :], in_=rstd[s][:, :]), reads=[stb[s]], writes=[stb[s]])
                    op("dve", lambda: nc.vector.tensor_scalar(out=hbuf[s][:, :], in0=hbuf[s][:, :], scalar1=mv[s][:, 0:1],
                                                              scalar2=rstd[s][:, 0:1], op0=ALU.subtract, op1=ALU.mult),
                       reads=[stb[s], hb_[s]], writes=[hb_[s]])
                    op("dve", lambda: nc.vector.tensor_tensor(out=hbuf[s][:, :], in0=hbuf[s][:, :], in1=g1[:, :], op=ALU.mult),
                       reads=[hb_[s], wB], writes=[hb_[s]])
                    op("pool", lambda: nc.gpsimd.tensor_tensor(out=hbuf[s][:, :], in0=hbuf[s][:, :], in1=b1[:, :], op=ALU.add),
                       reads=[hb_[s], wB], writes=[hb_[s]])
                for s in range(4):
                    gs = 4 * T + s
                    dma("sp", lambda: nc.sync.dma_start(out=h1_v[:, gs, :], in_=hbuf[s][:, :]), reads=[hb_[s]])
                    op("act", lambda: nc.scalar.copy(out=h1b[s][:, :], in_=hbuf[s][:, :]), reads=[hb_[s]], writes=[h1bb[s]])
                for s in range(4):
                    s2 = s
                    hT_ = h1T[s % 2]
                    hTb_ = h1Tb[s % 2]
                    tb0, tb1 = next_bank(), next_bank()

                    def fntr():
                        ins = None
                        for k in range(8):
                            tb = tb0 if k < 4 else tb1
                            ins = nc.tensor.transpose(psf[:, tb, (k % 4) * 128:(k % 4 + 1) * 128],
                                                      hbuf[s2][:, k * 128:(k + 1) * 128], ident_f[:, :])
                        return ins
                    op("pe", fntr, reads=[hb_[s2], b_const], writes=[bankbuf[tb0], bankbuf[tb1]])
                    op("act", lambda: nc.scalar.copy(out=hT_[:, 0:4, :], in_=psf[:, tb0, :].rearrange("p (k t) -> p k t", k=4)),
                       reads=[bankbuf[tb0]], writes=[hTb_])
                    op("dve", lambda: nc.vector.tensor_copy(out=hT_[:, 4:8, :], in_=psf[:, tb1, :].rearrange("p (k t) -> p k t", k=4)),
                       reads=[bankbuf[tb1]], writes=[hTb_])
                    lb = next_bank()
                    mm_group(psf[:, lb, 0:36], bankbuf[lb],
                             [(hT_[:, k, :], wr[:, k, :]) for k in range(8)], [hTb_, wB])
                    op("dve", lambda: nc.vector.tensor_tensor(out=Lg[:, s, :], in0=psf[:, lb, 0:36], in1=br[:, :], op=ALU.add),
                       reads=[bankbuf[lb], wB], writes=[Lgb])
                pending_route.append(T)
            flush_route()
            cx.barrier()
            if stage == 2:
                op("dve", lambda: nc.vector.tensor_copy(out=zeros[:, 0:32], in_=D1[:, :]), writes=[b_const])
                op("dve", lambda: nc.vector.tensor_copy(out=zeros[:, 32:64], in_=D2[:, :]), writes=[b_const])
                op("dve", lambda: nc.vector.tensor_copy(out=zeros[:, 64:96], in_=RW1[:, :]), writes=[b_const])
                op("dve", lambda: nc.vector.tensor_copy(out=zeros[:, 96:128], in_=RW2[:, :]), writes=[b_const])
                dma("sp", lambda: nc.sync.dma_start(out=dbg_r, in_=zeros[:, 0:128]), reads=[b_const])
                cx.barrier()
                return nc

        with ExitStack() as pc:
            NW = 3
            wgs = [sb(pc, "wgs%d" % i, [128, 8, 512], BF16) for i in range(NW)]
            wus = [sb(pc, "wus%d" % i, [128, 8, 512], BF16) for i in range(NW)]
            wds = [sb(pc, "wds%d" % i, [128, 4, D], BF16) for i in range(NW)]
            wEb = [[Buf(), Buf(), Buf()] for _ in range(NW)]
            xgs = [sb(pc, "xgs%d" % i, [128, 3, D], BF16) for i in range(NW)]
            xgb = [Buf() for _ in range(NW)]
            xbT = [sb(pc, "xbT%d" % i, [128, 8, CAP], BF16) for i in range(2)]
            xbTb = [Buf(), Buf()]
            sg = [sb(pc, "sg%d" % i, [128, CAP], F32) for i in range(2)]
            sgb = [Buf(), Buf()]
            hT = [sb(pc, "hT%d" % i, [128, 4, CAP], BF16) for i in range(2)]
            hTb = [Buf(), Buf()]
            ysb = [sb(pc, "ysb%d" % i, [128, D], F32) for i in range(2)]
            ysbb = [Buf(), Buf()]
            xg_v = xg_scr.rearrange("(e j p) d -> e p j d", p=128, j=3)
            ys_v = ys_scr.rearrange("(e j p) d -> e p j d", p=128, j=3)

            def load_w(e):
                sl = e % NW
                dma("pool", lambda: nc.gpsimd.dma_start(out=wgs[sl][:], in_=w_gate[e].rearrange("(k p) f -> p k f", p=128)), writes=[wEb[sl][0]])
                dma("pool", lambda: nc.gpsimd.dma_start(out=wus[sl][:], in_=w_up[e].rearrange("(k p) f -> p k f", p=128)), writes=[wEb[sl][1]])
                dma("pool", lambda: nc.gpsimd.dma_start(out=wds[sl][:], in_=w_down[e].rearrange("(k p) f -> p k f", p=128)), writes=[wEb[sl][2]])
                dma("sp", lambda: nc.sync.dma_start(out=xgs[sl][:], in_=xg_v[e]), writes=[xgb[sl]])

            def do_transposes(e):
                sl = e % NW
                xb = xbT[e % 2]
                for j in range(3):
                    def fnt():
                        ins = None
                        for k in range(8):
                            ins = nc.tensor.transpose(psb[:, j % 2, k * 128:(k + 1) * 128], xgs[sl][:, j, k * 128:(k + 1) * 128],
                                                      ident_bf[:, :])
                        return ins
                    op("pe", fnt, reads=[xgb[sl], b_const], writes=[pbbuf[j % 2]])
                    evac(xb[:, :, j * 128:(j + 1) * 128], psb[:, j % 2, :].rearrange("p (k t) -> p k t", k=8),
                         [pbbuf[j % 2]], [xbTb[e % 2]])

            def do_gate_up(e):
                sl = e % NW
                xb = xbT[e % 2]
                for f in range(4):
                    s2 = f % 2
                    bg, bu = next_bank(), next_bank()
                    mm_group(psf[:, bg, 0:CAP], bankbuf[bg],
                             [(wgs[sl][:, k, f * 128:(f + 1) * 128], xb[:, k, :]) for k in range(8)], wEb[sl] + [xbTb[e % 2]])
                    mm_group(psf[:, bu, 0:CAP], bankbuf[bu],
                             [(wus[sl][:, k, f * 128:(f + 1) * 128], xb[:, k, :]) for k in range(8)], wEb[sl] + [xbTb[e % 2]])
                    op("act", lambda: nc.scalar.activation(out=sg[s2][:, :], in_=psf[:, bg, 0:CAP], func=AF.Silu),
                       reads=[bankbuf[bg]], writes=[sgb[s2]])
                    op("dve", lambda: nc.vector.tensor_tensor(out=hT[e % 2][:, f, :], in0=sg[s2][:, :], in1=psf[:, bu, 0:CAP], op=ALU.mult),
                       reads=[sgb[s2], bankbuf[bu]], writes=[hTb[e % 2]])

            def do_down(e):
                sl = e % NW
                for j in range(3):
                    s2 = j % 2
                    for half in range(2):
                        bk = next_bank()
                        mm_group(psf[:, bk, :], bankbuf[bk],
                                 [(hT[e % 2][:, f, j * 128:(j + 1) * 128], wds[sl][:, f, half * 512:(half + 1) * 512]) for f in range(4)],
                                 wEb[sl] + [hTb[e % 2]])
                        op("act" if half == 0 else "dve",
                           (lambda: nc.scalar.copy(out=ysb[s2][:, 0:512], in_=psf[:, bk, :])) if half == 0 else
                           (lambda: nc.vector.tensor_copy(out=ysb[s2][:, 512:1024], in_=psf[:, bk, :])),
                           reads=[bankbuf[bk]], writes=[ysbb[s2]])
                    dma("sp", lambda: nc.sync.dma_start(out=ys_v[e][:, j, :], in_=ysb[s2][:, :]), reads=[ysbb[s2]])

            load_w(0)
            load_w(1)
            do_transposes(0)
            for e in range(32):
                if e + 2 < 32:
                    load_w(e + 2)
                do_gate_up(e)
                if e + 1 < 32:
                    do_transposes(e + 1)
                do_down(e)
            cx.barrier()

        if stage == 3:
            return nc
        with ExitStack() as pd:
            g2 = sb(pd, "g2", [128, D], F32)
            b2 = sb(pd, "b2", [128, D], F32)
            wD = Buf()
            dma("sp", lambda: nc.sync.dma_start(out=g2[:], in_=lnp[2].partition_broadcast(128)), writes=[wD])
            dma("sp", lambda: nc.sync.dma_start(out=b2[:], in_=lnp[3].partition_broadcast(128)), writes=[wD])
            cx.barrier()
            NR = 4
            r1 = [sb(pd, "r1%d" % i, [128, D], F32) for i in range(NR)]
            r2 = [sb(pd, "r2%d" % i, [128, D], F32) for i in range(NR)]
            hh = [sb(pd, "hh%d" % i, [128, D], F32) for i in range(NR)]
            stats = sb(pd, "stats2", [128, 2, 6], F32)
            mv = sb(pd, "mv2", [128, 2], F32)
            rstd = sb(pd, "rstd2", [128, 1], F32)
            stb = Buf()
            r1b, r2b, hhb = [Buf() for _ in range(NR)], [Buf() for _ in range(NR)], [Buf() for _ in range(NR)]
            h1_v = h1_scr.rearrange("(s p) d -> p s d", p=128)
            out_v = out.rearrange("(s p) d -> p s d", p=128)

            def loads_d(gs):
                s2 = gs % NR
                for rr, rrb in ((r1, r1b), (r2, r2b)):
                    for half in range(2):
                        op("act", lambda: nc.scalar.copy(out=rr[s2][:, half * 512:(half + 1) * 512], in_=zeros[:, :]),
                           reads=[b_const], writes=[rrb[s2]])
                dma("pool", lambda: nc.gpsimd.indirect_dma_start(
                    out=r1[s2][:, :], out_offset=None, in_=ys_scr,
                    in_offset=bass.IndirectOffsetOnAxis(ap=D1[:, gs:gs + 1], axis=0),
                    bounds_check=bc_reg, oob_is_err=False), writes=[r1b[s2]])
                dma("pool", lambda: nc.gpsimd.indirect_dma_start(
                    out=r2[s2][:, :], out_offset=None, in_=ys_scr,
                    in_offset=bass.IndirectOffsetOnAxis(ap=D2[:, gs:gs + 1], axis=0),
                    bounds_check=bc_reg, oob_is_err=False), writes=[r2b[s2]])
                dma("sp", lambda: nc.sync.dma_start(out=hh[s2][:, :], in_=h1_v[:, gs, :]), writes=[hhb[s2]])

            epsT2 = sb(pd, "epsT2", [128, 1], F32)
            rwb = Buf()
            op("pool", lambda: nc.gpsimd.memset(epsT2[:], EPS / (ALPHA * ALPHA)), writes=[rwb])
            op("dve", lambda: nc.vector.tensor_scalar(out=RW1[:, :], in0=RW1[:, :], scalar1=1.0 / ALPHA, scalar2=None, op0=ALU.mult),
               writes=[rwb])
            op("dve", lambda: nc.vector.tensor_scalar(out=RW2[:, :], in0=RW2[:, :], scalar1=1.0 / ALPHA, scalar2=None, op0=ALU.mult),
               writes=[rwb])
            cx.barrier()
            loads_d(0)
            loads_d(1)
            for gs in range(32):
                s2 = gs % NR
                if gs + 2 < 32:
                    loads_d(gs + 2)
                op("dve", lambda: nc.vector.scalar_tensor_tensor(out=hh[s2][:, :], in0=r1[s2][:, :], scalar=RW1[:, gs:gs + 1],
                                                                 in1=hh[s2][:, :], op0=ALU.mult, op1=ALU.add),
                   reads=[r1b[s2], hhb[s2]], writes=[hhb[s2]])
                op("dve", lambda: nc.vector.scalar_tensor_tensor(out=hh[s2][:, :], in0=r2[s2][:, :], scalar=RW2[:, gs:gs + 1],
                                                                 in1=hh[s2][:, :], op0=ALU.mult, op1=ALU.add),
                   reads=[r2b[s2], hhb[s2]], writes=[hhb[s2]])
                layer_norm(nc, op, hh[s2], hhb[s2], stats, mv, rstd, stb, g2, b2, wD, epsT2)
                dma("sp", lambda: nc.sync.dma_start(out=out_v[:, gs, :], in_=hh[s2][:, :]), reads=[hhb[s2]])
            cx.barrier()
    return nc


def fence(cx, op, nc, rstd_like=None):
    cx.barrier()


def layer_norm(nc, op, h, hb, stats, mv, rstd, stb, g, b, wB, epsT, gmul_on_dve=False):
    for half in range(2):
        op("dve", lambda: nc.vector.bn_stats(out=stats[:, half, :], in_=h[:, half * 512:(half + 1) * 512]),
           reads=[hb], writes=[stb])
    op("dve", lambda: nc.vector.bn_aggr(out=mv[:, :], in_=stats[:, :, :].rearrange("p a b -> p (a b)")), reads=[stb], writes=[stb])
    op("act", lambda: nc.scalar.activation(out=rstd[:, :], in_=mv[:, 1:2], func=AF.Sqrt, bias=epsT[:, 0:1]),
       reads=[stb, wB], writes=[stb])
    op("dve", lambda: nc.vector.reciprocal(out=rstd[:, :], in_=rstd[:, :]), reads=[stb], writes=[stb])
    op("dve", lambda: nc.vector.tensor_scalar(out=h[:, :], in0=h[:, :], scalar1=mv[:, 0:1], scalar2=rstd[:, 0:1],
                                              op0=ALU.subtract, op1=ALU.mult), reads=[stb, hb], writes=[hb])
    if gmul_on_dve:
        op("dve", lambda: nc.vector.tensor_tensor(out=h[:, :], in0=h[:, :], in1=g[:, :], op=ALU.mult), reads=[hb, wB], writes=[hb])
    else:
        op("pool", lambda: nc.gpsimd.tensor_tensor(out=h[:, :], in0=h[:, :], in1=g[:, :], op=ALU.mult), reads=[hb, wB], writes=[hb])
    op("pool", lambda: nc.gpsimd.tensor_tensor(out=h[:, :], in0=h[:, :], in1=b[:, :], op=ALU.add), reads=[hb, wB], writes=[hb])


def routing(nc, op, dma, R, Ind, Lg, Lgb, rb, ustr_bf, ones_bf, b_const, psf, bankbuf, next_bank,
            base, ebase, D1, D2, RW1, RW2, T, wB):
    V_ = nc.vector

    def dv(fn, extra_r=()):
        op("dve", fn, reads=[rb, Lgb] + list(extra_r), writes=[rb])
    lg = Lg[:, :, 0:4]
    le = Lg[:, :, 4:36].rearrange("p s (g e) -> p s g e", g=4)
    dv(lambda: V_.tensor_reduce(out=R["gmax"][:, :], in_=lg, axis=AX.X, op=ALU.max))
    dv(lambda: V_.tensor_tensor(out=R["ohg"][:, :, :], in0=lg, in1=R["gmax"][:, :].unsqueeze(2).to_broadcast([128, 4, 4]),
                                op=ALU.is_equal))
    dv(lambda: V_.tensor_tensor(out=R["eg"][:, :, :], in0=lg, in1=R["gmax"][:, :].unsqueeze(2).to_broadcast([128, 4, 4]),
                                op=ALU.subtract))
    op("act", lambda: nc.scalar.activation(out=R["eg"][:, :, :], in_=R["eg"][:, :, :], func=AF.Exp), reads=[rb], writes=[rb])
    dv(lambda: V_.tensor_reduce(out=R["sumg"][:, :], in_=R["eg"][:, :, :], axis=AX.X, op=ALU.add))
    dv(lambda: V_.reciprocal(out=R["gp"][:, :], in_=R["sumg"][:, :]))
    dv(lambda: V_.tensor_tensor(out=R["prod"][:, :, :, :], in0=le,
                                in1=R["ohg"][:, :, :].unsqueeze(3).to_broadcast([128, 4, 4, 8]), op=ALU.mult))
    dv(lambda: V_.tensor_reduce(out=R["sel"][:, :, :], in_=R["prod"][:, :, :, :].rearrange("p s g e -> p s e g"),
                                axis=AX.X, op=ALU.add))
    dv(lambda: V_.tensor_reduce(out=R["m1"][:, :], in_=R["sel"][:, :, :], axis=AX.X, op=ALU.max))
    dv(lambda: V_.tensor_tensor(out=R["oh1"][:, :, :], in0=R["sel"][:, :, :],
                                in1=R["m1"][:, :].unsqueeze(2).to_broadcast([128, 4, 8]), op=ALU.is_equal))
    dv(lambda: V_.scalar_tensor_tensor(out=R["sel2"][:, :, :], in0=R["oh1"][:, :, :], scalar=-1e30, in1=R["sel"][:, :, :],
                                       op0=ALU.mult, op1=ALU.add))
    dv(lambda: V_.tensor_reduce(out=R["m2"][:, :], in_=R["sel2"][:, :, :], axis=AX.X, op=ALU.max))
    dv(lambda: V_.tensor_tensor(out=R["oh2"][:, :, :], in0=R["sel2"][:, :, :],
                                in1=R["m2"][:, :].unsqueeze(2).to_broadcast([128, 4, 8]), op=ALU.is_equal))
    dv(lambda: V_.tensor_tensor(out=R["dm"][:, :], in0=R["m1"][:, :], in1=R["m2"][:, :], op=ALU.subtract))
    op("act", lambda: nc.scalar.activation(out=R["w1"][:, :], in_=R["dm"][:, :], func=AF.Sigmoid), reads=[rb], writes=[rb])
    dv(lambda: V_.tensor_scalar(out=R["w2"][:, :], in0=R["w1"][:, :], scalar1=-1.0, scalar2=1.0, op0=ALU.mult, op1=ALU.add))
    dv(lambda: V_.tensor_tensor(out=RW1[:, 4 * T:4 * T + 4], in0=R["w1"][:, :], in1=R["gp"][:, :], op=ALU.mult))
    dv(lambda: V_.tensor_tensor(out=RW2[:, 4 * T:4 * T + 4], in0=R["w2"][:, :], in1=R["gp"][:, :], op=ALU.mult))
    ohg_b = R["ohg"][:, :, :].unsqueeze(3).to_broadcast([128, 4, 4, 8])
    for nm, src in (("OH1", "oh1"), ("OH2", "oh2")):
        dv(lambda: V_.tensor_tensor(out=R[nm][:, :, :].rearrange("p s (g e) -> p s g e", g=4), in0=ohg_b,
                                    in1=R[src][:, :, :].unsqueeze(2).to_broadcast([128, 4, 4, 8]), op=ALU.mult))
    dv(lambda: V_.tensor_tensor(out=Ind[:, :, :], in0=R["OH1"][:, :, :], in1=R["OH2"][:, :, :], op=ALU.add))
    bR, bT = next_bank(), next_bank()
    ind2 = Ind[:, :, :].rearrange("p s e -> p (s e)")
    op("pe", lambda: nc.tensor.matmul(psf[:, bR, 0:128], ustr_bf[:, :], ind2, start=True, stop=True),
       reads=[rb, b_const], writes=[bankbuf[bR]])
    op("pe", lambda: nc.tensor.matmul(psf[:, bT, 0:128], ones_bf[:, :], ind2, start=True, stop=True),
       reads=[rb, b_const], writes=[bankbuf[bT]])
    for s in range(4):
        dv(lambda: V_.tensor_tensor(out=R["Rk"][:, s, :], in0=psf[:, bR, s * 32:(s + 1) * 32], in1=base[:, :], op=ALU.add),
           extra_r=[bankbuf[bR], wB])
        op("dve", lambda: V_.tensor_tensor(out=base[:, :], in0=base[:, :], in1=psf[:, bT, s * 32:(s + 1) * 32], op=ALU.add),
           reads=[rb, wB, bankbuf[bT]], writes=[rb, wB])
    dv(lambda: V_.tensor_scalar(out=R["ov"][:, :, :], in0=R["Rk"][:, :, :], scalar1=float(CAP) - 0.5, scalar2=1.0e6,
                                op0=ALU.is_ge, op1=ALU.mult))
    dv(lambda: V_.tensor_tensor(out=R["Rk"][:, :, :], in0=R["Rk"][:, :, :], in1=R["ov"][:, :, :], op=ALU.add))
    dv(lambda: V_.tensor_tensor(out=R["Rk"][:, :, :], in0=R["Rk"][:, :, :],
                                in1=ebase[:, :].unsqueeze(1).to_broadcast([128, 4, 32]), op=ALU.add), extra_r=[b_const])
    for nm, dst, Dk in (("OH1", "d1f", D1), ("OH2", "d2f", D2)):
        dv(lambda: V_.tensor_tensor(out=R[nm][:, :, :], in0=R[nm][:, :, :], in1=R["Rk"][:, :, :], op=ALU.mult))
        dv(lambda: V_.tensor_reduce(out=R[dst][:, :], in_=R[nm][:, :, :], axis=AX.X, op=ALU.add))
        dv(lambda: V_.tensor_copy(out=Dk[:, 4 * T:4 * T + 4], in_=R[dst][:, :]))


_NC_CACHE = {}


def _prep_core(c, x, shared):
    b, p = c // 2, c % 2
    xr = x[b, ::-1, :]
    blocks = [2 * i + p for i in range(NBLK)]
    rows_o = np.concatenate([np.arange(128 * a, 128 * a + 128) for a in blocks])
    xo = xr[rows_o]
    halo = np.zeros((64, D), np.float32)
    for i, a in enumerate(blocks):
        r0 = 128 * (a + 1)
        if r0 < S:
            halo[2 * i] = xr[r0]
            halo[2 * i + 1] = xr[r0 + 1]
    tri = np.where(np.arange(128)[None, :] <= np.arange(128)[:, None], MASKV, 0.0).astype(np.float32)
    if p == 0:
        mask = np.concatenate([tri, np.zeros((128, 128), np.float32)], axis=1)
    else:
        mask = np.concatenate([np.full((128, 128), MASKV, np.float32), tri], axis=1)
    ident = np.eye(128, dtype=np.float32)
    ustr = (np.arange(128)[:, None] < np.arange(128)[None, :]).astype(np.float32)
    ones = np.ones((128, 128), np.float32)
    eb = np.tile((np.arange(32, dtype=np.float32) * CAP)[None, :], (128, 1))
    m01 = (mask == 0.0).astype(np.float32)
    consts = np.ascontiguousarray(np.concatenate([ident, ustr, ones, mask, eb, m01], axis=1))
    m = dict(shared)
    m.update({
        "xT": np.ascontiguousarray(xr.T),
        "xTsh": np.ascontiguousarray(np.concatenate([xr.T[:, 1:], np.zeros((D, 1), np.float32)], axis=1)),
        "xTs": np.ascontiguousarray(xr[0:S:256].T),
        "xTo": np.ascontiguousarray(xo.T),
        "xTsho": np.ascontiguousarray(np.concatenate([xr, np.zeros((1, D), np.float32)], axis=0)[rows_o + 1].T),
        "xTh": np.ascontiguousarray(halo.T),
        "xo": np.ascontiguousarray(xo),
        "consts": consts,
    })
    return m


def _prep_shared(w_in, gate_bias, conv_w, w_branch_a, w_branch_b, w_out, ln1_g, ln1_b,
                 w_router_g, b_router_g, w_router_e, b_router_e, w_gate, w_up, w_down, ln2_g, ln2_b):
    f = lambda a: np.ascontiguousarray(np.asarray(a, dtype=np.float32))
    gb = f(gate_bias[0]).reshape(16, 128).T
    cwp = f(conv_w[0]).reshape(3, 4, 128).transpose(2, 1, 0).reshape(128, 12)
    return {
        "w_in": f(w_in[0]), "gbias": f(gb), "cw": f(cwp),
        "w_ba": f(w_branch_a[0]), "w_bb": f(w_branch_b[0]), "w_out": f(w_out[0]),
        "lnp": f(np.stack([ln1_g[0], ln1_b[0], ln2_g[0], ln2_b[0]], axis=0)),
        "w_r": f(np.concatenate([w_router_g[0], w_router_e[0]], axis=1)),
        "b_r": f(np.concatenate([b_router_g[0], b_router_e[0].reshape(-1)], axis=0)),
        "w_gate": f(w_gate[0]), "w_up": f(w_up[0]), "w_down": f(w_down[0]),
    }


def kernel(x, w_in, gate_bias, conv_w, w_branch_a, w_branch_b, w_out, ln1_g, ln1_b,
           w_router_g, b_router_g, w_router_e, b_router_e, w_gate, w_up, w_down, ln2_g, ln2_b):
    x = np.asarray(x, dtype=np.float32)
    shared = _prep_shared(w_in, gate_bias, conv_w, w_branch_a, w_branch_b, w_out, ln1_g, ln1_b,
                          w_router_g, b_router_g, w_router_e, b_router_e, w_gate, w_up, w_down, ln2_g, ln2_b)
    in_maps = [_prep_core(c, x, shared) for c in range(8)]
    nc = build(4)
    res = run_bass_kernel_spmd(nc, in_maps, core_ids=list(range(8)))
    out = np.zeros((4, S, D), np.float32)
    for c in range(8):
        b, p = c // 2, c % 2
        oc = np.asarray(res.results[c]["out"]).reshape(NOWN, D)
        for i in range(NBLK):
            a = 2 * i + p
            rr = np.arange(128 * a, 128 * a + 128)
            out[b, S - 1 - rr] = oc[128 * i:128 * i + 128]
    return out
```
